# Optimizing a Trainium2 kernel written in Bass

```python
import jax
import jax.numpy as jnp
from jax import lax
import numpy as np

D_MODEL = 2048
BATCH = 1
SEQ = 16384
DEPTH = 2

RW_HEADS = 8
RW_HEAD_DIM = 64
RW_WIDTH = RW_HEADS * RW_HEAD_DIM
RW_LORA_DECAY = 64
RW_LORA_ICLR = 64
RW_LORA_GATE = 128
RW_LORA_VRES = 32
RW_GN_EPS = 64e-5
ML_HEADS = 4
ML_QK_DIM = 128
ML_V_DIM = 256
ML_QK_WIDTH = ML_HEADS * ML_QK_DIM
ML_WIDTH = ML_HEADS * ML_V_DIM
ML_CONV = 4
ML_SOFTCAP = 15.0
HG_HEADS = 4
HG_EXPAND = 128
HG_HEAD_DIM = 128
HG_K_WIDTH = HG_HEADS * HG_EXPAND
HG_WIDTH = HG_HEADS * HG_HEAD_DIM
CHUNK = 64
MOE_GROUPS = 4
MOE_EXPERTS_PER_GROUP = 8
MOE_EXPERTS = MOE_GROUPS * MOE_EXPERTS_PER_GROUP
MOE_TOP_K = 2
MOE_D_FF = 1024
MOE_BLOCK = 128
NORM_EPS = 1e-6

RW_SPLITS = (RW_WIDTH, RW_WIDTH, RW_WIDTH, RW_LORA_DECAY, RW_LORA_ICLR, RW_LORA_GATE)
RW_COLS = sum(RW_SPLITS)
REST_SPLITS = (ML_QK_WIDTH, ML_QK_WIDTH, ML_WIDTH, ML_WIDTH, ML_HEADS, ML_HEADS,
               HG_K_WIDTH, HG_K_WIDTH, HG_WIDTH, HG_WIDTH,
               D_MODEL, D_MODEL, D_MODEL)
N_IN = RW_COLS + sum(REST_SPLITS)

kernel_name = 'hybrid_rwkv7_mlstm_hgrn2_hiermoe_adaln'


def rmsnorm(x, g):
    x32 = x.astype(jnp.float32)
    y = x32 * lax.rsqrt(jnp.mean(x32 * x32, axis=-1, keepdims=True) + NORM_EPS)
    return (y * g.astype(jnp.float32)).astype(x.dtype)


def head_rmsnorm(x, g):
    B, T, H, d = x.shape
    return rmsnorm(x, g.reshape(H, d)).reshape(B, T, H * d)


def split_cols(u, sizes):
    return jnp.split(u, np.cumsum(sizes)[:-1].tolist(), axis=-1)


def shift_right(x, n):
    return jnp.pad(x, ((0, 0), (n, 0), (0, 0)))[:, :x.shape[1]]


def causal_dwconv(x, w, b):
    K = w.shape[0]
    return b + sum(shift_right(x, K - 1 - j) * w[j] for j in range(K))


def softcap(x):
    return ML_SOFTCAP * jnp.tanh(x / ML_SOFTCAP)


def to_chunks(x):
    B, T, H = x.shape[:3]
    x = x.reshape(B, T // CHUNK, CHUNK, H, *x.shape[3:])
    return jnp.moveaxis(jnp.moveaxis(x, 1, 0), 2, 3)


def from_chunks(y):
    NC, B, H, L, d = y.shape
    return jnp.moveaxis(jnp.moveaxis(y, 3, 2), 0, 1).reshape(B, NC * L, H, d)


def rwkv7_recurrence(r, decay, k, v, a, b):
    B, T, H, N = r.shape

    def step(S, inp):
        r_t, w_t, k_t, v_t, a_t, b_t = inp
        sa = jnp.einsum('bhvk,bhk->bhv', S, a_t)
        S = S * w_t[:, :, None, :] + sa[..., None] * b_t[:, :, None, :] + v_t[..., None] * k_t[:, :, None, :]
        return S, jnp.einsum('bhvk,bhk->bhv', S, r_t)

    xs = tuple(jnp.moveaxis(t.astype(jnp.float32), 1, 0) for t in (r, decay, k, v, a, b))
    _, y = lax.scan(step, jnp.zeros((B, H, N, N), jnp.float32), xs)
    return jnp.moveaxis(y, 0, 1)


def rwkv7_branch(r, k, v, xw, xa, xg, v_first, vres, w0, w2, a0, a2, g2, k_k, k_a, r_k, ln_w, ln_b):
    B, T, _ = r.shape
    heads = lambda t: t.reshape(B, T, RW_HEADS, RW_HEAD_DIM)
    log_w = -jax.nn.softplus(-(w0 + jnp.tanh(xw) @ w2).astype(jnp.float32)) - 0.5
    decay = jnp.exp(-jnp.exp(log_w))
    iclr = jax.nn.sigmoid(a0 + xa @ a2)
    gate = jax.nn.sigmoid(xg) @ g2
    if vres is None:
        v_first = v
    else:
        v0, v1, v2 = vres
        v = v + (v_first - v) * jax.nn.sigmoid(v0 + (v @ v1) @ v2)
    kk = heads(k * k_k).astype(jnp.float32)
    kk = kk / jnp.maximum(jnp.sqrt(jnp.sum(kk * kk, axis=-1, keepdims=True)), 1e-12)
    k = k * (1.0 + (iclr - 1.0) * k_a)
    rh, kh, vh = heads(r), heads(k), heads(v)
    y = rwkv7_recurrence(rh, heads(decay), kh, vh, -kk, kk * heads(iclr))
    mu = jnp.mean(y, axis=-1, keepdims=True)
    var = jnp.mean(jnp.square(y - mu), axis=-1, keepdims=True)
    y = ((y - mu) * lax.rsqrt(var + RW_GN_EPS)).reshape(B, T, RW_WIDTH) * ln_w + ln_b
    bonus = jnp.sum(rh * kh * r_k.reshape(RW_HEADS, RW_HEAD_DIM), axis=-1, keepdims=True) * vh
    y = y.astype(r.dtype) + bonus.reshape(B, T, RW_WIDTH)
    return y * gate, v_first


def mlstm_chunkwise(q, k, v, log_i, log_f):
    B, T, H, Dk = q.shape
    Dv = v.shape[-1]
    causal = jnp.tril(jnp.ones((CHUNK, CHUNK), bool))

    def step(carry, inp):
        C, n, m = carry
        qc, kc, vc, li, lf = inp
        b = jnp.cumsum(lf, axis=-1)
        d = jnp.where(causal, b[..., :, None] - b[..., None, :] + li[..., None, :], -jnp.inf)
        m_inter = b + m[..., None]
        m_t = jnp.maximum(m_inter, jnp.max(d, axis=-1))
        s = jnp.einsum('bhtd,bhsd->bhts', qc, kc) * jnp.exp(d - m_t[..., None])
        inter = jnp.exp(m_inter - m_t)
        num = jnp.einsum('bhts,bhsv->bhtv', s, vc) + inter[..., None] * jnp.einsum('bhtd,bhdv->bhtv', qc, C)
        den = jnp.sum(s, axis=-1) + inter * jnp.einsum('bhtd,bhd->bht', qc, n)
        h = num / jnp.maximum(jnp.abs(den), jnp.exp(-m_t))[..., None]
        g = b[..., -1:] - b + li
        m_new = jnp.maximum(b[..., -1] + m, jnp.max(g, axis=-1))
        wgt = jnp.exp(g - m_new[..., None])
        keep = jnp.exp(b[..., -1] + m - m_new)
        C = keep[..., None, None] * C + jnp.einsum('bhs,bhsd,bhsv->bhdv', wgt, kc, vc)
        n = keep[..., None] * n + jnp.einsum('bhs,bhsd->bhd', wgt, kc)
        return (C, n, m_new), h

    xs = tuple(to_chunks(t.astype(jnp.float32)) for t in (q, k, v, log_i, log_f))
    init = (jnp.zeros((B, H, Dk, Dv), jnp.float32), jnp.zeros((B, H, Dk), jnp.float32), jnp.zeros((B, H), jnp.float32))
    _, h = lax.scan(step, init, xs)
    return from_chunks(h)


def mlstm_branch(q, k, v, o, i_pre, f_pre, norm_g):
    B, T, _ = q.shape
    qh = q.reshape(B, T, ML_HEADS, ML_QK_DIM)
    kh = k.reshape(B, T, ML_HEADS, ML_QK_DIM) * ML_QK_DIM ** -0.5
    vh = v.reshape(B, T, ML_HEADS, ML_V_DIM)
    log_i = softcap(i_pre.astype(jnp.float32))
    log_f = jax.nn.log_sigmoid(softcap(f_pre.astype(jnp.float32)))
    h = mlstm_chunkwise(qh, kh, vh, log_i, log_f)
    return head_rmsnorm(h.astype(q.dtype), norm_g) * jax.nn.sigmoid(o)


def hgrn2_chunkwise(q, k, i, log_f):
    B, T, H, Dk = q.shape
    Dv = i.shape[-1]
    causal = jnp.tril(jnp.ones((CHUNK, CHUNK), bool))

    def step(S, inp):
        qc, kc, ic, gc = inp
        cum = jnp.cumsum(gc, axis=2)
        diff = jnp.where(causal[..., None], cum[:, :, :, None, :] - cum[:, :, None, :, :], -jnp.inf)
        att = jnp.einsum('bhtd,bhtsd,bhsd->bhts', qc, jnp.exp(diff), kc)
        o = jnp.einsum('bhts,bhsv->bhtv', att, ic) + jnp.einsum('bhtd,bhdv->bhtv', qc * jnp.exp(cum), S)
        last = cum[:, :, -1:, :]
        S = jnp.exp(last[:, :, 0])[..., None] * S + jnp.einsum('bhsd,bhsv->bhdv', kc * jnp.exp(last - cum), ic)
        return S, o

    xs = tuple(to_chunks(t.astype(jnp.float32)) for t in (q, k, i, log_f))
    _, o = lax.scan(step, jnp.zeros((B, H, Dk, Dv), jnp.float32), xs)
    return from_chunks(o)


def hgrn2_branch(q, f_pre, i, g_pre, lb, norm_g):
    B, T, _ = q.shape
    f_pre = f_pre.astype(jnp.float32)
    lb = lb.astype(jnp.float32)
    log_f = jnp.logaddexp(jnp.log(lb), jnp.log1p(-lb) + jax.nn.log_sigmoid(f_pre))
    k = (1.0 - lb) * jax.nn.sigmoid(-f_pre)
    hk = lambda t: t.reshape(B, T, HG_HEADS, HG_EXPAND)
    o = hgrn2_chunkwise(hk(jax.nn.silu(q)), hk(k), i.reshape(B, T, HG_HEADS, HG_HEAD_DIM), hk(log_f))
    return head_rmsnorm(o.astype(q.dtype), norm_g) * jax.nn.silu(g_pre)


def hier_moe(h, gw, gb, ew, eb, w_gate, w_up, w_down):
    B, T, D = h.shape
    N = B * T
    A = N * MOE_TOP_K
    hf = h.reshape(N, D)
    g_logits = (hf @ gw + gb).astype(jnp.float32)
    g_prob = jax.nn.softmax(g_logits, axis=-1)
    _, g_idx = lax.top_k(g_logits, 1)
    p_group = jnp.take_along_axis(g_prob, g_idx, axis=-1)
    e_logits = (hf @ ew + eb).astype(jnp.float32).reshape(N, MOE_GROUPS, MOE_EXPERTS_PER_GROUP)
    e_logits = jnp.take_along_axis(e_logits, g_idx[:, :, None], axis=1)[:, 0]
    e_top, e_local = lax.top_k(e_logits, MOE_TOP_K)
    weights = p_group * jax.nn.softmax(e_top, axis=-1)
    expert = g_idx * MOE_EXPERTS_PER_GROUP + e_local
    flat = expert.reshape(A)
    order = jnp.argsort(flat)
    sorted_e = flat[order]
    counts = jnp.bincount(flat, length=MOE_EXPERTS)
    padded = (counts + MOE_BLOCK - 1) // MOE_BLOCK * MOE_BLOCK
    pad_end = jnp.cumsum(padded)
    start = jnp.cumsum(counts) - counts
    dest_sorted = pad_end[sorted_e] - padded[sorted_e] + jnp.arange(A, dtype=jnp.int32) - start[sorted_e]
    dest = jnp.zeros((A,), jnp.int32).at[order].set(dest_sorted.astype(jnp.int32))
    n_blocks = -(-A // MOE_BLOCK) + MOE_EXPERTS
    rows = n_blocks * MOE_BLOCK
    row_token = jnp.zeros((rows,), jnp.int32).at[dest].set(jnp.arange(A, dtype=jnp.int32) // MOE_TOP_K)
    block_start = jnp.arange(n_blocks, dtype=jnp.int32) * MOE_BLOCK
    block_expert = jnp.minimum(jnp.searchsorted(pad_end, block_start, side='right'), MOE_EXPERTS - 1)
    xb = hf[row_token].reshape(n_blocks, MOE_BLOCK, D)

    def expert_block(args):
        xe, e = args
        return (jax.nn.silu(xe @ w_gate[e]) * (xe @ w_up[e])) @ w_down[e]

    yb = lax.map(expert_block, (xb, block_expert)).reshape(rows, D)
    y = yb[dest].reshape(N, MOE_TOP_K, D)
    out = jnp.einsum('nk,nkd->nd', weights.astype(y.dtype), y)
    return out.reshape(B, T, D)


def setup_inputs(seed: int = 0) -> dict:
    key = jax.random.key(seed)
    ks = iter(jax.random.split(key, 64))

    def nrm(shape, scale):
        return scale * jax.random.normal(next(ks), shape, jnp.float32)

    def uni(shape, lo, hi):
        return jax.random.uniform(next(ks), shape, jnp.float32, lo, hi)

    def gain(shape):
        return 1.0 + nrm(shape, 0.02)

    L, D = DEPTH, D_MODEL
    return {
        'x': nrm((BATCH, SEQ, D), 1.0),
        'c': nrm((BATCH, D), 1.0),
        'ada_w': nrm((L, D, 6 * D), 0.5 * D ** -0.5),
        'ada_b': nrm((L, 6 * D), 0.01),
        'norm_mix': gain((L, D)),
        'norm_ffn': gain((L, D)),
        'w_in': nrm((L, D, N_IN), D ** -0.5),
        'rw_mu': uni((L, RW_COLS), 0.0, 1.0),
        'rw_w0': uni((L, RW_WIDTH), -6.0, 0.0),
        'rw_w2': nrm((L, RW_LORA_DECAY, RW_WIDTH), 0.5 * RW_LORA_DECAY ** -0.5),
        'rw_a0': nrm((L, RW_WIDTH), 0.1),
        'rw_a2': nrm((L, RW_LORA_ICLR, RW_WIDTH), 0.5 * RW_LORA_ICLR ** -0.5),
        'rw_g2': nrm((L, RW_LORA_GATE, RW_WIDTH), RW_LORA_GATE ** -0.5),
        'rw_kk': 0.85 + nrm((L, RW_WIDTH), 0.02),
        'rw_ka': gain((L, RW_WIDTH)),
        'rw_rk': nrm((L, RW_WIDTH), 0.1),
        'rw_lnw': gain((L, RW_WIDTH)),
        'rw_lnb': nrm((L, RW_WIDTH), 0.02),
        'rw_v0': nrm((L - 1, RW_WIDTH), 0.1),
        'rw_v1': nrm((L - 1, RW_WIDTH, RW_LORA_VRES), RW_WIDTH ** -0.5),
        'rw_v2': nrm((L - 1, RW_LORA_VRES, RW_WIDTH), 0.5 * RW_LORA_VRES ** -0.5),
        'ml_conv_w': nrm((L, ML_CONV, 2 * ML_QK_WIDTH), ML_CONV ** -0.5),
        'ml_conv_b': nrm((L, 2 * ML_QK_WIDTH), 0.02),
        'ml_ib': nrm((L, ML_HEADS), 0.1),
        'ml_fb': uni((L, ML_HEADS), 3.0, 6.0),
        'ml_norm': gain((L, ML_WIDTH)),
        'hg_lb': nrm((L, HG_K_WIDTH), 1.0),
        'hg_norm': gain((L, HG_WIDTH)),
        'p_a': nrm((L, RW_WIDTH, D), RW_WIDTH ** -0.5),
        'p_b': nrm((L, ML_WIDTH, D), ML_WIDTH ** -0.5),
        'p_c': nrm((L, HG_WIDTH, D), HG_WIDTH ** -0.5),
        'w_out': nrm((L, D, D), D ** -0.5),
        'moe_gw': nrm((L, D, MOE_GROUPS), D ** -0.5),
        'moe_gb': nrm((L, MOE_GROUPS), 0.01),
        'moe_ew': nrm((L, D, MOE_EXPERTS), D ** -0.5),
        'moe_eb': nrm((L, MOE_EXPERTS), 0.01),
        'ex_gate': nrm((L, MOE_EXPERTS, D, MOE_D_FF), D ** -0.5),
        'ex_up': nrm((L, MOE_EXPERTS, D, MOE_D_FF), D ** -0.5),
        'ex_down': nrm((L, MOE_EXPERTS, MOE_D_FF, D), MOE_D_FF ** -0.5),
        'final_norm': gain((D,)),
    }


def reference(x, c, ada_w, ada_b, norm_mix, norm_ffn, w_in, rw_mu, rw_w0, rw_w2, rw_a0, rw_a2, rw_g2,
              rw_kk, rw_ka, rw_rk, rw_lnw, rw_lnb, rw_v0, rw_v1, rw_v2, ml_conv_w, ml_conv_b, ml_ib, ml_fb,
              ml_norm, hg_lb, hg_norm, p_a, p_b, p_c, w_out, moe_gw, moe_gb, moe_ew, moe_eb,
              ex_gate, ex_up, ex_down, final_norm):
    lbs = jnp.cumsum(jax.nn.softmax(hg_lb.astype(jnp.float32), axis=0), axis=0)
    lbs = lbs - lbs[:1]
    cond = jax.nn.silu(c)
    v_first = None
    for l in range(DEPTH):
        mod = (cond @ ada_w[l] + ada_b[l])[:, None, :]
        sh_m, sc_m, gt_m, sh_f, sc_f, gt_f = jnp.split(mod, 6, axis=-1)
        h = rmsnorm(x, norm_mix[l]) * (1.0 + sc_m) + sh_m
        u = h @ w_in[l]
        u_rw = u[..., :RW_COLS]
        u_rw = u_rw + rw_mu[l] * (shift_right(u_rw, 1) - u_rw)
        rw_r, rw_k, rw_v, rw_xw, rw_xa, rw_xg = split_cols(u_rw, RW_SPLITS)
        (ml_q, ml_k, ml_v, ml_o, ml_i, ml_f, hg_q, hg_f, hg_i, hg_g,
         g_a, g_b, g_c) = split_cols(u[..., RW_COLS:], REST_SPLITS)
        vres = None if l == 0 else (rw_v0[l - 1], rw_v1[l - 1], rw_v2[l - 1])
        y_a, v_first = rwkv7_branch(rw_r, rw_k, rw_v, rw_xw, rw_xa, rw_xg, v_first, vres,
                                    rw_w0[l], rw_w2[l], rw_a0[l], rw_a2[l], rw_g2[l],
                                    rw_kk[l], rw_ka[l], rw_rk[l], rw_lnw[l], rw_lnb[l])
        ml_qk = jax.nn.silu(causal_dwconv(jnp.concatenate([ml_q, ml_k], axis=-1), ml_conv_w[l], ml_conv_b[l]))
        ml_q, ml_k = jnp.split(ml_qk, 2, axis=-1)
        y_b = mlstm_branch(ml_q, ml_k, ml_v, ml_o, ml_i + ml_ib[l], ml_f + ml_fb[l], ml_norm[l])
        y_c = hgrn2_branch(hg_q, hg_f, hg_i, hg_g, lbs[l], hg_norm[l])
        y = (jax.nn.sigmoid(g_a) * (y_a @ p_a[l])
             + jax.nn.sigmoid(g_b) * (y_b @ p_b[l])
             + jax.nn.sigmoid(g_c) * (y_c @ p_c[l])) @ w_out[l]
        x = x + gt_m * y
        h = rmsnorm(x, norm_ffn[l]) * (1.0 + sc_f) + sh_f
        x = x + gt_f * hier_moe(h, moe_gw[l], moe_gb[l], moe_ew[l], moe_eb[l], ex_gate[l], ex_up[l], ex_down[l])
    return rmsnorm(x, final_norm)
```

```python
import math
CUT, PJ, PX = 99, 5, 1
P2CUT = 99
from contextlib import ExitStack
import numpy as np
import concourse.bass as bass
import concourse.mybir as mybir
from concourse.bass_utils import run_bass_kernel_spmd

F32 = mybir.dt.float32
BF16 = mybir.dt.bfloat16
AF = mybir.ActivationFunctionType
ALU = mybir.AluOpType
AX = mybir.AxisListType

D = 2048
SEQ = 16384
NCORES = 8
C0 = math.exp(-0.5)
EPS = 1e-6


class V:
    __slots__ = ("ap", "keys")

    def __init__(self, ap, keys):
        self.ap = ap
        self.keys = tuple(keys)

    def __getitem__(self, idx):
        return V(self.ap[idx], self.keys)

    def k(self, *keys):
        return V(self.ap, keys)


class KB:
    def __init__(self, nc, es):
        self.nc = nc
        self.es = es
        self.E = {"pe": nc.tensor, "act": nc.scalar, "dve": nc.vector, "pool": nc.gpsimd, "sp": nc.sync}
        self.sem = {}
        self.cnt = {}
        for e in ("pe", "act", "dve", "pool"):
            self.sem[e] = es.enter_context(nc.semaphore("s_" + e))
            self.cnt[e] = 0
        self.waited = {e: {} for e in self.E}
        self.buf = {}
        self.dsem = {}
        self.dcnt = {}
        self.dq = {}
        self.main_es = es

    def sb(self, name, shape, dt=F32):
        t = self.es.enter_context(self.nc.sbuf_tensor("sb_" + name, list(shape), dt))
        return V(t[:], (name,))

    def ps(self, name, shape, dt=F32):
        t = self.es.enter_context(self.nc.psum_tensor("ps_" + name, list(shape), dt))
        return V(t[:], (name,))

    def _deps(self, eng, outs, ins):
        toks = set()
        for v in ins:
            for key in v.keys:
                b = self.buf.get(key)
                if b is not None and b[0] is not None:
                    toks.add(b[0])
        for v in outs:
            for key in v.keys:
                b = self.buf.get(key)
                if b is not None:
                    if b[0] is not None:
                        toks.add(b[0])
                    toks.update(b[1].values())
        w = self.waited[eng]
        for (sname, val, owner) in sorted(toks):
            if owner == "pe" and eng == "pe":
                continue
            if w.get(sname, 0) < val:
                sem = self.sem[sname] if sname in self.sem else self.dsem[sname]
                self.E[eng].wait_ge(sem, val)
                w[sname] = val

    def _record(self, tok, outs, ins):
        for v in ins:
            for key in v.keys:
                b = self.buf.setdefault(key, [None, {}])
                b[1][tok[0]] = tok
        for v in outs:
            for key in v.keys:
                self.buf[key] = [tok, {}]

    def op(self, eng, fn, outs, ins):
        self._deps(eng, outs, ins)
        ins_obj = fn()
        self.cnt[eng] += 1
        ins_obj.then_inc(self.sem[eng], 1)
        tok = (eng, self.cnt[eng], eng)
        self._record(tok, outs, ins)

    NDSEM = 6

    def dma(self, q, out, in_, stream=None):
        n = self.dq.get(q, 0)
        self.dq[q] = n + 1
        stream = f"{q}{n % self.NDSEM}"
        if stream not in self.dsem:
            self.dsem[stream] = self.main_es.enter_context(self.nc.semaphore("d_" + stream))
            self.dcnt[stream] = 0
        self._deps(q, [out], [in_])
        w = self.waited[q]
        if w.get(stream, 0) < self.dcnt[stream]:
            self.E[q].wait_ge(self.dsem[stream], self.dcnt[stream])
            w[stream] = self.dcnt[stream]
        self.dcnt[stream] += 16
        self.E[q].dma_start(out=out.ap, in_=in_.ap).then_inc(self.dsem[stream], 16)
        tok = (stream, self.dcnt[stream], "dma")
        self._record(tok, [out], [in_])

    def wait_all(self, eng, vs):
        self._deps(eng, vs, vs)

    def mm(self, out, lhsT, rhs, start=True, stop=True):
        self.op("pe", lambda: self.nc.tensor.matmul(out.ap, lhsT=lhsT.ap, rhs=rhs.ap, start=start, stop=stop),
                [out], [lhsT, rhs])

    def tr(self, out, in_, ident):
        self.op("pe", lambda: self.nc.tensor.transpose(out.ap, in_.ap, ident.ap), [out], [in_, ident])

    def act(self, out, in_, func, bias=None, scale=None, accum=None):
        ins = [in_]
        kw = {}
        if bias is not None:
            if isinstance(bias, V):
                ins.append(bias)
                kw["bias"] = bias.ap
            else:
                kw["bias"] = float(bias)
        if scale is not None:
            if isinstance(scale, V):
                ins.append(scale)
                kw["scale"] = scale.ap
            else:
                kw["scale"] = float(scale)
        outs = [out]
        if accum is not None:
            outs.append(accum)
            kw["accum_out"] = accum.ap
        self.op("act", lambda: self.nc.scalar.activation(out=out.ap, in_=in_.ap, func=func, **kw), outs, ins)

    def ts(self, eng, out, in0, s1, s2=None, op0=ALU.mult, op1=None):
        ins = [in0]
        a1 = s1.ap if isinstance(s1, V) else float(s1)
        if isinstance(s1, V):
            ins.append(s1)
        kw = {}
        if op1 is not None:
            a2 = s2.ap if isinstance(s2, V) else float(s2)
            if isinstance(s2, V):
                ins.append(s2)
            kw["op1"] = op1
        else:
            a2 = None
        self.op(eng, lambda: self.E[eng].tensor_scalar(out=out.ap, in0=in0.ap, scalar1=a1, scalar2=a2, op0=op0, **kw),
                [out], ins)

    def tt(self, eng, out, in0, in1, op):
        self.op(eng, lambda: self.E[eng].tensor_tensor(out=out.ap, in0=in0.ap, in1=in1.ap, op=op), [out], [in0, in1])

    def stt(self, out, in0, scalar, in1, op0, op1):
        ins = [in0, in1]
        a = scalar.ap if isinstance(scalar, V) else float(scalar)
        if isinstance(scalar, V):
            ins.append(scalar)
        self.op("dve", lambda: self.nc.vector.scalar_tensor_tensor(out=out.ap, in0=in0.ap, scalar=a, in1=in1.ap,
                                                                   op0=op0, op1=op1), [out], ins)

    def cp(self, eng, out, in_):
        if eng == "act":
            self.act(out, in_, AF.Copy)
        else:
            self.op(eng, lambda: self.E[eng].tensor_copy(out=out.ap, in_=in_.ap), [out], [in_])

    def scan(self, out, d0, d1, init, op0, op1):
        self.op("dve", lambda: self.nc.vector.tensor_tensor_scan(out=out.ap, data0=d0.ap, data1=d1.ap, initial=init,
                                                                 op0=op0, op1=op1), [out], [d0, d1])

    def memset(self, eng, out, val):
        self.op(eng, lambda: self.E[eng].memset(out.ap, val), [out], [])

    def recip(self, out, in_):
        self.op("dve", lambda: self.nc.vector.reciprocal(out=out.ap, in_=in_.ap), [out], [in_])

    def barrier(self):
        for eng in self.E:
            w = self.waited[eng]
            for e2, sem in self.sem.items():
                if e2 != eng and self.cnt[e2] > w.get(e2, 0):
                    self.E[eng].wait_ge(sem, self.cnt[e2])
                    w[e2] = self.cnt[e2]
            for st_, sem in self.dsem.items():
                if self.dcnt[st_] > w.get(st_, 0):
                    self.E[eng].wait_ge(sem, self.dcnt[st_])
                    w[st_] = self.dcnt[st_]

    def finish(self, outs):
        self._deps("sp", outs, outs)


def dram_in(nc, name, shape, dt=F32):
    return V(nc.dram_tensor(name, list(shape), dt, kind="ExternalInput").ap(), ("dram_" + name,))


def dram_out(nc, name, shape, dt=F32):
    return V(nc.dram_tensor(name, list(shape), dt, kind="ExternalOutput").ap(), ("dram_" + name,))


TB = 256
PC_N = 96
PC_G, PC_SC, PC_SH = 0, 16, 32
PC_MU = 48
PC_W0, PC_A0, PC_KK, PC_KA, PC_RK, PC_LNW, PC_LNB, PC_V0 = 54, 55, 56, 57, 58, 59, 60, 61
PC_CQ, PC_CK, PC_CBQ, PC_CBK = 62, 66, 70, 71
PC_IB, PC_FB = 72, 73
PC_LB0, PC_LB1 = 74, 75
PC_MUV = 76
PM_W2, PM_A2, PM_G2, PM_V2, PM_V1 = 0, 64, 128, 192, 256
PM_ID, PM_ONES, PM_M5, PM_MM, PM_TRI, PM_MH, PM_RST = 384, 512, 640, 960, 1088, 1216, 1280
PM_RSTH = PM_RST + TB
PM_N = PM_RSTH + TB
LH = 32


def p1_cols(layer):
    cols = [("r", 64), ("k", 64), ("v", 64), ("xw", 64), ("xa", 64), ("xg", 128),
            ("mq", 128), ("mk", 128), ("mv", 128), ("mif", 2),
            ("hq", 128), ("hf", 128), ("hi", 64)]
    if layer == 1:
        cols += [("va0", 128), ("va1", 128), ("va2", 128), ("va3", 128)]
    return cols


def build_p1(layer, T, stage=99):
    nc = bass.Bass("TRN2", target_bir_lowering=False)
    cols = p1_cols(layer)
    NC1 = sum(m for _, m in cols)
    coff = {}
    o = 0
    for n, m in cols:
        coff[n] = (o, m)
        o += m
    NB = T // TB
    NCH = TB // 64
    NCM = TB // 128
    NCHH = TB // LH
    x_d = dram_in(nc, "x", [T, D])
    w1_d = dram_in(nc, "w1", [D, NC1])
    pc_d = dram_in(nc, "pc", [128, PC_N])
    pm_d = dram_in(nc, "pm", [128, PM_N])
    if layer == 1:
        vf_d = dram_in(nc, "vfirst", [64, T])
    ya_d = dram_out(nc, "yaT", [64, T])
    if layer == 0:
        vo_d = dram_out(nc, "vT", [64, T])
    ob_d = dram_out(nc, "obT", [128, T])
    oc_d = dram_out(nc, "ocT", [64, T])
    ssb_d = dram_out(nc, "ssb", [128, T // 128])
    ssc_d = dram_out(nc, "ssc", [LH, T // LH])

    with ExitStack() as es:
        kb = KB(nc, es)
        sb, ps = kb.sb, kb.ps
        pc = sb("pc", [128, PC_N])
        pm = sb("pm", [128, PM_N])
        Wb = sb("Wb", [128, 16, NC1], BF16)
        kb.dma("sp", pc, pc_d, "pc")
        kb.dma("sp", pm, pm_d, "pm")
        w1v = V(w1_d.ap.rearrange("(kc p) n -> p kc n", p=128), w1_d.keys)
        for kc in range(16):
            kb.dma("pool", Wb[:, kc, :], w1v[:, kc, :], "w1")
        ident = pm[:, PM_ID:PM_ID + 128]
        ones = pm[:, PM_ONES:PM_ONES + 128]
        col = lambda j, n=128: pc[0:n, j:j + 1]

        Acol = sb("Acol", [128, 16])
        kb.stt(Acol, pc[:, PC_SC:PC_SC + 16], 1.0, pc[:, PC_G:PC_G + 16], ALU.add, ALU.mult)
        Bcol = pc[:, PC_SH:PC_SH + 16]
        omka = sb("omka", [64, 1])
        kb.ts("dve", omka, col(PC_KA, 64), -1.0, 1.0, ALU.mult, ALU.add)
        ib15 = sb("ib15", [128, 1])
        kb.ts("dve", ib15, col(PC_IB), 1.0 / 15.0)
        fb15 = sb("fb15", [128, 1])
        kb.ts("dve", fb15, col(PC_FB), 1.0 / 15.0)
        lb = sb("lb", [128, 1])
        oml = sb("oml", [128, 1])
        if layer == 0:
            kb.memset("dve", lb, 0.0)
        else:
            dlb = sb("dlb", [128, 1])
            kb.tt("dve", dlb, col(PC_LB1), col(PC_LB0), ALU.subtract)
            kb.act(lb, dlb, AF.Sigmoid)
        kb.ts("dve", oml, lb, -1.0, 1.0, ALU.mult, ALU.add)

        xs = [sb(f"xs{i}", [128, TB // 128, D]) for i in range(2)]
        junk = sb("junk", [128, D], BF16)
        ss = sb("ss", [128, 4])
        rstd = sb("rstd", [128, 4])
        hT = [sb("hT0", [128, 16, TB], BF16)] * 2
        psT = [ps(f"psT{i}", [128, 512]) for i in range(2)]
        psP = [ps(f"psP{i}", [128, 512]) for i in range(2)]
        psR = ps("psR", [128, 512])
        psR2 = ps("psR2", [128, 512])
        psM = ps("psM", [128, 512])
        psH = ps("psH", [128, 512])
        qa = psR[0:64, 0:320].k("psR")
        qb = psR[0:64, 320:384].k("psR")
        qc = psR[0:64, 384:448].k("psR")
        qd = psR[0:64, 448:512].k("psR")
        qblk0 = psR[0:64, 0:TB].k("psR")
        qblk1 = psR[0:64, 256:256 + TB].k("psR")
        sa = psR2[0:64, 0:128].k("psR2")
        sbx = psR2[0:64, 128:192].k("psR2")
        sc_ = psR2[0:64, 192:256].k("psR2")
        sd = psR2[0:64, 256:448].k("psR2")
        se = psR2[0:64, 448:512].k("psR2")
        sblk0 = psR2[0:64, 0:TB].k("psR2")
        sblk1 = psR2[0:64, 256:256 + TB].k("psR2")
        mA = psM[:, 0:129].k("psM")
        mG = psM[:, 132:136].k("psM")
        hS = psM[:, 136:200].k("psM")
        hO = psM[0:64, 200:200 + LH].k("psM")
        hA = psM[0:LH, 264:264 + LH].k("psM")
        hT2 = psH[0:LH, 0:192].k("psH")
        ho = psH[0:LH, 192:256].k("psH")
        mB = psT[0][:, 256:384].k("psT0")
        mC = psT[1][:, 256:384].k("psT1")
        mD = psP[0][:, 256:385].k("psP0")
        mE = psP[1][:, 256:384].k("psP1")

        HAL = {"r": 1, "k": 1, "v": 1, "xw": 1, "xa": 1, "xg": 1, "mq": 3, "mk": 3,
               "va0": 1, "va1": 1, "va2": 1, "va3": 1}
        raw = {}
        for n, m in cols:
            h = HAL.get(n, 0)
            raw[n] = sb("raw_" + n, [m, h + TB])
            if h:
                kb.memset("pool", raw[n][:, 0:h], 0.0)

        def newt(name, m=64, w=TB):
            return sb(name, [m, w])

        tmp64 = newt("tmp64")
        tmp128 = newt("tmp128", 128)
        r_, k0, v_, xw, xa = [newt("rw_" + n) for n in ("r", "k0", "v", "xw", "xa")]
        xg = newt("rw_xg", 128)
        sg, cumS, p_, pinv, pprev, dcl = [newt("rw_" + n) for n in ("sg", "cumS", "p", "pinv", "pprev", "dcl")]
        txw = xw
        cumP = xa
        pLp = dcl
        pL = newt("rw_pL", 64, NCH)
        icl, kk, rn, t1, bt_, Bp = [newt("rw_" + n) for n in ("icl", "kk", "rn", "t1", "bt", "Bp")]
        kk2 = tmp64
        kkn = kk
        kmod = t1
        bvec = icl
        at_ = pprev
        kt_ = pinv
        rt_ = p_
        Kp = dcl
        sxg = xg
        rk = tmp64
        gate, bonus, ynT = [newt("rw_" + n) for n in ("gate", "bonus", "ynT")]
        yf = ynT
        if layer == 1:
            vall = sb("rw_vall", [128, 4, TB])
            vv1 = newt("rw_vv1", 32)
            vg, vfb, dv = [newt("rw_" + n) for n in ("vg", "vfb", "dv")]
        A5 = sb("rw_A5", [64, 320])
        PT = [sb(f"rw_PT{j}", [64, 128]) for j in range(5)]
        XT = [sb(f"rw_XT{j}", [64, 64]) for j in range(2)]
        tok3 = sb("rw_tok3", [64, 192])
        M0 = sb("rw_M0", [64, 64])
        U_ = sb("rw_U", [64, 64])
        Hst = sb("rw_H", [64, 64])
        kb.memset("pool", Hst, 0.0)
        Ysb = sb("rw_Y", [64, 64])
        ysq = sb("rw_ysq", [64, 64])
        st = sb("rw_st", [64, 8])
        yn = sb("rw_yn", [64, 64])

        mq, mk = [newt("ml_" + n, 128) for n in ("q", "k")]
        mq_acc, mk_acc = mq, mk
        gif = sb("ml_gif", [128, 2])
        mg = sb("ml_g", [128, 12])
        S0T = sb("ml_S0T", [128, 128])
        Vp = sb("ml_Vp", [128, 132])
        kb.memset("pool", Vp, 1.0)
        ktg = sb("ml_ktg", [128, 128])
        numE = sb("ml_numE", [128, 132])
        mh = sb("ml_h", [128, 128])
        mjunk = sb("ml_junk", [128, 128])
        Cst = sb("ml_C", [128, 132])
        kb.memset("pool", Cst, 0.0)
        obuf = newt("ml_obuf", 128)
        ssb_all = sb("ssb_all", [128, T // 128])
        kb.memset("pool", ssb_all, 0.0)

        hsf, hkraw, hcum, hp, hpinv, hdcl, hqs = [newt("hg_" + n, 128) for n in (
            "sf", "kraw", "cum", "p", "pinv", "dcl", "qs")]
        hf_ = hsf
        hlogf = hsf
        hpLp = hdcl
        hqt = hqs
        hkt = hpinv
        hkp = hdcl
        hpL = newt("hg_pL", 128, NCHH)
        attT = sb("hg_attT", [LH, LH])
        tok2 = sb("hg_tok2", [LH, 192])
        Sst = sb("hg_S", [128, 64])
        kb.memset("pool", Sst, 0.0)
        osb = sb("hg_o", [LH, 64])
        hjunk = sb("hg_junk", [LH, 64])
        ocbuf = newt("hg_ocbuf", 64)
        ssc_all = sb("ssc_all", [LH, T // LH])
        kb.memset("pool", ssc_all, 0.0)

        m5 = pm[0:64, PM_M5:PM_M5 + 320]
        maskM = pm[:, PM_MM:PM_MM + 128]
        triM = pm[:, PM_TRI:PM_TRI + 128]
        maskH = pm[0:LH, PM_MH:PM_MH + LH]
        rstH = pm[:, PM_RSTH:PM_RSTH + TB]
        idLH = pm[0:LH, PM_ID:PM_ID + LH]
        rst64 = pm[0:64, PM_RST:PM_RST + TB]
        rst128 = pm[:, PM_RST:PM_RST + TB]
        id64 = pm[0:64, PM_ID:PM_ID + 64]
        ones64 = pm[0:64, PM_ONES:PM_ONES + 64]

        evac_i = [0]

        def evac(out, in_):
            e = ("act", "dve")[evac_i[0] % 2]
            evac_i[0] += 1
            kb.cp(e, out, in_)

        def shift(out, rawt, mucol, m, eng="pool"):
            t = tmp64 if m == 64 else tmp128
            kb.tt(eng, t, rawt[:, 0:TB], rawt[:, 1:1 + TB], ALU.subtract)
            kb.stt(out, t, mucol, rawt[:, 1:1 + TB], ALU.mult, ALU.add)

        for b in range(NB):
            t0 = b * TB
            sl = b % 2
            X = xs[sl]
            H = hT[sl]
            if b == 0:
                for tt in range(TB // 128):
                    kb.dma("sp", X[:, tt, :], x_d[tt * 128:(tt + 1) * 128, :], f"x{sl}{tt}")
            if b + 1 < NB:
                for tt in range(TB // 128):
                    kb.dma("sp", xs[1 - sl][:, tt, :], x_d[t0 + TB + tt * 128:t0 + TB + (tt + 1) * 128, :],
                           f"x{1 - sl}{tt}")
            for tt in range(TB // 128):
                kb.act(junk, X[:, tt, :], AF.Square, accum=ss[:, tt:tt + 1])
                kb.act(rstd[:, tt:tt + 1], ss[:, tt:tt + 1], AF.Sqrt, bias=EPS, scale=1.0 / D)
                kb.recip(rstd[:, tt:tt + 1], rstd[:, tt:tt + 1])
                kb.ts("pool", X[:, tt, :], X[:, tt, :], rstd[:, tt:tt + 1])
            for kc in range(16):
                pT = psT[kc % 2][:, 0:TB].k(f"psT{kc % 2}")
                for tt in range(TB // 128):
                    kb.tr(pT[:, tt * 128:(tt + 1) * 128], X[:, tt, kc * 128:(kc + 1) * 128], ident)
                if kc % 2 == 0:
                    kb.act(H[:, kc, :], pT[:, 0:TB], AF.Identity, bias=Bcol[:, kc:kc + 1], scale=Acol[:, kc:kc + 1])
                else:
                    kb.ts("dve", H[:, kc, :], pT[:, 0:TB], Acol[:, kc:kc + 1], Bcol[:, kc:kc + 1], ALU.mult, ALU.add)
            for ci, (n, m) in enumerate(cols):
                off = coff[n][0]
                pp = psP[ci % 2][:, 0:TB].k(f"psP{ci % 2}")
                for kc in range(16):
                    kb.mm(pp[0:m, 0:TB], Wb[:, kc, off:off + m], H[:, kc, :], start=(kc == 0), stop=(kc == 15))
                h = HAL.get(n, 0)
                evac(raw[n][:, h:h + TB], pp[0:m, 0:TB])

            if stage < 1:
                continue
            shift(r_, raw["r"], col(PC_MU + 0, 64), 64)
            shift(k0, raw["k"], col(PC_MU + 1, 64), 64)
            shift(v_, raw["v"], col(PC_MU + 2, 64), 64)
            shift(xw, raw["xw"], col(PC_MU + 3, 64), 64)
            shift(xa, raw["xa"], col(PC_MU + 4, 64), 64)
            shift(xg, raw["xg"], col(PC_MU + 5, 128), 128)
            kb.act(txw, xw, AF.Tanh)
            kb.mm(qblk0, pm[0:64, PM_W2:PM_W2 + 64], txw)
            kb.act(sg, qblk0, AF.Sigmoid, bias=col(PC_W0, 64))
            kb.scan(cumS, rst64, sg, 0.0, ALU.mult, ALU.add)
            kb.mm(qblk1, pm[0:64, PM_A2:PM_A2 + 64], xa)
            kb.act(icl, qblk1, AF.Sigmoid, bias=col(PC_A0, 64))
            kb.tt("pool", cumP, cumS, sg, ALU.subtract)
            kb.act(p_, cumS, AF.Exp, scale=-C0)
            kb.act(pinv, cumS, AF.Exp, scale=C0)
            kb.act(pprev, cumP, AF.Exp, scale=-C0)
            cum3 = V(cumS.ap.rearrange("p (c l) -> p c l", l=64), cumS.keys)
            dcl3 = V(dcl.ap.rearrange("p (c l) -> p c l", l=64), dcl.keys)
            lastb = V(cum3.ap[:, :, 63:64].to_broadcast([64, NCH, 64]), cumS.keys)
            kb.tt("dve", dcl3, lastb, cum3, ALU.subtract)
            kb.act(pLp, dcl, AF.Exp, scale=-C0)
            kb.act(pL, V(cum3.ap[:, :, 63], cumS.keys), AF.Exp, scale=-C0)
            kb.ts("pool", kk, k0, col(PC_KK, 64))
            kb.tt("pool", kk2, kk, kk, ALU.mult)
            kb.mm(sblk0, ones64, kk2)
            kb.ts("dve", rn, sblk0, 1e-24, None, ALU.max)
            kb.act(rn, rn, AF.Sqrt)
            kb.recip(rn, rn)
            kb.tt("pool", kkn, kk, rn, ALU.mult)
            kb.ts("dve", t1, icl, col(PC_KA, 64), omka, ALU.mult, ALU.add)
            kb.tt("pool", kmod, k0, t1, ALU.mult)
            kb.tt("pool", bvec, kkn, icl, ALU.mult)
            kb.stt(at_, kkn, -1.0, pprev, ALU.mult, ALU.mult)
            kb.tt("pool", bt_, bvec, pinv, ALU.mult)
            kb.tt("dve", kt_, kmod, pinv, ALU.mult)
            kb.tt("pool", rt_, r_, p_, ALU.mult)
            kb.tt("dve", Bp, bvec, pLp, ALU.mult)
            kb.tt("pool", Kp, kmod, pLp, ALU.mult)
            if layer == 1:
                for i in range(4):
                    kb.tt("pool", tmp128, raw[f"va{i}"][:, 0:TB], raw[f"va{i}"][:, 1:1 + TB], ALU.subtract)
                    kb.stt(vall[:, i, :], tmp128, pc[:, PC_MUV + i:PC_MUV + i + 1], raw[f"va{i}"][:, 1:1 + TB],
                           ALU.mult, ALU.add)
                for i in range(4):
                    kb.mm(sblk1[0:32, :], pm[:, PM_V1 + 32 * i:PM_V1 + 32 * (i + 1)], vall[:, i, :],
                          start=(i == 0), stop=(i == 3))
                kb.cp("act", vv1, sblk1[0:32, :])
                kb.mm(sblk1, pm[0:32, PM_V2:PM_V2 + 64], vv1)
                kb.act(vg, sblk1, AF.Sigmoid, bias=col(PC_V0, 64))
                kb.dma("sp", vfb, vf_d[:, t0:t0 + TB], "vf")
                kb.tt("pool", dv, vfb, v_, ALU.subtract)
                kb.tt("pool", dv, dv, vg, ALU.mult)
                kb.tt("pool", v_, v_, dv, ALU.add)
            else:
                kb.dma("sp", vo_d[:, t0:t0 + TB], v_, "vo")
            kb.act(sxg, xg, AF.Sigmoid)
            kb.mm(sblk0, pm[:, PM_G2:PM_G2 + 64], sxg)
            kb.cp("act", gate, sblk0)
            kb.stt(rk, r_, col(PC_RK, 64), kmod, ALU.mult, ALU.mult)
            kb.mm(sblk1, ones64, rk)
            kb.tt("dve", bonus, sblk1, v_, ALU.mult)

            for c in range(NCH if stage >= 2 else 0):
                cs = slice(c * 64, (c + 1) * 64)
                P5 = qa
                kb.mm(qa[:, 0:64], bt_[:, cs], at_[:, cs])
                kb.mm(qa[:, 64:128], at_[:, cs], bt_[:, cs])
                kb.mm(qa[:, 128:192], kt_[:, cs], at_[:, cs])
                kb.mm(qa[:, 192:256], bt_[:, cs], rt_[:, cs])
                kb.mm(qa[:, 256:320], kt_[:, cs], rt_[:, cs])
                kb.tt("dve", A5, P5, m5, ALU.mult)
                if CUT < 1:
                    continue
                Tj, Pj = A5[:, 0:64], A5[:, 64:128]
                kb.tt("pool", XT[0], Tj, id64, ALU.add)
                xcur = 0
                for j in range(PJ):
                    kb.mm(sa[:, 0:64], Pj, Tj)
                    kb.mm(sa[:, 64:128], Tj, Pj)
                    evac(PT[j], sa)
                    Tj, Pj = PT[j][:, 0:64], PT[j][:, 64:128]
                    if not PX:
                        continue
                    kb.mm(sbx, Pj, XT[xcur])
                    kb.tt("dve", XT[1 - xcur], XT[xcur], sbx, ALU.add)
                    xcur = 1 - xcur
                XTf = XT[xcur]
                if CUT < 2:
                    continue
                kb.tr(sd[:, 0:64], v_[:, cs], id64)
                kb.tr(sd[:, 64:128], Bp[:, cs], id64)
                kb.tr(sd[:, 128:192], Kp[:, cs], id64)
                evac(tok3, sd)
                Vt, Bt, Kt = tok3[:, 0:64], tok3[:, 64:128], tok3[:, 128:192]
                if CUT < 3:
                    continue
                kb.mm(qb, at_[:, cs], Hst, start=True, stop=False)
                kb.mm(qb, A5[:, 128:192], Vt, start=False, stop=True)
                evac(M0, qb)
                kb.mm(qc, XTf, M0)
                evac(U_, qc)
                kb.mm(qd, rt_[:, cs], Hst, start=True, stop=False)
                kb.mm(qd, A5[:, 192:256], U_, start=False, stop=False)
                kb.mm(qd, A5[:, 256:320], Vt, start=False, stop=True)
                kb.mm(sc_, Bt, U_, start=True, stop=False)
                kb.mm(sc_, Kt, Vt, start=False, stop=True)
                kb.stt(Hst, Hst, pL[:, c:c + 1], sc_, ALU.mult, ALU.add)
                if CUT < 4:
                    continue
                kb.act(Ysb, qd, AF.Identity, accum=st[:, 0:1])
                kb.act(ysq, Ysb, AF.Square, accum=st[:, 1:2])
                kb.ts("dve", st[:, 2:3], st[:, 0:1], 1.0 / 64.0)
                kb.tt("dve", st[:, 3:4], st[:, 2:3], st[:, 2:3], ALU.mult)
                kb.stt(st[:, 4:5], st[:, 1:2], 1.0 / 64.0, st[:, 3:4], ALU.mult, ALU.subtract)
                kb.act(st[:, 5:6], st[:, 4:5], AF.Sqrt, bias=64e-5)
                kb.recip(st[:, 5:6], st[:, 5:6])
                kb.ts("dve", yn, Ysb, st[:, 2:3], st[:, 5:6], ALU.subtract, ALU.mult)
                kb.tr(se, yn, id64)
                evac(ynT[:, cs], se)
            kb.ts("dve", yf, ynT, col(PC_LNW, 64), col(PC_LNB, 64), ALU.mult, ALU.add)
            kb.tt("pool", yf, yf, bonus, ALU.add)
            kb.tt("pool", yf, yf, gate, ALU.mult)
            kb.dma("sp", ya_d[:, t0:t0 + TB], yf, "ya")

            if stage < 3:
                continue
            for (acc, rw, cw, cb, outq) in ((mq_acc, raw["mq"], PC_CQ, PC_CBQ, mq), (mk_acc, raw["mk"], PC_CK, PC_CBK, mk)):
                kb.ts("dve", acc, rw[:, 0:TB], col(cw), col(cb), ALU.mult, ALU.add)
                for j in range(1, 4):
                    kb.stt(acc, rw[:, j:j + TB], col(cw + j), acc, ALU.mult, ALU.add)
                kb.act(outq, acc, AF.Silu)
            for c in range(NCM):
                cs = slice(c * 128, (c + 1) * 128)
                gi = b * NCM + c
                kb.tr(mG[:, 0:2], raw["mif"][:, cs], pm[0:2, PM_ID:PM_ID + 2])
                kb.cp("dve", gif, mG[:, 0:2])
                kb.act(mg[:, 0:1], gif[:, 0:1], AF.Tanh, bias=ib15, scale=1.0 / 15.0)
                kb.act(mg[:, 1:2], gif[:, 1:2], AF.Tanh, bias=fb15, scale=1.0 / 15.0)
                kb.act(mg[:, 2:3], mg[:, 1:2], AF.Exp, scale=-15.0)
                kb.act(mg[:, 3:4], mg[:, 2:3], AF.Ln, bias=1.0)
                kb.mm(mG[:, 2:3], triM, mg[:, 3:4])
                kb.mm(mG[:, 3:4], ones, mg[:, 3:4])
                kb.cp("dve", mg[:, 10:12], mG[:, 2:4])
                kb.act(mg[:, 4:5], mg[:, 10:11], AF.Exp, scale=-1.0)
                kb.stt(mg[:, 5:6], mg[:, 0:1], 15.0, mg[:, 10:11], ALU.mult, ALU.add)
                kb.act(mg[:, 6:7], mg[:, 5:6], AF.Exp, bias=math.log(128.0 ** -0.5))
                kb.act(mg[:, 7:8], mg[:, 11:12], AF.Exp, scale=-1.0)
                kb.mm(mA[:, 0:128], mk[:, cs], mq[:, cs])
                kb.stt(S0T, mA[:, 0:128], mg[:, 6:7], maskM, ALU.mult, ALU.mult)
                kb.tr(mB, raw["mv"][:, cs], ident)
                kb.cp("act", Vp[:, 0:128], mB)
                kb.tr(mC, mk[:, cs], ident)
                kb.ts("dve", ktg, mC, mg[:, 6:7])
                kb.mm(mD, S0T, Vp[:, 0:129], start=True, stop=False)
                kb.mm(mD, mq[:, cs], Cst[:, 0:129], start=False, stop=True)
                kb.ts("dve", numE[:, 0:129], mD, mg[:, 4:5])
                kb.act(mg[:, 8:9], numE[:, 128:129], AF.Abs)
                kb.ts("dve", mg[:, 8:9], mg[:, 8:9], 1.0, None, ALU.max)
                kb.recip(mg[:, 9:10], mg[:, 8:9])
                kb.ts("dve", mh, numE[:, 0:128], mg[:, 9:10])
                kb.act(mjunk, mh, AF.Square, accum=ssb_all[:, gi:gi + 1])
                kb.tr(mE, mh, ident)
                evac(obuf[:, cs], mE)
                kb.mm(mA, ktg, Vp[:, 0:129])
                kb.ts("dve", Cst[:, 0:129], Cst[:, 0:129], mg[:, 7:8])
                kb.stt(Cst[:, 0:129], mA, mg[:, 7:8], Cst[:, 0:129], ALU.mult, ALU.add)
            kb.dma("sp", ob_d[:, t0:t0 + TB], obuf, "ob")

            if stage < 4:
                continue
            kb.act(hsf, raw["hf"], AF.Sigmoid)
            kb.ts("dve", hf_, hsf, oml, lb, ALU.mult, ALU.add)
            kb.act(hlogf, hf_, AF.Ln)
            kb.act(hkraw, raw["hf"], AF.Sigmoid, scale=-1.0)
            kb.scan(hcum, rstH, hlogf, 0.0, ALU.mult, ALU.add)
            kb.act(hp, hcum, AF.Exp)
            kb.act(hpinv, hcum, AF.Exp, scale=-1.0)
            hc3 = V(hcum.ap.rearrange("p (c l) -> p c l", l=LH), hcum.keys)
            hd3 = V(hdcl.ap.rearrange("p (c l) -> p c l", l=LH), hdcl.keys)
            hlast = V(hc3.ap[:, :, LH - 1:LH].to_broadcast([128, NCHH, LH]), hcum.keys)
            kb.tt("dve", hd3, hlast, hc3, ALU.subtract)
            kb.act(hpLp, hdcl, AF.Exp)
            kb.act(hpL, V(hc3.ap[:, :, LH - 1], hcum.keys), AF.Exp)
            kb.act(hqs, raw["hq"], AF.Silu)
            kb.tt("pool", hqt, hqs, hp, ALU.mult)
            kb.stt(hkt, hkraw, oml, hpinv, ALU.mult, ALU.mult)
            kb.stt(hkp, hkraw, oml, hpLp, ALU.mult, ALU.mult)
            for c in range(NCHH):
                cs = slice(c * LH, (c + 1) * LH)
                gi = b * NCHH + c
                kb.mm(hA, hkt[:, cs], hqt[:, cs])
                kb.tt("dve", attT, hA, maskH, ALU.mult)
                kb.tr(hT2[:, 0:64], raw["hi"][:, cs], id64)
                kb.tr(hT2[:, 64:192], hkp[:, cs], ident)
                evac(tok2, hT2)
                kb.mm(ho, attT, tok2[:, 0:64], start=True, stop=False)
                kb.mm(ho, hqt[:, cs], Sst, start=False, stop=True)
                kb.mm(hS, tok2[:, 64:192], tok2[:, 0:64])
                kb.stt(Sst, Sst, hpL[:, c:c + 1], hS, ALU.mult, ALU.add)
                kb.cp("act", osb, ho)
                kb.act(hjunk, osb, AF.Square, accum=ssc_all[:, gi:gi + 1])
                kb.tr(hO, osb, idLH)
                evac(ocbuf[:, cs], hO)
            kb.dma("sp", oc_d[:, t0:t0 + TB], ocbuf, "oc")

            for n, h in HAL.items():
                if n in raw:
                    kb.cp("pool", raw[n][:, 0:h], raw[n][:, TB:TB + h])

        kb.dma("sp", ssb_d, ssb_all, "ssb")
        kb.dma("sp", ssc_d, ssc_all, "ssc")
        outs = [ya_d, ob_d, oc_d, ssb_d, ssc_d] + ([vo_d] if layer == 0 else [])
        kb.finish(outs)
    return nc


def p1_host_inputs(layer, inp, mod, core, T):
    l = layer
    hd = core
    ph, hf = core // 2, core % 2
    w_in = inp["w_in"][l]
    RW = 512
    o_r, o_k, o_v, o_xw, o_xa, o_xg = 0, 512, 1024, 1536, 1600, 1664
    o_rest = 1792
    o_mq, o_mk, o_mv, o_mo, o_mi, o_mf = (o_rest, o_rest + 512, o_rest + 1024, o_rest + 2048, o_rest + 3072, o_rest + 3076)
    o_hq = o_rest + 3080
    o_hf, o_hi, o_hg = o_hq + 512, o_hq + 1024, o_hq + 1536
    h64 = slice(hd * 64, hd * 64 + 64)

    def rng(o, n):
        return list(range(o, o + n))

    idx = (rng(o_r + hd * 64, 64) + rng(o_k + hd * 64, 64) + rng(o_v + hd * 64, 64) + rng(o_xw, 64) + rng(o_xa, 64)
           + rng(o_xg, 128)
           + rng(o_mq + ph * 128, 128) + rng(o_mk + ph * 128, 128) + rng(o_mv + ph * 256 + hf * 128, 128)
           + [o_mi + ph, o_mf + ph]
           + rng(o_hq + ph * 128, 128) + rng(o_hf + ph * 128, 128) + rng(o_hi + ph * 128 + hf * 64, 64))
    if l == 1:
        idx += rng(o_v, 512)
    w1 = np.ascontiguousarray(w_in[:, idx])
    pc = np.zeros((128, PC_N), np.float32)
    pc[:, PC_G:PC_G + 16] = inp["norm_mix"][l].reshape(16, 128).T
    sh_m, sc_m = mod[l][0:D], mod[l][D:2 * D]
    pc[:, PC_SC:PC_SC + 16] = sc_m.reshape(16, 128).T
    pc[:, PC_SH:PC_SH + 16] = sh_m.reshape(16, 128).T
    mu = inp["rw_mu"][l]
    pc[0:64, PC_MU + 0] = mu[o_r + hd * 64:o_r + hd * 64 + 64]
    pc[0:64, PC_MU + 1] = mu[o_k + hd * 64:o_k + hd * 64 + 64]
    pc[0:64, PC_MU + 2] = mu[o_v + hd * 64:o_v + hd * 64 + 64]
    pc[0:64, PC_MU + 3] = mu[o_xw:o_xw + 64]
    pc[0:64, PC_MU + 4] = mu[o_xa:o_xa + 64]
    pc[0:128, PC_MU + 5] = mu[o_xg:o_xg + 128]
    pc[0:64, PC_W0] = inp["rw_w0"][l][h64]
    pc[0:64, PC_A0] = inp["rw_a0"][l][h64]
    pc[0:64, PC_KK] = inp["rw_kk"][l][h64]
    pc[0:64, PC_KA] = inp["rw_ka"][l][h64]
    pc[0:64, PC_RK] = inp["rw_rk"][l][h64]
    pc[0:64, PC_LNW] = inp["rw_lnw"][l][h64]
    pc[0:64, PC_LNB] = inp["rw_lnb"][l][h64]
    if l == 1:
        pc[0:64, PC_V0] = inp["rw_v0"][0][h64]
        pc[:, PC_MUV:PC_MUV + 4] = mu[o_v:o_v + 512].reshape(4, 128).T
    cw = inp["ml_conv_w"][l]
    cb = inp["ml_conv_b"][l]
    for j in range(4):
        pc[:, PC_CQ + j] = cw[j, ph * 128:(ph + 1) * 128]
        pc[:, PC_CK + j] = cw[j, 512 + ph * 128:512 + (ph + 1) * 128]
    pc[:, PC_CBQ] = cb[ph * 128:(ph + 1) * 128]
    pc[:, PC_CBK] = cb[512 + ph * 128:512 + (ph + 1) * 128]
    pc[:, PC_IB] = inp["ml_ib"][l][ph]
    pc[:, PC_FB] = inp["ml_fb"][l][ph]
    pc[:, PC_LB0] = inp["hg_lb"][0][ph * 128:(ph + 1) * 128]
    pc[:, PC_LB1] = inp["hg_lb"][1][ph * 128:(ph + 1) * 128]
    pm = np.zeros((128, PM_N), np.float32)
    pm[0:64, PM_W2:PM_W2 + 64] = inp["rw_w2"][l][:, h64]
    pm[0:64, PM_A2:PM_A2 + 64] = inp["rw_a2"][l][:, h64]
    pm[0:128, PM_G2:PM_G2 + 64] = inp["rw_g2"][l][:, h64]
    if l == 1:
        pm[0:32, PM_V2:PM_V2 + 64] = inp["rw_v2"][0][:, h64]
        pm[:, PM_V1:PM_V1 + 128] = inp["rw_v1"][0].reshape(4, 128, 32).transpose(1, 0, 2).reshape(128, 128)
    pm[:, PM_ID:PM_ID + 128] = np.eye(128, dtype=np.float32)
    pm[:, PM_ONES:PM_ONES + 128] = 1.0
    s_ = np.arange(64)[:, None]
    t_ = np.arange(64)[None, :]
    mu_s = (s_ < t_).astype(np.float32)
    ml_s = (s_ > t_).astype(np.float32)
    mu_i = (s_ <= t_).astype(np.float32)
    pm[0:64, PM_M5:PM_M5 + 320] = np.concatenate([mu_s, ml_s, mu_s, mu_i, mu_i], axis=1)
    s2 = np.arange(128)[:, None]
    t2 = np.arange(128)[None, :]
    pm[:, PM_MM:PM_MM + 128] = (s2 <= t2).astype(np.float32)
    pm[:, PM_TRI:PM_TRI + 128] = (s2 <= t2).astype(np.float32)
    pm[0:LH, PM_MH:PM_MH + LH] = mu_i[0:LH, 0:LH]
    rsth = np.ones((128, TB), np.float32)
    rsth[:, ::LH] = 0.0
    pm[:, PM_RSTH:PM_RSTH + TB] = rsth
    rst = np.ones((128, TB), np.float32)
    rst[:, ::64] = 0.0
    pm[:, PM_RST:PM_RST + TB] = rst
    return {"w1": w1, "pc": pc, "pm": pm}


def build_p0():
    nc = bass.Bass("TRN2", target_bir_lowering=False)
    NCT = 24
    c_d = dram_in(nc, "c", [128, 16])
    w_d = dram_in(nc, "w", [D, NCT * 128])
    b_d = dram_in(nc, "b", [128, NCT])
    o_d = dram_out(nc, "mod", [128, NCT])
    with ExitStack() as es:
        kb = KB(nc, es)
        cc = kb.sb("cc", [128, 16])
        bb = kb.sb("bb", [128, NCT])
        oo = kb.sb("oo", [128, NCT])
        wb = [kb.sb(f"w{i}", [128, 16, 512]) for i in range(2)]
        pp = kb.ps("pp", [128, 512])
        kb.dma("sp", cc, c_d, "c")
        kb.dma("sp", bb, b_d, "b")
        kb.act(cc, cc, AF.Silu)
        wv = V(w_d.ap.rearrange("(kc p) n -> p kc n", p=128), w_d.keys)
        for pc_ in range(NCT // 4):
            W = wb[pc_ % 2]
            kb.dma("sp", W, wv[:, :, pc_ * 512:(pc_ + 1) * 512], f"w{pc_ % 2}")
            for j in range(4):
                ct = pc_ * 4 + j
                for kc in range(16):
                    kb.mm(pp[:, ct:ct + 1], W[:, kc, j * 128:(j + 1) * 128], cc[:, kc:kc + 1],
                          start=(kc == 0), stop=(kc == 15))
        kb.tt("dve", oo, pp[:, 0:NCT], bb, ALU.add)
        kb.dma("sp", o_d, oo, "o")
        kb.finish([o_d])
    return nc


NT2 = 2048
SPA = 512
SPB = 1024
P2C_N = 128
(P2_GM, P2_SCM, P2_SHM, P2_GF, P2_SCF, P2_SHF, P2_MLN, P2_HGN) = (0, 16, 32, 48, 64, 80, 96, 104)
NWG = 7680


def build_p2(final, NT=NT2, nexp=32, a_only=False):
    nc = bass.Bass("TRN2", target_bir_lowering=False)
    x_d = dram_in(nc, "x", [NT, D])
    ya_d = dram_in(nc, "yaT", [512, NT])
    ob_d = dram_in(nc, "obT", [1024, NT])
    oc_d = dram_in(nc, "ocT", [512, NT])
    ssb_d = dram_in(nc, "ssb", [8, NT])
    ssc_d = dram_in(nc, "ssc", [8, NT])
    sel_d = dram_in(nc, "sel", [8, 4 * 128])
    wg_d = dram_in(nc, "wg", [D, NWG])
    pabc_d = dram_in(nc, "pabc", [D, D])
    wo_d = dram_in(nc, "wout", [D, D])
    wr_d = dram_in(nc, "wr", [D, 36])
    rb_d = dram_in(nc, "rb", [128, 36])
    nh = nexp // 2
    eg_ds = [dram_in(nc, f"eg{i}", [nh, D, 1024]) for i in range(2)]
    eu_ds = [dram_in(nc, f"eu{i}", [nh, D, 1024]) for i in range(2)]
    ed_ds = [dram_in(nc, f"ed{i}", [nh, 1024, D]) for i in range(2)]
    pc_d = dram_in(nc, "pc2", [128, P2C_N])
    rowb_d = dram_in(nc, "rowb", [128, 3 * D])
    id_d = dram_in(nc, "ident", [128, 128])
    if a_only:
        xm_d = dram_out(nc, "xmid", [NT, D])
    else:
        xm_d = V(nc.dram_tensor("xmid", [NT, D], F32, kind="Internal").ap(), ("dram_xmid",))
    out_d = dram_out(nc, "xout", [NT, D])

    with ExitStack() as es:
        kb = KB(nc, es)
        sb, ps = kb.sb, kb.ps
        pc = sb("pc2", [128, P2C_N])
        rowb = sb("rowb", [128, D])
        ident = sb("ident", [128, 128])
        sel = sb("sel", [8, 512])
        kb.dma("sp", pc, pc_d, "pc")
        kb.dma("sp", rowb, rowb_d[:, D:2 * D], "rowb")
        kb.dma("sp", ident, id_d, "id")
        kb.dma("sp", sel, sel_d, "sel")
        ones = sb("ones", [128, 128])
        kb.memset("dve", ones, 1.0)
        Am = sb("Am", [128, 16])
        kb.stt(Am, pc[:, P2_SCM:P2_SCM + 16], 1.0, pc[:, P2_GM:P2_GM + 16], ALU.add, ALU.mult)
        Bm = pc[:, P2_SHM:P2_SHM + 16]
        Af = sb("Af", [128, 16])
        kb.stt(Af, pc[:, P2_SCF:P2_SCF + 16], 1.0, pc[:, P2_GF:P2_GF + 16], ALU.add, ALU.mult)
        Bf = pc[:, P2_SHF:P2_SHF + 16]
        gtf_b = rowb[:, 0:D]

        banks = [ps(f"bk{i}", [128, 512]) for i in range(8)]
        bk = lambda i: banks[i]

        RING = 4
        ring = [sb(f"ring{i}", [128, 4096], BF16) for i in range(RING)]
        ring_i = [0]

        def wload(src_view, shape3):
            r = ring[ring_i[0] % RING]
            nm = f"ring{ring_i[0] % RING}"
            ring_i[0] += 1
            a, b_ = shape3
            dst = V(r.ap[:, 0:a * b_].rearrange("p (a b) -> p a b", b=b_), r.keys)
            kb.dma("pool", dst, src_view, nm)
            return dst

        class Stream:
            def __init__(self, look):
                self.items = []
                self.bufs = {}
                self.next = 0
                self.look = look

            def add(self, fn):
                self.items.append(fn)
                return len(self.items) - 1

            def get(self, i):
                while self.next < len(self.items) and self.next <= i + self.look:
                    self.bufs[self.next] = self.items[self.next]()
                    self.next += 1
                return self.bufs.pop(i)

        wgv = V(wg_d.ap.rearrange("(kc p) n -> p kc n", p=128), wg_d.keys)
        pav = V(pabc_d.ap.rearrange("(kc p) n -> p kc n", p=128), pabc_d.keys)
        wov = V(wo_d.ap.rearrange("(kc p) n -> p kc n", p=128), wo_d.keys)

        es_main = kb.es
        es_a = ExitStack()
        kb.es = es_a
        xs = sb("xs", [128, SPA // 128, D])
        gtm_b = sb("gtm_b", [128, D])
        kb.dma("sp", gtm_b, rowb_d[:, 0:D], "gtm")
        xn = [sb(f"xn{i}", [128, D]) for i in range(2)]
        junk = sb("junk", [128, D], BF16)
        ss = sb("ss", [128, 8])
        rstd = sb("rstd", [128, 8])
        hT = sb("hT", [128, 16, SPA], BF16)
        yT = sb("yT", [128, 16, SPA], BF16)
        zT = sb("zT", [128, 16, SPA], BF16)
        ssrow = sb("ssrow", [8, 2, SPA])
        rsb = sb("rsb", [128, SPA])
        otmp = sb("otmp", [128, SPA])
        sgt = sb("sgt", [128, SPA])
        zacc = sb("zacc", [128, SPA])
        ztmp = sb("ztmp", [128, SPA])
        pbuf = [sb(f"pbuf{i}", [128, 16, 128], BF16) for i in range(2)]

        def norm_T(X, ntile, Acol, Bcol, Hout, wtok, b0, rt32=None):
            for tt in range(ntile):
                kb.act(junk, X[:, tt, :], AF.Square, accum=ss[:, tt:tt + 1])
                kb.act(rstd[:, tt:tt + 1], ss[:, tt:tt + 1], AF.Sqrt, bias=EPS, scale=1.0 / D)
                kb.recip(rstd[:, tt:tt + 1], rstd[:, tt:tt + 1])
                xx = xn[tt % 2]
                kb.ts("dve", xx, X[:, tt, :], rstd[:, tt:tt + 1])
                for k4 in range(4):
                    pb = bk(b0 + k4 % 2).k(f"bk{b0 + k4 % 2}")
                    for j in range(4):
                        kc = k4 * 4 + j
                        kb.tr(pb[:, j * 128:(j + 1) * 128], xx[:, kc * 128:(kc + 1) * 128], ident)
                    src32 = None
                    if rt32 is not None:
                        src32 = rt32(tt, k4, pb, 0)
                    for j in range(4):
                        kc = k4 * 4 + j
                        src = pb[:, j * 128:(j + 1) * 128] if src32 is None else src32[:, kc, :]
                        kb.act(Hout[:, kc, tt * 128:(tt + 1) * 128], src, AF.Identity,
                               bias=Bcol[:, kc:kc + 1], scale=Acol[:, kc:kc + 1])
                    if rt32 is not None:
                        rt32(tt, k4, pb, 1)

        for sp_ in range(NT // SPA):
            t0 = sp_ * SPA
            for tt in range(SPA // 128):
                kb.dma("sp", xs[:, tt, :], x_d[t0 + tt * 128:t0 + (tt + 1) * 128, :], f"xa{tt}")
            norm_T(xs, SPA // 128, Am, Bm, hT, SPA, 0)
            kb.dma("sp", ssrow[:, 0, :], ssb_d[:, t0:t0 + SPA], "ssr0")
            kb.dma("sp", ssrow[:, 1, :], ssc_d[:, t0:t0 + SPA], "ssr1")
            for i in range(4):
                kb.dma("pool", yT[:, i, :], ya_d[i * 128:(i + 1) * 128, t0:t0 + SPA], "yTa")
            st = Stream(2)
            for i in range(12):
                c0 = 6144 + i * 128
                st.add(lambda c0=c0: wload(wgv[:, :, c0:c0 + 128], (16, 128)))
            for i in range(12):
                isb = i < 8
                hd = (i // 2) if isb else (i - 8)
                if (isb and i % 2 == 0) or (not isb):
                    pr = bk(2).k("bk2")
                    kb.mm(pr[:, 0:SPA], sel[:, hd * 128:(hd + 1) * 128], ssrow[:, 0 if isb else 1, :])
                    kb.act(rsb, pr[:, 0:SPA], AF.Sqrt, bias=EPS, scale=(1.0 / 256.0 if isb else 1.0 / 128.0))
                    kb.recip(rsb, rsb)
                src = ob_d[i * 128:(i + 1) * 128, t0:t0 + SPA] if isb else oc_d[(i - 8) * 128:(i - 7) * 128, t0:t0 + SPA]
                kb.dma("sp", otmp, src, "otmp")
                W = st.get(i)
                pg = bk(3).k("bk3")
                for kc in range(16):
                    kb.mm(pg[:, 0:SPA], W[:, kc, :], hT[:, kc, :], start=(kc == 0), stop=(kc == 15))
                kb.act(sgt, pg[:, 0:SPA], AF.Sigmoid if isb else AF.Silu)
                kb.tt("dve", otmp, otmp, rsb, ALU.mult)
                ncol = (P2_MLN + i) if isb else (P2_HGN + i - 8)
                kb.stt(yT[:, 4 + i, :], otmp, pc[:, ncol:ncol + 1], sgt, ALU.mult, ALU.mult)
            st = Stream(3)
            for dt in range(16):
                for br in range(3):
                    c0 = br * 2048 + dt * 128
                    st.add(lambda c0=c0: wload(wgv[:, :, c0:c0 + 128], (16, 128)))
            CH = ((0, 4), (4, 12), (12, 16))
            kb.dma("pool", pbuf[0], pav[:, :, 0:128], "pbuf0")
            for dt in range(16):
                if dt + 1 < 16:
                    kb.dma("pool", pbuf[(dt + 1) % 2], pav[:, :, (dt + 1) * 128:(dt + 2) * 128], f"pbuf{(dt + 1) % 2}")
                Wp = pbuf[dt % 2]
                Wg3 = [None, None, None]
                for br in range(3):
                    Wg3[br] = st.get(dt * 3 + br)
                    pg = bk(4 + br % 2).k(f"bk{4 + br % 2}")
                    for kc in range(16):
                        kb.mm(pg[:, 0:SPA], Wg3[br][:, kc, :], hT[:, kc, :], start=(kc == 0), stop=(kc == 15))
                    kb.act(sgt, pg[:, 0:SPA], AF.Sigmoid)
                    pq = bk(6 + br % 2).k(f"bk{6 + br % 2}")
                    lo, hi = CH[br]
                    for ch in range(lo, hi):
                        kb.mm(pq[:, 0:SPA], Wp[:, ch, :], yT[:, ch, :], start=(ch == lo), stop=(ch == hi - 1))
                    if br == 0:
                        kb.tt("dve", zacc, sgt, pq[:, 0:SPA], ALU.mult)
                    elif br == 1:
                        kb.tt("dve", ztmp, sgt, pq[:, 0:SPA], ALU.mult)
                        kb.tt("dve", zacc, zacc, ztmp, ALU.add)
                    else:
                        kb.tt("dve", ztmp, sgt, pq[:, 0:SPA], ALU.mult)
                        kb.tt("dve", zT[:, dt, :], zacc, ztmp, ALU.add)
            st = Stream(2)
            for p8 in range(8):
                st.add(lambda p8=p8: wload(wov[:, :, p8 * 256:(p8 + 1) * 256], (16, 256)))
            for p8 in range(8):
                W = st.get(p8)
                cs = slice(p8 * 256, (p8 + 1) * 256)
                for tt in range(SPA // 128):
                    po = bk(tt % 2).k(f"bk{tt % 2}")
                    for kc in range(16):
                        kb.mm(po[:, 0:256], zT[:, kc, tt * 128:(tt + 1) * 128], W[:, kc, :], start=(kc == 0), stop=(kc == 15))
                    kb.tt("dve", ztmp[:, 0:256], po[:, 0:256], gtm_b[:, cs], ALU.mult)
                    kb.tt("dve", xs[:, tt, cs], xs[:, tt, cs], ztmp[:, 0:256], ALU.add)
            for tt in range(SPA // 128):
                kb.dma("sp", xm_d[t0 + tt * 128:t0 + (tt + 1) * 128, :], xs[:, tt, :], f"xm{tt}")

        if a_only:
            kb.finish([xm_d])
            kb.barrier()
            es_a.close()
            return nc
        kb.barrier()
        es_a.close()
        kb.es = es_main
        actT_raw = sb("actT", [128, 8 * SPB], BF16)
        actT = V(actT_raw.ap.rearrange("p (a b) -> p a b", b=SPB), actT_raw.keys)
        xn = [V(actT_raw.ap[:, 0:2 * D].bitcast(F32), actT_raw.keys)] * 2
        junk = actT_raw[:, 2 * D:3 * D]
        ss = sb("ssb_", [128, 8])
        rstd = sb("rstdb", [128, 8])
        xacc = sb("xacc", [128, SPB // 128, D])
        hT2 = sb("hT2", [128, 16, SPB], BF16)
        xT32 = sb("xT32", [128, 16, 128])
        wr = sb("wr", [128, 16, 36])
        wr2 = wr
        rbrow = sb("rbrow", [128, 36])
        brow = sb("brow", [128, 36])
        lg = sb("lg", [128, 36])
        rt = sb("rt", [128, 48])
        em = sb("em", [128, 32])
        em2 = sb("em2", [128, 32])
        oh1 = sb("oh1", [128, 32])
        oh2 = sb("oh2", [128, 32])
        coef = sb("coef", [128, SPB // 128, 32])
        gsil = sb("gsil", [128, 512])
        dtmp = sb("dtmp", [128, 512])
        if P2CUT < 0:
            kb.memset("dve", xacc[:, 0, :], 1.0)
            kb.dma("sp", out_d[0:128, :], xacc[:, 0, :])
            kb.finish([out_d])
            return nc
        kb.dma("sp", wr, V(wr_d.ap.rearrange("(kc p) n -> p kc n", p=128), wr_d.keys), "wr")
        kb.dma("sp", rbrow, rb_d, "rb")
        pbr = bk(7).k("bk7")
        kb.cp("dve", xT32, V(Bf.ap.unsqueeze(2).to_broadcast([128, 16, 128]), Bf.keys))
        for kc in range(16):
            kb.mm(pbr[:, 0:36], xT32[:, kc, :], wr[:, kc, :], start=(kc == 0), stop=(kc == 15))
        kb.tt("dve", brow, pbr[:, 0:36], rbrow, ALU.add)
        kb.tt("dve", wr2, wr, V(Af.ap.unsqueeze(2).to_broadcast([128, 16, 36]), Af.keys), ALU.mult)
        egvs = [V(t.ap.rearrange("e (kc p) n -> p e kc n", p=128), t.keys) for t in eg_ds]
        euvs = [V(t.ap.rearrange("e (kc p) n -> p e kc n", p=128), t.keys) for t in eu_ds]
        edvs = [V(t.ap.rearrange("e (kc p) n -> p e kc n", p=128), t.keys) for t in ed_ds]

        if P2CUT == 0:
            kb.memset("dve", xacc[:, 0, :], 1.0)
            kb.cp("dve", xacc[:, 0, 0:36], brow)
            kb.dma("sp", out_d[0:128, :], xacc[:, 0, :])
            kb.finish([out_d])
            return nc
        for pb_ in range(NT // SPB):
            t0 = pb_ * SPB
            NTT = SPB // 128
            kb.wait_all("sp", [xm_d])
            for tt in range(NTT):
                kb.dma("sp", xacc[:, tt, :], xm_d[t0 + tt * 128:t0 + (tt + 1) * 128, :], f"xb{tt}")

            def rt32(tt, k4, pbank, phase):
                if phase == 0:
                    kb.cp("dve", V(xT32.ap[:, k4 * 4:(k4 + 1) * 4, :].rearrange("p a b -> p (a b)"), xT32.keys), pbank[:, 0:512])
                    return xT32
                if k4 == 3:
                    pl = bk(6).k("bk6")
                    for kc in range(16):
                        kb.mm(pl[:, 0:36], xT32[:, kc, :], wr2[:, kc, :], start=(kc == 0), stop=(kc == 15))
                    if P2CUT != 25:
                        route(tt, pl)
                    else:
                        kb.cp("dve", lg, pl[:, 0:36])

            def route(tt, pl):
                kb.tt("dve", lg, pl[:, 0:36], brow, ALU.add)
                r_ = lambda j: rt[:, j:j + 1]
                kb.op("dve", lambda: nc.vector.tensor_reduce(out=r_(0).ap, in_=lg[:, 0:4].ap, axis=AX.X, op=ALU.max),
                      [rt], [lg])
                kb.ts("dve", r_(1), r_(0), -1.0)
                kb.act(rt[:, 8:12], lg[:, 0:4], AF.Exp, bias=r_(1), accum=r_(2))
                kb.recip(r_(3), r_(2))
                kb.ts("dve", rt[:, 12:16], lg[:, 0:4], r_(0), None, ALU.is_equal)
                kb.ts("dve", rt[:, 16:20], rt[:, 12:16], 1e30, -1e30, ALU.mult, ALU.add)
                em3 = V(em.ap.rearrange("p (g e) -> p g e", e=8), em.keys)
                el3 = V(lg.ap[:, 4:36].rearrange("p (g e) -> p g e", e=8), lg.keys)
                pen = V(rt.ap[:, 16:20].unsqueeze(2).to_broadcast([128, 4, 8]), rt.keys)
                kb.tt("dve", em3, el3, pen, ALU.add)
                kb.op("dve", lambda: nc.vector.tensor_reduce(out=r_(4).ap, in_=em.ap, axis=AX.X, op=ALU.max), [rt], [em])
                kb.ts("dve", oh1, em, r_(4), None, ALU.is_equal)
                kb.stt(em2, oh1, -1e30, em, ALU.mult, ALU.add)
                kb.op("dve", lambda: nc.vector.tensor_reduce(out=r_(5).ap, in_=em2.ap, axis=AX.X, op=ALU.max), [rt], [em2])
                kb.ts("dve", oh2, em2, r_(5), None, ALU.is_equal)
                kb.ts("dve", r_(6), r_(4), -1.0)
                kb.act(r_(7), r_(5), AF.Exp, bias=r_(6))
                kb.ts("dve", r_(20), r_(7), 1.0, None, ALU.add)
                kb.recip(r_(21), r_(20))
                kb.tt("dve", r_(22), r_(7), r_(21), ALU.mult)
                kb.tt("dve", r_(23), r_(21), r_(3), ALU.mult)
                kb.tt("dve", r_(24), r_(22), r_(3), ALU.mult)
                kb.ts("dve", coef[:, tt, :], oh1, r_(23))
                kb.stt(coef[:, tt, :], oh2, r_(24), coef[:, tt, :], ALU.mult, ALU.add)

            if P2CUT >= 2:
                norm_T(xacc, NTT, Af, Bf, hT2, SPB, 4, rt32=(rt32 if (P2CUT >= 3 or P2CUT == 25) else None))

            st = Stream(2)
            for e in range(nexp):
                egv, euv, edv, el_ = egvs[e // nh], euvs[e // nh], edvs[e // nh], e % nh
                for q in range(4):
                    st.add(lambda v=egv, e=el_, q=q: wload(v[:, e, :, q * 256:(q + 1) * 256], (16, 256)))
                    st.add(lambda v=euv, e=el_, q=q: wload(v[:, e, :, q * 256:(q + 1) * 256], (16, 256)))
                for q in range(4):
                    st.add(lambda v=edv, e=el_, q=q: wload(v[:, e, :, q * 512:(q + 1) * 512], (8, 512)))
            for e in range(nexp if (P2CUT >= 4 and P2CUT != 25) else 0):
                base = e * 12
                for q in range(4):
                    Wg = st.get(base + 2 * q)
                    Wu = st.get(base + 2 * q + 1)
                    for f2 in range(2):
                        ft = q * 2 + f2
                        for hh in range(SPB // 512):
                            pgt = bk(2 * hh).k(f"bk{2 * hh}")
                            put = bk(2 * hh + 1).k(f"bk{2 * hh + 1}")
                            tsl = slice(hh * 512, (hh + 1) * 512)
                            for kc in range(16):
                                kb.mm(pgt, Wg[:, kc, f2 * 128:(f2 + 1) * 128], hT2[:, kc, tsl], start=(kc == 0), stop=(kc == 15))
                            for kc in range(16):
                                kb.mm(put, Wu[:, kc, f2 * 128:(f2 + 1) * 128], hT2[:, kc, tsl], start=(kc == 0), stop=(kc == 15))
                            kb.act(gsil, pgt, AF.Silu)
                            kb.tt("dve", actT[:, ft, tsl], gsil, put, ALU.mult)
                for q in range(4):
                    Wd = st.get(base + 8 + q)
                    cs = slice(q * 512, (q + 1) * 512)
                    for tt in range(NTT):
                        pd = bk(4 + tt % 2).k(f"bk{4 + tt % 2}")
                        for ft in range(8):
                            kb.mm(pd, actT[:, ft, tt * 128:(tt + 1) * 128], Wd[:, ft, :], start=(ft == 0), stop=(ft == 7))
                        kb.tt("dve", dtmp, pd, gtf_b[:, cs], ALU.mult)
                        kb.stt(xacc[:, tt, cs], dtmp, coef[:, tt, e:e + 1], xacc[:, tt, cs], ALU.mult, ALU.add)
            if final:
                fng_b = V(ring[0].ap.bitcast(F32), ring[0].keys)
                kb.dma("sp", fng_b, rowb_d[:, 2 * D:3 * D], "fng")
            for tt in range(NTT):
                if final:
                    kb.act(junk, xacc[:, tt, :], AF.Square, accum=ss[:, tt:tt + 1])
                    kb.act(rstd[:, tt:tt + 1], ss[:, tt:tt + 1], AF.Sqrt, bias=EPS, scale=1.0 / D)
                    kb.recip(rstd[:, tt:tt + 1], rstd[:, tt:tt + 1])
                    kb.stt(xacc[:, tt, :], xacc[:, tt, :], rstd[:, tt:tt + 1], fng_b, ALU.mult, ALU.mult)
                kb.dma("sp", out_d[t0 + tt * 128:t0 + (tt + 1) * 128, :], xacc[:, tt, :], f"xo{tt}")
        kb.finish([out_d])
    return nc


def p2_host_inputs(layer, inp, mod, final):
    l = layer
    w_in = inp["w_in"][l]
    o_rest = 1792
    o_mo = o_rest + 2048
    o_hg = o_rest + 3080 + 1536
    o_ga = o_rest + 3080 + 2048
    wg = np.ascontiguousarray(np.concatenate([w_in[:, o_ga:o_ga + 6144], w_in[:, o_mo:o_mo + 1024],
                                              w_in[:, o_hg:o_hg + 512]], axis=1))
    pabc = np.ascontiguousarray(np.concatenate([inp["p_a"][l], inp["p_b"][l], inp["p_c"][l]], axis=0))
    m = mod[l]
    sh_m, sc_m, gt_m, sh_f, sc_f, gt_f = [m[i * D:(i + 1) * D] for i in range(6)]
    colz = lambda v: v.reshape(-1, 128).T
    pc = np.zeros((128, P2C_N), np.float32)
    pc[:, P2_GM:P2_GM + 16] = colz(inp["norm_mix"][l])
    pc[:, P2_SCM:P2_SCM + 16] = colz(sc_m)
    pc[:, P2_SHM:P2_SHM + 16] = colz(sh_m)
    pc[:, P2_GF:P2_GF + 16] = colz(inp["norm_ffn"][l])
    pc[:, P2_SCF:P2_SCF + 16] = colz(sc_f)
    pc[:, P2_SHF:P2_SHF + 16] = colz(sh_f)
    pc[:, P2_MLN:P2_MLN + 8] = colz(inp["ml_norm"][l])
    pc[:, P2_HGN:P2_HGN + 4] = colz(inp["hg_norm"][l])
    rowb = np.ascontiguousarray(np.broadcast_to(np.concatenate([gt_m, gt_f, inp["final_norm"]])[None, :], (128, 3 * D)))
    sel = np.zeros((8, 4, 128), np.float32)
    for h in range(4):
        sel[2 * h:2 * h + 2, h, :] = 1.0
    return {
        "wg": wg, "pabc": pabc, "wout": np.ascontiguousarray(inp["w_out"][l]),
        "wr": np.ascontiguousarray(np.concatenate([inp["moe_gw"][l], inp["moe_ew"][l]], axis=1)),
        "rb": np.ascontiguousarray(np.broadcast_to(np.concatenate([inp["moe_gb"][l], inp["moe_eb"][l]])[None, :], (128, 36))),
        "eg0": inp["ex_gate"][l][:16], "eg1": inp["ex_gate"][l][16:],
        "eu0": inp["ex_up"][l][:16], "eu1": inp["ex_up"][l][16:],
        "ed0": inp["ex_down"][l][:16], "ed1": inp["ex_down"][l][16:],
        "pc2": pc, "rowb": rowb.astype(np.float32), "ident": np.eye(128, dtype=np.float32),
        "sel": sel.reshape(8, 512),
    }


_CACHE = {}


def _prog(key, fn):
    if key not in _CACHE:
        _CACHE[key] = fn()
    return _CACHE[key]


def kernel(**inp):
    inp = {k: np.asarray(v) for k, v in inp.items()}
    T = SEQ
    cores = list(range(NCORES))
    c_col = np.ascontiguousarray(inp["c"][0].reshape(16, 128).T)
    adaw = inp["ada_w"]
    adab = inp["ada_b"]
    in_maps = []
    for c in cores:
        l, part = c // 4, c % 4
        cols = slice(part * 3072, (part + 1) * 3072)
        in_maps.append({"c": c_col, "w": np.ascontiguousarray(adaw[l][:, cols]),
                        "b": np.ascontiguousarray(adab[l][cols].reshape(24, 128).T)})
    res = run_bass_kernel_spmd(_prog("p0", build_p0), in_maps, core_ids=cores)
    mod = [np.concatenate([res.results[l * 4 + p]["mod"].T.reshape(-1) for p in range(4)]) for l in range(2)]

    x = np.ascontiguousarray(inp["x"][0])
    vfirst = None
    for l in range(2):
        in_maps = []
        for c in cores:
            m_ = p1_host_inputs(l, inp, mod, c, T)
            m_["x"] = x
            if l == 1:
                m_["vfirst"] = vfirst[c]
            in_maps.append(m_)
        r1 = run_bass_kernel_spmd(_prog(("p1", l), lambda: build_p1(l, T)), in_maps, core_ids=cores).results
        if l == 0:
            vfirst = [np.ascontiguousarray(r1[c]["vT"]) for c in cores]
        yaT = np.concatenate([r1[c]["yaT"] for c in cores], axis=0)
        obT = np.concatenate([r1[c]["obT"] for c in cores], axis=0)
        ocT = np.concatenate([r1[c]["ocT"] for c in cores], axis=0)
        ssb = np.stack([r1[c]["ssb"].T.reshape(-1) for c in cores], axis=0)
        ssc = np.stack([r1[c]["ssc"].T.reshape(-1) for c in cores], axis=0)
        common = p2_host_inputs(l, inp, mod, l == 1)
        in_maps = []
        for c in cores:
            ts_ = slice(c * NT2, (c + 1) * NT2)
            m_ = dict(common)
            m_["x"] = np.ascontiguousarray(x[ts_])
            m_["yaT"] = np.ascontiguousarray(yaT[:, ts_])
            m_["obT"] = np.ascontiguousarray(obT[:, ts_])
            m_["ocT"] = np.ascontiguousarray(ocT[:, ts_])
            m_["ssb"] = np.ascontiguousarray(ssb[:, ts_])
            m_["ssc"] = np.ascontiguousarray(ssc[:, ts_])
            in_maps.append(m_)
        p2 = _prog(("p2", l == 1), lambda: build_p2(l == 1))
        r2 = []
        for g4 in range(2):
            r2 += run_bass_kernel_spmd(p2, in_maps[g4 * 4:(g4 + 1) * 4], core_ids=list(range(4))).results
        x = np.ascontiguousarray(np.concatenate([r2[c]["xout"] for c in cores], axis=0))
    return x[None].astype(np.float32)
```

```python
import math
CUT, PJ, PX = 99, 5, 1
P2CUT = 99
from contextlib import ExitStack
import numpy as np
import concourse.bass as bass
import concourse.mybir as mybir
from concourse.bass_utils import run_bass_kernel_spmd

F32 = mybir.dt.float32
BF16 = mybir.dt.bfloat16
AF = mybir.ActivationFunctionType
ALU = mybir.AluOpType
AX = mybir.AxisListType

D = 2048
SEQ = 16384
NCORES = 8
C0 = math.exp(-0.5)
EPS = 1e-6


class V:
    __slots__ = ("ap", "keys")

    def __init__(self, ap, keys):
        self.ap = ap
        self.keys = tuple(keys)

    def __getitem__(self, idx):
        return V(self.ap[idx], self.keys)

    def k(self, *keys):
        return V(self.ap, keys)


class KB:
    def __init__(self, nc, es):
        self.nc = nc
        self.es = es
        self.E = {"pe": nc.tensor, "act": nc.scalar, "dve": nc.vector, "pool": nc.gpsimd, "sp": nc.sync}
        self.sem = {}
        self.cnt = {}
        for e in ("pe", "act", "dve", "pool"):
            self.sem[e] = es.enter_context(nc.semaphore("s_" + e))
            self.cnt[e] = 0
        self.waited = {e: {} for e in self.E}
        self.buf = {}
        self.dsem = {}
        self.dcnt = {}
        self.dq = {}
        self.main_es = es

    def sb(self, name, shape, dt=F32):
        t = self.es.enter_context(self.nc.sbuf_tensor("sb_" + name, list(shape), dt))
        return V(t[:], (name,))

    def ps(self, name, shape, dt=F32):
        t = self.es.enter_context(self.nc.psum_tensor("ps_" + name, list(shape), dt))
        return V(t[:], (name,))

    def _deps(self, eng, outs, ins):
        toks = set()
        for v in ins:
            for key in v.keys:
                b = self.buf.get(key)
                if b is not None and b[0] is not None:
                    toks.add(b[0])
        for v in outs:
            for key in v.keys:
                b = self.buf.get(key)
                if b is not None:
                    if b[0] is not None:
                        toks.add(b[0])
                    toks.update(b[1].values())
        w = self.waited[eng]
        for (sname, val, owner) in sorted(toks):
            if owner == "pe" and eng == "pe":
                continue
            if w.get(sname, 0) < val:
                sem = self.sem[sname] if sname in self.sem else self.dsem[sname]
                self.E[eng].wait_ge(sem, val)
                w[sname] = val

    def _record(self, tok, outs, ins):
        for v in ins:
            for key in v.keys:
                b = self.buf.setdefault(key, [None, {}])
                b[1][tok[0]] = tok
        for v in outs:
            for key in v.keys:
                self.buf[key] = [tok, {}]

    def op(self, eng, fn, outs, ins):
        self._deps(eng, outs, ins)
        ins_obj = fn()
        self.cnt[eng] += 1
        ins_obj.then_inc(self.sem[eng], 1)
        tok = (eng, self.cnt[eng], eng)
        self._record(tok, outs, ins)

    NDSEM = 6

    def dma(self, q, out, in_, stream=None):
        n = self.dq.get(q, 0)
        self.dq[q] = n + 1
        stream = f"{q}{n % self.NDSEM}"
        if stream not in self.dsem:
            self.dsem[stream] = self.main_es.enter_context(self.nc.semaphore("d_" + stream))
            self.dcnt[stream] = 0
        self._deps(q, [out], [in_])
        w = self.waited[q]
        if w.get(stream, 0) < self.dcnt[stream]:
            self.E[q].wait_ge(self.dsem[stream], self.dcnt[stream])
            w[stream] = self.dcnt[stream]
        self.dcnt[stream] += 16
        self.E[q].dma_start(out=out.ap, in_=in_.ap).then_inc(self.dsem[stream], 16)
        tok = (stream, self.dcnt[stream], "dma")
        self._record(tok, [out], [in_])

    def wait_all(self, eng, vs):
        self._deps(eng, vs, vs)

    def mm(self, out, lhsT, rhs, start=True, stop=True):
        self.op("pe", lambda: self.nc.tensor.matmul(out.ap, lhsT=lhsT.ap, rhs=rhs.ap, start=start, stop=stop),
                [out], [lhsT, rhs])

    def tr(self, out, in_, ident):
        self.op("pe", lambda: self.nc.tensor.transpose(out.ap, in_.ap, ident.ap), [out], [in_, ident])

    def act(self, out, in_, func, bias=None, scale=None, accum=None):
        ins = [in_]
        kw = {}
        if bias is not None:
            if isinstance(bias, V):
                ins.append(bias)
                kw["bias"] = bias.ap
            else:
                kw["bias"] = float(bias)
        if scale is not None:
            if isinstance(scale, V):
                ins.append(scale)
                kw["scale"] = scale.ap
            else:
                kw["scale"] = float(scale)
        outs = [out]
        if accum is not None:
            outs.append(accum)
            kw["accum_out"] = accum.ap
        self.op("act", lambda: self.nc.scalar.activation(out=out.ap, in_=in_.ap, func=func, **kw), outs, ins)

    def ts(self, eng, out, in0, s1, s2=None, op0=ALU.mult, op1=None):
        ins = [in0]
        a1 = s1.ap if isinstance(s1, V) else float(s1)
        if isinstance(s1, V):
            ins.append(s1)
        kw = {}
        if op1 is not None:
            a2 = s2.ap if isinstance(s2, V) else float(s2)
            if isinstance(s2, V):
                ins.append(s2)
            kw["op1"] = op1
        else:
            a2 = None
        self.op(eng, lambda: self.E[eng].tensor_scalar(out=out.ap, in0=in0.ap, scalar1=a1, scalar2=a2, op0=op0, **kw),
                [out], ins)

    def tt(self, eng, out, in0, in1, op):
        self.op(eng, lambda: self.E[eng].tensor_tensor(out=out.ap, in0=in0.ap, in1=in1.ap, op=op), [out], [in0, in1])

    def stt(self, out, in0, scalar, in1, op0, op1):
        ins = [in0, in1]
        a = scalar.ap if isinstance(scalar, V) else float(scalar)
        if isinstance(scalar, V):
            ins.append(scalar)
        self.op("dve", lambda: self.nc.vector.scalar_tensor_tensor(out=out.ap, in0=in0.ap, scalar=a, in1=in1.ap,
                                                                   op0=op0, op1=op1), [out], ins)

    def cp(self, eng, out, in_):
        if eng == "act":
            self.act(out, in_, AF.Copy)
        else:
            self.op(eng, lambda: self.E[eng].tensor_copy(out=out.ap, in_=in_.ap), [out], [in_])

    def scan(self, out, d0, d1, init, op0, op1):
        self.op("dve", lambda: self.nc.vector.tensor_tensor_scan(out=out.ap, data0=d0.ap, data1=d1.ap, initial=init,
                                                                 op0=op0, op1=op1), [out], [d0, d1])

    def memset(self, eng, out, val):
        self.op(eng, lambda: self.E[eng].memset(out.ap, val), [out], [])

    def recip(self, out, in_):
        self.op("dve", lambda: self.nc.vector.reciprocal(out=out.ap, in_=in_.ap), [out], [in_])

    def barrier(self):
        for eng in self.E:
            w = self.waited[eng]
            for e2, sem in self.sem.items():
                if e2 != eng and self.cnt[e2] > w.get(e2, 0):
                    self.E[eng].wait_ge(sem, self.cnt[e2])
                    w[e2] = self.cnt[e2]
            for st_, sem in self.dsem.items():
                if self.dcnt[st_] > w.get(st_, 0):
                    self.E[eng].wait_ge(sem, self.dcnt[st_])
                    w[st_] = self.dcnt[st_]

    def finish(self, outs):
        self._deps("sp", outs, outs)


def dram_in(nc, name, shape, dt=F32):
    return V(nc.dram_tensor(name, list(shape), dt, kind="ExternalInput").ap(), ("dram_" + name,))


def dram_out(nc, name, shape, dt=F32):
    return V(nc.dram_tensor(name, list(shape), dt, kind="ExternalOutput").ap(), ("dram_" + name,))


TB = 256
PC_N = 96
PC_G, PC_SC, PC_SH = 0, 16, 32
PC_MU = 48
PC_W0, PC_A0, PC_KK, PC_KA, PC_RK, PC_LNW, PC_LNB, PC_V0 = 54, 55, 56, 57, 58, 59, 60, 61
PC_CQ, PC_CK, PC_CBQ, PC_CBK = 62, 66, 70, 71
PC_IB, PC_FB = 72, 73
PC_LB0, PC_LB1 = 74, 75
PC_MUV = 76
PM_W2, PM_A2, PM_G2, PM_V2, PM_V1 = 0, 64, 128, 192, 256
PM_ID, PM_ONES, PM_M5, PM_MM, PM_TRI, PM_MH, PM_RST = 384, 512, 640, 960, 1088, 1216, 1280
PM_RSTH = PM_RST + TB
PM_N = PM_RSTH + TB
LH = 32


def p1_cols(layer):
    cols = [("r", 64), ("k", 64), ("v", 64), ("xw", 64), ("xa", 64), ("xg", 128),
            ("mq", 128), ("mk", 128), ("mv", 128), ("mif", 2),
            ("hq", 128), ("hf", 128), ("hi", 64)]
    if layer == 1:
        cols += [("va0", 128), ("va1", 128), ("va2", 128), ("va3", 128)]
    return cols


def build_p1(layer, T, stage=99):
    nc = bass.Bass("TRN2", target_bir_lowering=False)
    cols = p1_cols(layer)
    NC1 = sum(m for _, m in cols)
    coff = {}
    o = 0
    for n, m in cols:
        coff[n] = (o, m)
        o += m
    NB = T // TB
    NCH = TB // 64
    NCM = TB // 128
    NCHH = TB // LH
    x_d = dram_in(nc, "x", [T, D])
    w1_d = dram_in(nc, "w1", [D, NC1])
    pc_d = dram_in(nc, "pc", [128, PC_N])
    pm_d = dram_in(nc, "pm", [128, PM_N])
    if layer == 1:
        vf_d = dram_in(nc, "vfirst", [64, T])
    ya_d = dram_out(nc, "yaT", [64, T])
    if layer == 0:
        vo_d = dram_out(nc, "vT", [64, T])
    ob_d = dram_out(nc, "obT", [128, T])
    oc_d = dram_out(nc, "ocT", [64, T])
    ssb_d = dram_out(nc, "ssb", [128, T // 128])
    ssc_d = dram_out(nc, "ssc", [LH, T // LH])

    with ExitStack() as es:
        kb = KB(nc, es)
        sb, ps = kb.sb, kb.ps
        pc = sb("pc", [128, PC_N])
        pm = sb("pm", [128, PM_N])
        Wb = sb("Wb", [128, 16, NC1], BF16)
        kb.dma("sp", pc, pc_d, "pc")
        kb.dma("sp", pm, pm_d, "pm")
        w1v = V(w1_d.ap.rearrange("(kc p) n -> p kc n", p=128), w1_d.keys)
        for kc in range(16):
            kb.dma("pool", Wb[:, kc, :], w1v[:, kc, :], "w1")
        ident = pm[:, PM_ID:PM_ID + 128]
        ones = pm[:, PM_ONES:PM_ONES + 128]
        col = lambda j, n=128: pc[0:n, j:j + 1]

        Acol = sb("Acol", [128, 16])
        kb.stt(Acol, pc[:, PC_SC:PC_SC + 16], 1.0, pc[:, PC_G:PC_G + 16], ALU.add, ALU.mult)
        Bcol = pc[:, PC_SH:PC_SH + 16]
        omka = sb("omka", [64, 1])
        kb.ts("dve", omka, col(PC_KA, 64), -1.0, 1.0, ALU.mult, ALU.add)
        ib15 = sb("ib15", [128, 1])
        kb.ts("dve", ib15, col(PC_IB), 1.0 / 15.0)
        fb15 = sb("fb15", [128, 1])
        kb.ts("dve", fb15, col(PC_FB), 1.0 / 15.0)
        lb = sb("lb", [128, 1])
        oml = sb("oml", [128, 1])
        if layer == 0:
            kb.memset("dve", lb, 0.0)
        else:
            dlb = sb("dlb", [128, 1])
            kb.tt("dve", dlb, col(PC_LB1), col(PC_LB0), ALU.subtract)
            kb.act(lb, dlb, AF.Sigmoid)
        kb.ts("dve", oml, lb, -1.0, 1.0, ALU.mult, ALU.add)

        xs = [sb(f"xs{i}", [128, TB // 128, D]) for i in range(2)]
        junk = sb("junk", [128, D], BF16)
        ss = sb("ss", [128, 4])
        rstd = sb("rstd", [128, 4])
        hT = [sb("hT0", [128, 16, TB], BF16)] * 2
        psT = [ps(f"psT{i}", [128, 512]) for i in range(2)]
        psP = [ps(f"psP{i}", [128, 512]) for i in range(2)]
        psR = ps("psR", [128, 512])
        psR2 = ps("psR2", [128, 512])
        psM = ps("psM", [128, 512])
        psH = ps("psH", [128, 512])
        qa = psR[0:64, 0:320].k("psR")
        qb = psR[0:64, 320:384].k("psR")
        qc = psR[0:64, 384:448].k("psR")
        qd = psR[0:64, 448:512].k("psR")
        qblk0 = psR[0:64, 0:TB].k("psR")
        qblk1 = psR[0:64, 256:256 + TB].k("psR")
        sa = psR2[0:64, 0:128].k("psR2")
        sbx = psR2[0:64, 128:192].k("psR2")
        sc_ = psR2[0:64, 192:256].k("psR2")
        sd = psR2[0:64, 256:448].k("psR2")
        se = psR2[0:64, 448:512].k("psR2")
        sblk0 = psR2[0:64, 0:TB].k("psR2")
        sblk1 = psR2[0:64, 256:256 + TB].k("psR2")
        mA = psM[:, 0:129].k("psM")
        mG = psM[:, 132:136].k("psM")
        hS = psM[:, 136:200].k("psM")
        hO = psM[0:64, 200:200 + LH].k("psM")
        hA = psM[0:LH, 264:264 + LH].k("psM")
        hT2 = psH[0:LH, 0:192].k("psH")
        ho = psH[0:LH, 192:256].k("psH")
        mB = psT[0][:, 256:384].k("psT0")
        mC = psT[1][:, 256:384].k("psT1")
        mD = psP[0][:, 256:385].k("psP0")
        mE = psP[1][:, 256:384].k("psP1")

        HAL = {"r": 1, "k": 1, "v": 1, "xw": 1, "xa": 1, "xg": 1, "mq": 3, "mk": 3,
               "va0": 1, "va1": 1, "va2": 1, "va3": 1}
        raw = {}
        for n, m in cols:
            h = HAL.get(n, 0)
            raw[n] = sb("raw_" + n, [m, h + TB])
            if h:
                kb.memset("pool", raw[n][:, 0:h], 0.0)

        def newt(name, m=64, w=TB):
            return sb(name, [m, w])

        tmp64 = newt("tmp64")
        tmp128 = newt("tmp128", 128)
        r_, k0, v_, xw, xa = [newt("rw_" + n) for n in ("r", "k0", "v", "xw", "xa")]
        xg = newt("rw_xg", 128)
        sg, cumS, p_, pinv, pprev, dcl = [newt("rw_" + n) for n in ("sg", "cumS", "p", "pinv", "pprev", "dcl")]
        txw = xw
        cumP = xa
        pLp = dcl
        pL = newt("rw_pL", 64, NCH)
        icl, kk, rn, t1, bt_, Bp = [newt("rw_" + n) for n in ("icl", "kk", "rn", "t1", "bt", "Bp")]
        kk2 = tmp64
        kkn = kk
        kmod = t1
        bvec = icl
        at_ = pprev
        kt_ = pinv
        rt_ = p_
        Kp = dcl
        sxg = xg
        rk = tmp64
        gate, bonus, ynT = [newt("rw_" + n) for n in ("gate", "bonus", "ynT")]
        yf = ynT
        if layer == 1:
            vall = sb("rw_vall", [128, 4, TB])
            vv1 = newt("rw_vv1", 32)
            vg, vfb, dv = [newt("rw_" + n) for n in ("vg", "vfb", "dv")]
        A5 = sb("rw_A5", [64, 320])
        PT = [sb(f"rw_PT{j}", [64, 128]) for j in range(5)]
        XT = [sb(f"rw_XT{j}", [64, 64]) for j in range(2)]
        tok3 = sb("rw_tok3", [64, 192])
        M0 = sb("rw_M0", [64, 64])
        U_ = sb("rw_U", [64, 64])
        Hst = sb("rw_H", [64, 64])
        kb.memset("pool", Hst, 0.0)
        Ysb = sb("rw_Y", [64, 64])
        ysq = sb("rw_ysq", [64, 64])
        st = sb("rw_st", [64, 8])
        yn = sb("rw_yn", [64, 64])

        mq, mk = [newt("ml_" + n, 128) for n in ("q", "k")]
        mq_acc, mk_acc = mq, mk
        gif = sb("ml_gif", [128, 2])
        mg = sb("ml_g", [128, 12])
        S0T = sb("ml_S0T", [128, 128])
        Vp = sb("ml_Vp", [128, 132])
        kb.memset("pool", Vp, 1.0)
        ktg = sb("ml_ktg", [128, 128])
        numE = sb("ml_numE", [128, 132])
        mh = sb("ml_h", [128, 128])
        mjunk = sb("ml_junk", [128, 128])
        Cst = sb("ml_C", [128, 132])
        kb.memset("pool", Cst, 0.0)
        obuf = newt("ml_obuf", 128)
        ssb_all = sb("ssb_all", [128, T // 128])
        kb.memset("pool", ssb_all, 0.0)

        hsf, hkraw, hcum, hp, hpinv, hdcl, hqs = [newt("hg_" + n, 128) for n in (
            "sf", "kraw", "cum", "p", "pinv", "dcl", "qs")]
        hf_ = hsf
        hlogf = hsf
        hpLp = hdcl
        hqt = hqs
        hkt = hpinv
        hkp = hdcl
        hpL = newt("hg_pL", 128, NCHH)
        attT = sb("hg_attT", [LH, LH])
        tok2 = sb("hg_tok2", [LH, 192])
        Sst = sb("hg_S", [128, 64])
        kb.memset("pool", Sst, 0.0)
        osb = sb("hg_o", [LH, 64])
        hjunk = sb("hg_junk", [LH, 64])
        ocbuf = newt("hg_ocbuf", 64)
        ssc_all = sb("ssc_all", [LH, T // LH])
        kb.memset("pool", ssc_all, 0.0)

        m5 = pm[0:64, PM_M5:PM_M5 + 320]
        maskM = pm[:, PM_MM:PM_MM + 128]
        triM = pm[:, PM_TRI:PM_TRI + 128]
        maskH = pm[0:LH, PM_MH:PM_MH + LH]
        rstH = pm[:, PM_RSTH:PM_RSTH + TB]
        idLH = pm[0:LH, PM_ID:PM_ID + LH]
        rst64 = pm[0:64, PM_RST:PM_RST + TB]
        rst128 = pm[:, PM_RST:PM_RST + TB]
        id64 = pm[0:64, PM_ID:PM_ID + 64]
        ones64 = pm[0:64, PM_ONES:PM_ONES + 64]

        evac_i = [0]

        def evac(out, in_):
            e = ("act", "dve")[evac_i[0] % 2]
            evac_i[0] += 1
            kb.cp(e, out, in_)

        def shift(out, rawt, mucol, m, eng="pool"):
            t = tmp64 if m == 64 else tmp128
            kb.tt(eng, t, rawt[:, 0:TB], rawt[:, 1:1 + TB], ALU.subtract)
            kb.stt(out, t, mucol, rawt[:, 1:1 + TB], ALU.mult, ALU.add)

        for b in range(NB):
            t0 = b * TB
            sl = b % 2
            X = xs[sl]
            H = hT[sl]
            if b == 0:
                for tt in range(TB // 128):
                    kb.dma("sp", X[:, tt, :], x_d[tt * 128:(tt + 1) * 128, :], f"x{sl}{tt}")
            if b + 1 < NB:
                for tt in range(TB // 128):
                    kb.dma("sp", xs[1 - sl][:, tt, :], x_d[t0 + TB + tt * 128:t0 + TB + (tt + 1) * 128, :],
                           f"x{1 - sl}{tt}")
            for tt in range(TB // 128):
                kb.act(junk, X[:, tt, :], AF.Square, accum=ss[:, tt:tt + 1])
                kb.act(rstd[:, tt:tt + 1], ss[:, tt:tt + 1], AF.Sqrt, bias=EPS, scale=1.0 / D)
                kb.recip(rstd[:, tt:tt + 1], rstd[:, tt:tt + 1])
                kb.ts("pool", X[:, tt, :], X[:, tt, :], rstd[:, tt:tt + 1])
            for kc in range(16):
                pT = psT[kc % 2][:, 0:TB].k(f"psT{kc % 2}")
                for tt in range(TB // 128):
                    kb.tr(pT[:, tt * 128:(tt + 1) * 128], X[:, tt, kc * 128:(kc + 1) * 128], ident)
                if kc % 2 == 0:
                    kb.act(H[:, kc, :], pT[:, 0:TB], AF.Identity, bias=Bcol[:, kc:kc + 1], scale=Acol[:, kc:kc + 1])
                else:
                    kb.ts("dve", H[:, kc, :], pT[:, 0:TB], Acol[:, kc:kc + 1], Bcol[:, kc:kc + 1], ALU.mult, ALU.add)
            for ci, (n, m) in enumerate(cols):
                off = coff[n][0]
                pp = psP[ci % 2][:, 0:TB].k(f"psP{ci % 2}")
                for kc in range(16):
                    kb.mm(pp[0:m, 0:TB], Wb[:, kc, off:off + m], H[:, kc, :], start=(kc == 0), stop=(kc == 15))
                h = HAL.get(n, 0)
                evac(raw[n][:, h:h + TB], pp[0:m, 0:TB])

            if stage < 1:
                continue
            shift(r_, raw["r"], col(PC_MU + 0, 64), 64)
            shift(k0, raw["k"], col(PC_MU + 1, 64), 64)
            shift(v_, raw["v"], col(PC_MU + 2, 64), 64)
            shift(xw, raw["xw"], col(PC_MU + 3, 64), 64)
            shift(xa, raw["xa"], col(PC_MU + 4, 64), 64)
            shift(xg, raw["xg"], col(PC_MU + 5, 128), 128)
            kb.act(txw, xw, AF.Tanh)
            kb.mm(qblk0, pm[0:64, PM_W2:PM_W2 + 64], txw)
            kb.act(sg, qblk0, AF.Sigmoid, bias=col(PC_W0, 64))
            kb.scan(cumS, rst64, sg, 0.0, ALU.mult, ALU.add)
            kb.mm(qblk1, pm[0:64, PM_A2:PM_A2 + 64], xa)
            kb.act(icl, qblk1, AF.Sigmoid, bias=col(PC_A0, 64))
            kb.tt("pool", cumP, cumS, sg, ALU.subtract)
            kb.act(p_, cumS, AF.Exp, scale=-C0)
            kb.act(pinv, cumS, AF.Exp, scale=C0)
            kb.act(pprev, cumP, AF.Exp, scale=-C0)
            cum3 = V(cumS.ap.rearrange("p (c l) -> p c l", l=64), cumS.keys)
            dcl3 = V(dcl.ap.rearrange("p (c l) -> p c l", l=64), dcl.keys)
            lastb = V(cum3.ap[:, :, 63:64].to_broadcast([64, NCH, 64]), cumS.keys)
            kb.tt("dve", dcl3, lastb, cum3, ALU.subtract)
            kb.act(pLp, dcl, AF.Exp, scale=-C0)
            kb.act(pL, V(cum3.ap[:, :, 63], cumS.keys), AF.Exp, scale=-C0)
            kb.ts("pool", kk, k0, col(PC_KK, 64))
            kb.tt("pool", kk2, kk, kk, ALU.mult)
            kb.mm(sblk0, ones64, kk2)
            kb.ts("dve", rn, sblk0, 1e-24, None, ALU.max)
            kb.act(rn, rn, AF.Sqrt)
            kb.recip(rn, rn)
            kb.tt("pool", kkn, kk, rn, ALU.mult)
            kb.ts("dve", t1, icl, col(PC_KA, 64), omka, ALU.mult, ALU.add)
            kb.tt("pool", kmod, k0, t1, ALU.mult)
            kb.tt("pool", bvec, kkn, icl, ALU.mult)
            kb.stt(at_, kkn, -1.0, pprev, ALU.mult, ALU.mult)
            kb.tt("pool", bt_, bvec, pinv, ALU.mult)
            kb.tt("dve", kt_, kmod, pinv, ALU.mult)
            kb.tt("pool", rt_, r_, p_, ALU.mult)
            kb.tt("dve", Bp, bvec, pLp, ALU.mult)
            kb.tt("pool", Kp, kmod, pLp, ALU.mult)
            if layer == 1:
                for i in range(4):
                    kb.tt("pool", tmp128, raw[f"va{i}"][:, 0:TB], raw[f"va{i}"][:, 1:1 + TB], ALU.subtract)
                    kb.stt(vall[:, i, :], tmp128, pc[:, PC_MUV + i:PC_MUV + i + 1], raw[f"va{i}"][:, 1:1 + TB],
                           ALU.mult, ALU.add)
                for i in range(4):
                    kb.mm(sblk1[0:32, :], pm[:, PM_V1 + 32 * i:PM_V1 + 32 * (i + 1)], vall[:, i, :],
                          start=(i == 0), stop=(i == 3))
                kb.cp("act", vv1, sblk1[0:32, :])
                kb.mm(sblk1, pm[0:32, PM_V2:PM_V2 + 64], vv1)
                kb.act(vg, sblk1, AF.Sigmoid, bias=col(PC_V0, 64))
                kb.dma("sp", vfb, vf_d[:, t0:t0 + TB], "vf")
                kb.tt("pool", dv, vfb, v_, ALU.subtract)
                kb.tt("pool", dv, dv, vg, ALU.mult)
                kb.tt("pool", v_, v_, dv, ALU.add)
            else:
                kb.dma("sp", vo_d[:, t0:t0 + TB], v_, "vo")
            kb.act(sxg, xg, AF.Sigmoid)
            kb.mm(sblk0, pm[:, PM_G2:PM_G2 + 64], sxg)
            kb.cp("act", gate, sblk0)
            kb.stt(rk, r_, col(PC_RK, 64), kmod, ALU.mult, ALU.mult)
            kb.mm(sblk1, ones64, rk)
            kb.tt("dve", bonus, sblk1, v_, ALU.mult)

            for c in range(NCH if stage >= 2 else 0):
                cs = slice(c * 64, (c + 1) * 64)
                P5 = qa
                kb.mm(qa[:, 0:64], bt_[:, cs], at_[:, cs])
                kb.mm(qa[:, 64:128], at_[:, cs], bt_[:, cs])
                kb.mm(qa[:, 128:192], kt_[:, cs], at_[:, cs])
                kb.mm(qa[:, 192:256], bt_[:, cs], rt_[:, cs])
                kb.mm(qa[:, 256:320], kt_[:, cs], rt_[:, cs])
                kb.tt("dve", A5, P5, m5, ALU.mult)
                if CUT < 1:
                    continue
                Tj, Pj = A5[:, 0:64], A5[:, 64:128]
                kb.tt("pool", XT[0], Tj, id64, ALU.add)
                xcur = 0
                for j in range(PJ):
                    kb.mm(sa[:, 0:64], Pj, Tj)
                    kb.mm(sa[:, 64:128], Tj, Pj)
                    evac(PT[j], sa)
                    Tj, Pj = PT[j][:, 0:64], PT[j][:, 64:128]
                    if not PX:
                        continue
                    kb.mm(sbx, Pj, XT[xcur])
                    kb.tt("dve", XT[1 - xcur], XT[xcur], sbx, ALU.add)
                    xcur = 1 - xcur
                XTf = XT[xcur]
                if CUT < 2:
                    continue
                kb.tr(sd[:, 0:64], v_[:, cs], id64)
                kb.tr(sd[:, 64:128], Bp[:, cs], id64)
                kb.tr(sd[:, 128:192], Kp[:, cs], id64)
                evac(tok3, sd)
                Vt, Bt, Kt = tok3[:, 0:64], tok3[:, 64:128], tok3[:, 128:192]
                if CUT < 3:
                    continue
                kb.mm(qb, at_[:, cs], Hst, start=True, stop=False)
                kb.mm(qb, A5[:, 128:192], Vt, start=False, stop=True)
                evac(M0, qb)
                kb.mm(qc, XTf, M0)
                evac(U_, qc)
                kb.mm(qd, rt_[:, cs], Hst, start=True, stop=False)
                kb.mm(qd, A5[:, 192:256], U_, start=False, stop=False)
                kb.mm(qd, A5[:, 256:320], Vt, start=False, stop=True)
                kb.mm(sc_, Bt, U_, start=True, stop=False)
                kb.mm(sc_, Kt, Vt, start=False, stop=True)
                kb.stt(Hst, Hst, pL[:, c:c + 1], sc_, ALU.mult, ALU.add)
                if CUT < 4:
                    continue
                kb.act(Ysb, qd, AF.Identity, accum=st[:, 0:1])
                kb.act(ysq, Ysb, AF.Square, accum=st[:, 1:2])
                kb.ts("dve", st[:, 2:3], st[:, 0:1], 1.0 / 64.0)
                kb.tt("dve", st[:, 3:4], st[:, 2:3], st[:, 2:3], ALU.mult)
                kb.stt(st[:, 4:5], st[:, 1:2], 1.0 / 64.0, st[:, 3:4], ALU.mult, ALU.subtract)
                kb.act(st[:, 5:6], st[:, 4:5], AF.Sqrt, bias=64e-5)
                kb.recip(st[:, 5:6], st[:, 5:6])
                kb.ts("dve", yn, Ysb, st[:, 2:3], st[:, 5:6], ALU.subtract, ALU.mult)
                kb.tr(se, yn, id64)
                evac(ynT[:, cs], se)
            kb.ts("dve", yf, ynT, col(PC_LNW, 64), col(PC_LNB, 64), ALU.mult, ALU.add)
            kb.tt("pool", yf, yf, bonus, ALU.add)
            kb.tt("pool", yf, yf, gate, ALU.mult)
            kb.dma("sp", ya_d[:, t0:t0 + TB], yf, "ya")

            if stage < 3:
                continue
            for (acc, rw, cw, cb, outq) in ((mq_acc, raw["mq"], PC_CQ, PC_CBQ, mq), (mk_acc, raw["mk"], PC_CK, PC_CBK, mk)):
                kb.ts("dve", acc, rw[:, 0:TB], col(cw), col(cb), ALU.mult, ALU.add)
                for j in range(1, 4):
                    kb.stt(acc, rw[:, j:j + TB], col(cw + j), acc, ALU.mult, ALU.add)
                kb.act(outq, acc, AF.Silu)
            for c in range(NCM):
                cs = slice(c * 128, (c + 1) * 128)
                gi = b * NCM + c
                kb.tr(mG[:, 0:2], raw["mif"][:, cs], pm[0:2, PM_ID:PM_ID + 2])
                kb.cp("dve", gif, mG[:, 0:2])
                kb.act(mg[:, 0:1], gif[:, 0:1], AF.Tanh, bias=ib15, scale=1.0 / 15.0)
                kb.act(mg[:, 1:2], gif[:, 1:2], AF.Tanh, bias=fb15, scale=1.0 / 15.0)
                kb.act(mg[:, 2:3], mg[:, 1:2], AF.Exp, scale=-15.0)
                kb.act(mg[:, 3:4], mg[:, 2:3], AF.Ln, bias=1.0)
                kb.mm(mG[:, 2:3], triM, mg[:, 3:4])
                kb.mm(mG[:, 3:4], ones, mg[:, 3:4])
                kb.cp("dve", mg[:, 10:12], mG[:, 2:4])
                kb.act(mg[:, 4:5], mg[:, 10:11], AF.Exp, scale=-1.0)
                kb.stt(mg[:, 5:6], mg[:, 0:1], 15.0, mg[:, 10:11], ALU.mult, ALU.add)
                kb.act(mg[:, 6:7], mg[:, 5:6], AF.Exp, bias=math.log(128.0 ** -0.5))
                kb.act(mg[:, 7:8], mg[:, 11:12], AF.Exp, scale=-1.0)
                kb.mm(mA[:, 0:128], mk[:, cs], mq[:, cs])
                kb.stt(S0T, mA[:, 0:128], mg[:, 6:7], maskM, ALU.mult, ALU.mult)
                kb.tr(mB, raw["mv"][:, cs], ident)
                kb.cp("act", Vp[:, 0:128], mB)
                kb.tr(mC, mk[:, cs], ident)
                kb.ts("dve", ktg, mC, mg[:, 6:7])
                kb.mm(mD, S0T, Vp[:, 0:129], start=True, stop=False)
                kb.mm(mD, mq[:, cs], Cst[:, 0:129], start=False, stop=True)
                kb.ts("dve", numE[:, 0:129], mD, mg[:, 4:5])
                kb.act(mg[:, 8:9], numE[:, 128:129], AF.Abs)
                kb.ts("dve", mg[:, 8:9], mg[:, 8:9], 1.0, None, ALU.max)
                kb.recip(mg[:, 9:10], mg[:, 8:9])
                kb.ts("dve", mh, numE[:, 0:128], mg[:, 9:10])
                kb.act(mjunk, mh, AF.Square, accum=ssb_all[:, gi:gi + 1])
                kb.tr(mE, mh, ident)
                evac(obuf[:, cs], mE)
                kb.mm(mA, ktg, Vp[:, 0:129])
                kb.ts("dve", Cst[:, 0:129], Cst[:, 0:129], mg[:, 7:8])
                kb.stt(Cst[:, 0:129], mA, mg[:, 7:8], Cst[:, 0:129], ALU.mult, ALU.add)
            kb.dma("sp", ob_d[:, t0:t0 + TB], obuf, "ob")

            if stage < 4:
                continue
            kb.act(hsf, raw["hf"], AF.Sigmoid)
            kb.ts("dve", hf_, hsf, oml, lb, ALU.mult, ALU.add)
            kb.act(hlogf, hf_, AF.Ln)
            kb.act(hkraw, raw["hf"], AF.Sigmoid, scale=-1.0)
            kb.scan(hcum, rstH, hlogf, 0.0, ALU.mult, ALU.add)
            kb.act(hp, hcum, AF.Exp)
            kb.act(hpinv, hcum, AF.Exp, scale=-1.0)
            hc3 = V(hcum.ap.rearrange("p (c l) -> p c l", l=LH), hcum.keys)
            hd3 = V(hdcl.ap.rearrange("p (c l) -> p c l", l=LH), hdcl.keys)
            hlast = V(hc3.ap[:, :, LH - 1:LH].to_broadcast([128, NCHH, LH]), hcum.keys)
            kb.tt("dve", hd3, hlast, hc3, ALU.subtract)
            kb.act(hpLp, hdcl, AF.Exp)
            kb.act(hpL, V(hc3.ap[:, :, LH - 1], hcum.keys), AF.Exp)
            kb.act(hqs, raw["hq"], AF.Silu)
            kb.tt("pool", hqt, hqs, hp, ALU.mult)
            kb.stt(hkt, hkraw, oml, hpinv, ALU.mult, ALU.mult)
            kb.stt(hkp, hkraw, oml, hpLp, ALU.mult, ALU.mult)
            for c in range(NCHH):
                cs = slice(c * LH, (c + 1) * LH)
                gi = b * NCHH + c
                kb.mm(hA, hkt[:, cs], hqt[:, cs])
                kb.tt("dve", attT, hA, maskH, ALU.mult)
                kb.tr(hT2[:, 0:64], raw["hi"][:, cs], id64)
                kb.tr(hT2[:, 64:192], hkp[:, cs], ident)
                evac(tok2, hT2)
                kb.mm(ho, attT, tok2[:, 0:64], start=True, stop=False)
                kb.mm(ho, hqt[:, cs], Sst, start=False, stop=True)
                kb.mm(hS, tok2[:, 64:192], tok2[:, 0:64])
                kb.stt(Sst, Sst, hpL[:, c:c + 1], hS, ALU.mult, ALU.add)
                kb.cp("act", osb, ho)
                kb.act(hjunk, osb, AF.Square, accum=ssc_all[:, gi:gi + 1])
                kb.tr(hO, osb, idLH)
                evac(ocbuf[:, cs], hO)
            kb.dma("sp", oc_d[:, t0:t0 + TB], ocbuf, "oc")

            for n, h in HAL.items():
                if n in raw:
                    kb.cp("pool", raw[n][:, 0:h], raw[n][:, TB:TB + h])

        kb.dma("sp", ssb_d, ssb_all, "ssb")
        kb.dma("sp", ssc_d, ssc_all, "ssc")
        outs = [ya_d, ob_d, oc_d, ssb_d, ssc_d] + ([vo_d] if layer == 0 else [])
        kb.finish(outs)
    return nc


def p1_host_inputs(layer, inp, mod, core, T):
    l = layer
    hd = core
    ph, hf = core // 2, core % 2
    w_in = inp["w_in"][l]
    RW = 512
    o_r, o_k, o_v, o_xw, o_xa, o_xg = 0, 512, 1024, 1536, 1600, 1664
    o_rest = 1792
    o_mq, o_mk, o_mv, o_mo, o_mi, o_mf = (o_rest, o_rest + 512, o_rest + 1024, o_rest + 2048, o_rest + 3072, o_rest + 3076)
    o_hq = o_rest + 3080
    o_hf, o_hi, o_hg = o_hq + 512, o_hq + 1024, o_hq + 1536
    h64 = slice(hd * 64, hd * 64 + 64)

    def rng(o, n):
        return list(range(o, o + n))

    idx = (rng(o_r + hd * 64, 64) + rng(o_k + hd * 64, 64) + rng(o_v + hd * 64, 64) + rng(o_xw, 64) + rng(o_xa, 64)
           + rng(o_xg, 128)
           + rng(o_mq + ph * 128, 128) + rng(o_mk + ph * 128, 128) + rng(o_mv + ph * 256 + hf * 128, 128)
           + [o_mi + ph, o_mf + ph]
           + rng(o_hq + ph * 128, 128) + rng(o_hf + ph * 128, 128) + rng(o_hi + ph * 128 + hf * 64, 64))
    if l == 1:
        idx += rng(o_v, 512)
    w1 = np.ascontiguousarray(w_in[:, idx])
    pc = np.zeros((128, PC_N), np.float32)
    pc[:, PC_G:PC_G + 16] = inp["norm_mix"][l].reshape(16, 128).T
    sh_m, sc_m = mod[l][0:D], mod[l][D:2 * D]
    pc[:, PC_SC:PC_SC + 16] = sc_m.reshape(16, 128).T
    pc[:, PC_SH:PC_SH + 16] = sh_m.reshape(16, 128).T
    mu = inp["rw_mu"][l]
    pc[0:64, PC_MU + 0] = mu[o_r + hd * 64:o_r + hd * 64 + 64]
    pc[0:64, PC_MU + 1] = mu[o_k + hd * 64:o_k + hd * 64 + 64]
    pc[0:64, PC_MU + 2] = mu[o_v + hd * 64:o_v + hd * 64 + 64]
    pc[0:64, PC_MU + 3] = mu[o_xw:o_xw + 64]
    pc[0:64, PC_MU + 4] = mu[o_xa:o_xa + 64]
    pc[0:128, PC_MU + 5] = mu[o_xg:o_xg + 128]
    pc[0:64, PC_W0] = inp["rw_w0"][l][h64]
    pc[0:64, PC_A0] = inp["rw_a0"][l][h64]
    pc[0:64, PC_KK] = inp["rw_kk"][l][h64]
    pc[0:64, PC_KA] = inp["rw_ka"][l][h64]
    pc[0:64, PC_RK] = inp["rw_rk"][l][h64]
    pc[0:64, PC_LNW] = inp["rw_lnw"][l][h64]
    pc[0:64, PC_LNB] = inp["rw_lnb"][l][h64]
    if l == 1:
        pc[0:64, PC_V0] = inp["rw_v0"][0][h64]
        pc[:, PC_MUV:PC_MUV + 4] = mu[o_v:o_v + 512].reshape(4, 128).T
    cw = inp["ml_conv_w"][l]
    cb = inp["ml_conv_b"][l]
    for j in range(4):
        pc[:, PC_CQ + j] = cw[j, ph * 128:(ph + 1) * 128]
        pc[:, PC_CK + j] = cw[j, 512 + ph * 128:512 + (ph + 1) * 128]
    pc[:, PC_CBQ] = cb[ph * 128:(ph + 1) * 128]
    pc[:, PC_CBK] = cb[512 + ph * 128:512 + (ph + 1) * 128]
    pc[:, PC_IB] = inp["ml_ib"][l][ph]
    pc[:, PC_FB] = inp["ml_fb"][l][ph]
    pc[:, PC_LB0] = inp["hg_lb"][0][ph * 128:(ph + 1) * 128]
    pc[:, PC_LB1] = inp["hg_lb"][1][ph * 128:(ph + 1) * 128]
    pm = np.zeros((128, PM_N), np.float32)
    pm[0:64, PM_W2:PM_W2 + 64] = inp["rw_w2"][l][:, h64]
    pm[0:64, PM_A2:PM_A2 + 64] = inp["rw_a2"][l][:, h64]
    pm[0:128, PM_G2:PM_G2 + 64] = inp["rw_g2"][l][:, h64]
    if l == 1:
        pm[0:32, PM_V2:PM_V2 + 64] = inp["rw_v2"][0][:, h64]
        pm[:, PM_V1:PM_V1 + 128] = inp["rw_v1"][0].reshape(4, 128, 32).transpose(1, 0, 2).reshape(128, 128)
    pm[:, PM_ID:PM_ID + 128] = np.eye(128, dtype=np.float32)
    pm[:, PM_ONES:PM_ONES + 128] = 1.0
    s_ = np.arange(64)[:, None]
    t_ = np.arange(64)[None, :]
    mu_s = (s_ < t_).astype(np.float32)
    ml_s = (s_ > t_).astype(np.float32)
    mu_i = (s_ <= t_).astype(np.float32)
    pm[0:64, PM_M5:PM_M5 + 320] = np.concatenate([mu_s, ml_s, mu_s, mu_i, mu_i], axis=1)
    s2 = np.arange(128)[:, None]
    t2 = np.arange(128)[None, :]
    pm[:, PM_MM:PM_MM + 128] = (s2 <= t2).astype(np.float32)
    pm[:, PM_TRI:PM_TRI + 128] = (s2 <= t2).astype(np.float32)
    pm[0:LH, PM_MH:PM_MH + LH] = mu_i[0:LH, 0:LH]
    rsth = np.ones((128, TB), np.float32)
    rsth[:, ::LH] = 0.0
    pm[:, PM_RSTH:PM_RSTH + TB] = rsth
    rst = np.ones((128, TB), np.float32)
    rst[:, ::64] = 0.0
    pm[:, PM_RST:PM_RST + TB] = rst
    return {"w1": w1, "pc": pc, "pm": pm}


def build_p0():
    nc = bass.Bass("TRN2", target_bir_lowering=False)
    NCT = 24
    c_d = dram_in(nc, "c", [128, 16])
    w_d = dram_in(nc, "w", [D, NCT * 128])
    b_d = dram_in(nc, "b", [128, NCT])
    o_d = dram_out(nc, "mod", [128, NCT])
    with ExitStack() as es:
        kb = KB(nc, es)
        cc = kb.sb("cc", [128, 16])
        bb = kb.sb("bb", [128, NCT])
        oo = kb.sb("oo", [128, NCT])
        wb = [kb.sb(f"w{i}", [128, 16, 512]) for i in range(2)]
        pp = kb.ps("pp", [128, 512])
        kb.dma("sp", cc, c_d, "c")
        kb.dma("sp", bb, b_d, "b")
        kb.act(cc, cc, AF.Silu)
        wv = V(w_d.ap.rearrange("(kc p) n -> p kc n", p=128), w_d.keys)
        for pc_ in range(NCT // 4):
            W = wb[pc_ % 2]
            kb.dma("sp", W, wv[:, :, pc_ * 512:(pc_ + 1) * 512], f"w{pc_ % 2}")
            for j in range(4):
                ct = pc_ * 4 + j
                for kc in range(16):
                    kb.mm(pp[:, ct:ct + 1], W[:, kc, j * 128:(j + 1) * 128], cc[:, kc:kc + 1],
                          start=(kc == 0), stop=(kc == 15))
        kb.tt("dve", oo, pp[:, 0:NCT], bb, ALU.add)
        kb.dma("sp", o_d, oo, "o")
        kb.finish([o_d])
    return nc


NT2 = 2048
SPA = 512
SPB = 1024
P2C_N = 128
(P2_GM, P2_SCM, P2_SHM, P2_GF, P2_SCF, P2_SHF, P2_MLN, P2_HGN) = (0, 16, 32, 48, 64, 80, 96, 104)
NWG = 7680


def build_p2(final, NT=NT2, nexp=32, a_only=False):
    nc = bass.Bass("TRN2", target_bir_lowering=False)
    x_d = dram_in(nc, "x", [NT, D])
    ya_d = dram_in(nc, "yaT", [512, NT])
    ob_d = dram_in(nc, "obT", [1024, NT])
    oc_d = dram_in(nc, "ocT", [512, NT])
    ssb_d = dram_in(nc, "ssb", [8, NT])
    ssc_d = dram_in(nc, "ssc", [8, NT])
    sel_d = dram_in(nc, "sel", [8, 4 * 128])
    wg_d = dram_in(nc, "wg", [D, NWG])
    pabc_d = dram_in(nc, "pabc", [D, D])
    wo_d = dram_in(nc, "wout", [D, D])
    wr_d = dram_in(nc, "wr", [D, 36])
    rb_d = dram_in(nc, "rb", [128, 36])
    nh = nexp // 2
    eg_ds = [dram_in(nc, f"eg{i}", [nh, D, 1024]) for i in range(2)]
    eu_ds = [dram_in(nc, f"eu{i}", [nh, D, 1024]) for i in range(2)]
    ed_ds = [dram_in(nc, f"ed{i}", [nh, 1024, D]) for i in range(2)]
    pc_d = dram_in(nc, "pc2", [128, P2C_N])
    rowb_d = dram_in(nc, "rowb", [128, 3 * D])
    id_d = dram_in(nc, "ident", [128, 128])
    if a_only:
        xm_d = dram_out(nc, "xmid", [NT, D])
    else:
        xm_d = V(nc.dram_tensor("xmid", [NT, D], F32, kind="Internal").ap(), ("dram_xmid",))
    out_d = dram_out(nc, "xout", [NT, D])

    with ExitStack() as es:
        kb = KB(nc, es)
        sb, ps = kb.sb, kb.ps
        pc = sb("pc2", [128, P2C_N])
        rowb = sb("rowb", [128, D])
        ident = sb("ident", [128, 128])
        sel = sb("sel", [8, 512])
        kb.dma("sp", pc, pc_d, "pc")
        kb.dma("sp", rowb, rowb_d[:, D:2 * D], "rowb")
        kb.dma("sp", ident, id_d, "id")
        kb.dma("sp", sel, sel_d, "sel")
        ones = sb("ones", [128, 128])
        kb.memset("dve", ones, 1.0)
        Am = sb("Am", [128, 16])
        kb.stt(Am, pc[:, P2_SCM:P2_SCM + 16], 1.0, pc[:, P2_GM:P2_GM + 16], ALU.add, ALU.mult)
        Bm = pc[:, P2_SHM:P2_SHM + 16]
        Af = sb("Af", [128, 16])
        kb.stt(Af, pc[:, P2_SCF:P2_SCF + 16], 1.0, pc[:, P2_GF:P2_GF + 16], ALU.add, ALU.mult)
        Bf = pc[:, P2_SHF:P2_SHF + 16]
        gtf_b = rowb[:, 0:D]

        banks = [ps(f"bk{i}", [128, 512]) for i in range(8)]
        bk = lambda i: banks[i]

        RING = 4
        ring = [sb(f"ring{i}", [128, 4096], BF16) for i in range(RING)]
        ring_i = [0]

        def wload(src_view, shape3):
            r = ring[ring_i[0] % RING]
            nm = f"ring{ring_i[0] % RING}"
            ring_i[0] += 1
            a, b_ = shape3
            dst = V(r.ap[:, 0:a * b_].rearrange("p (a b) -> p a b", b=b_), r.keys)
            kb.dma("pool", dst, src_view, nm)
            return dst

        class Stream:
            def __init__(self, look):
                self.items = []
                self.bufs = {}
                self.next = 0
                self.look = look

            def add(self, fn):
                self.items.append(fn)
                return len(self.items) - 1

            def get(self, i):
                while self.next < len(self.items) and self.next <= i + self.look:
                    self.bufs[self.next] = self.items[self.next]()
                    self.next += 1
                return self.bufs.pop(i)

        wgv = V(wg_d.ap.rearrange("(kc p) n -> p kc n", p=128), wg_d.keys)
        pav = V(pabc_d.ap.rearrange("(kc p) n -> p kc n", p=128), pabc_d.keys)
        wov = V(wo_d.ap.rearrange("(kc p) n -> p kc n", p=128), wo_d.keys)

        es_main = kb.es
        es_a = ExitStack()
        kb.es = es_a
        xs = sb("xs", [128, SPA // 128, D])
        gtm_b = sb("gtm_b", [128, D])
        kb.dma("sp", gtm_b, rowb_d[:, 0:D], "gtm")
        xn = [sb(f"xn{i}", [128, D]) for i in range(2)]
        junk = sb("junk", [128, D], BF16)
        ss = sb("ss", [128, 8])
        rstd = sb("rstd", [128, 8])
        hT = sb("hT", [128, 16, SPA], BF16)
        yT = sb("yT", [128, 16, SPA], BF16)
        zT = sb("zT", [128, 16, SPA], BF16)
        ssrow = sb("ssrow", [8, 2, SPA])
        rsb = sb("rsb", [128, SPA])
        otmp = sb("otmp", [128, SPA])
        sgt = sb("sgt", [128, SPA])
        zacc = sb("zacc", [128, SPA])
        ztmp = sb("ztmp", [128, SPA])
        pbuf = [sb(f"pbuf{i}", [128, 16, 128], BF16) for i in range(2)]

        def norm_T(X, ntile, Acol, Bcol, Hout, wtok, b0, rt32=None):
            for tt in range(ntile):
                kb.act(junk, X[:, tt, :], AF.Square, accum=ss[:, tt:tt + 1])
                kb.act(rstd[:, tt:tt + 1], ss[:, tt:tt + 1], AF.Sqrt, bias=EPS, scale=1.0 / D)
                kb.recip(rstd[:, tt:tt + 1], rstd[:, tt:tt + 1])
                xx = xn[tt % 2]
                kb.ts("dve", xx, X[:, tt, :], rstd[:, tt:tt + 1])
                for k4 in range(4):
                    pb = bk(b0 + k4 % 2).k(f"bk{b0 + k4 % 2}")
                    for j in range(4):
                        kc = k4 * 4 + j
                        kb.tr(pb[:, j * 128:(j + 1) * 128], xx[:, kc * 128:(kc + 1) * 128], ident)
                    src32 = None
                    if rt32 is not None:
                        src32 = rt32(tt, k4, pb, 0)
                    for j in range(4):
                        kc = k4 * 4 + j
                        src = pb[:, j * 128:(j + 1) * 128] if src32 is None else src32[:, kc, :]
                        kb.act(Hout[:, kc, tt * 128:(tt + 1) * 128], src, AF.Identity,
                               bias=Bcol[:, kc:kc + 1], scale=Acol[:, kc:kc + 1])
                    if rt32 is not None:
                        rt32(tt, k4, pb, 1)

        for sp_ in range(NT // SPA):
            t0 = sp_ * SPA
            for tt in range(SPA // 128):
                kb.dma("sp", xs[:, tt, :], x_d[t0 + tt * 128:t0 + (tt + 1) * 128, :], f"xa{tt}")
            norm_T(xs, SPA // 128, Am, Bm, hT, SPA, 0)
            kb.dma("sp", ssrow[:, 0, :], ssb_d[:, t0:t0 + SPA], "ssr0")
            kb.dma("sp", ssrow[:, 1, :], ssc_d[:, t0:t0 + SPA], "ssr1")
            for i in range(4):
                kb.dma("pool", yT[:, i, :], ya_d[i * 128:(i + 1) * 128, t0:t0 + SPA], "yTa")
            st = Stream(2)
            for i in range(12):
                c0 = 6144 + i * 128
                st.add(lambda c0=c0: wload(wgv[:, :, c0:c0 + 128], (16, 128)))
            for i in range(12):
                isb = i < 8
                hd = (i // 2) if isb else (i - 8)
                if (isb and i % 2 == 0) or (not isb):
                    pr = bk(2).k("bk2")
                    kb.mm(pr[:, 0:SPA], sel[:, hd * 128:(hd + 1) * 128], ssrow[:, 0 if isb else 1, :])
                    kb.act(rsb, pr[:, 0:SPA], AF.Sqrt, bias=EPS, scale=(1.0 / 256.0 if isb else 1.0 / 128.0))
                    kb.recip(rsb, rsb)
                src = ob_d[i * 128:(i + 1) * 128, t0:t0 + SPA] if isb else oc_d[(i - 8) * 128:(i - 7) * 128, t0:t0 + SPA]
                kb.dma("sp", otmp, src, "otmp")
                W = st.get(i)
                pg = bk(3).k("bk3")
                for kc in range(16):
                    kb.mm(pg[:, 0:SPA], W[:, kc, :], hT[:, kc, :], start=(kc == 0), stop=(kc == 15))
                kb.act(sgt, pg[:, 0:SPA], AF.Sigmoid if isb else AF.Silu)
                kb.tt("dve", otmp, otmp, rsb, ALU.mult)
                ncol = (P2_MLN + i) if isb else (P2_HGN + i - 8)
                kb.stt(yT[:, 4 + i, :], otmp, pc[:, ncol:ncol + 1], sgt, ALU.mult, ALU.mult)
            st = Stream(3)
            for dt in range(16):
                for br in range(3):
                    c0 = br * 2048 + dt * 128
                    st.add(lambda c0=c0: wload(wgv[:, :, c0:c0 + 128], (16, 128)))
            CH = ((0, 4), (4, 12), (12, 16))
            kb.dma("pool", pbuf[0], pav[:, :, 0:128], "pbuf0")
            for dt in range(16):
                if dt + 1 < 16:
                    kb.dma("pool", pbuf[(dt + 1) % 2], pav[:, :, (dt + 1) * 128:(dt + 2) * 128], f"pbuf{(dt + 1) % 2}")
                Wp = pbuf[dt % 2]
                Wg3 = [None, None, None]
                for br in range(3):
                    Wg3[br] = st.get(dt * 3 + br)
                    pg = bk(4 + br % 2).k(f"bk{4 + br % 2}")
                    for kc in range(16):
                        kb.mm(pg[:, 0:SPA], Wg3[br][:, kc, :], hT[:, kc, :], start=(kc == 0), stop=(kc == 15))
                    kb.act(sgt, pg[:, 0:SPA], AF.Sigmoid)
                    pq = bk(6 + br % 2).k(f"bk{6 + br % 2}")
                    lo, hi = CH[br]
                    for ch in range(lo, hi):
                        kb.mm(pq[:, 0:SPA], Wp[:, ch, :], yT[:, ch, :], start=(ch == lo), stop=(ch == hi - 1))
                    if br == 0:
                        kb.tt("dve", zacc, sgt, pq[:, 0:SPA], ALU.mult)
                    elif br == 1:
                        kb.tt("dve", ztmp, sgt, pq[:, 0:SPA], ALU.mult)
                        kb.tt("dve", zacc, zacc, ztmp, ALU.add)
                    else:
                        kb.tt("dve", ztmp, sgt, pq[:, 0:SPA], ALU.mult)
                        kb.tt("dve", zT[:, dt, :], zacc, ztmp, ALU.add)
            st = Stream(2)
            for p8 in range(8):
                st.add(lambda p8=p8: wload(wov[:, :, p8 * 256:(p8 + 1) * 256], (16, 256)))
            for p8 in range(8):
                W = st.get(p8)
                cs = slice(p8 * 256, (p8 + 1) * 256)
                for tt in range(SPA // 128):
                    po = bk(tt % 2).k(f"bk{tt % 2}")
                    for kc in range(16):
                        kb.mm(po[:, 0:256], zT[:, kc, tt * 128:(tt + 1) * 128], W[:, kc, :], start=(kc == 0), stop=(kc == 15))
                    kb.tt("dve", ztmp[:, 0:256], po[:, 0:256], gtm_b[:, cs], ALU.mult)
                    kb.tt("dve", xs[:, tt, cs], xs[:, tt, cs], ztmp[:, 0:256], ALU.add)
            for tt in range(SPA // 128):
                kb.dma("sp", xm_d[t0 + tt * 128:t0 + (tt + 1) * 128, :], xs[:, tt, :], f"xm{tt}")

        if a_only:
            kb.finish([xm_d])
            kb.barrier()
            es_a.close()
            return nc
        kb.barrier()
        es_a.close()
        kb.es = es_main
        actT_raw = sb("actT", [128, 8 * SPB], BF16)
        actT = V(actT_raw.ap.rearrange("p (a b) -> p a b", b=SPB), actT_raw.keys)
        xn = [V(actT_raw.ap[:, 0:2 * D].bitcast(F32), actT_raw.keys)] * 2
        junk = actT_raw[:, 2 * D:3 * D]
        ss = sb("ssb_", [128, 8])
        rstd = sb("rstdb", [128, 8])
        xacc = sb("xacc", [128, SPB // 128, D])
        hT2 = sb("hT2", [128, 16, SPB], BF16)
        xT32 = sb("xT32", [128, 16, 128])
        wr = sb("wr", [128, 16, 36])
        wr2 = wr
        rbrow = sb("rbrow", [128, 36])
        brow = sb("brow", [128, 36])
        lg = sb("lg", [128, 36])
        rt = sb("rt", [128, 48])
        em = sb("em", [128, 32])
        em2 = sb("em2", [128, 32])
        oh1 = sb("oh1", [128, 32])
        oh2 = sb("oh2", [128, 32])
        coef = sb("coef", [128, SPB // 128, 32])
        gsil = sb("gsil", [128, 512])
        dtmp = sb("dtmp", [128, 512])
        if P2CUT < 0:
            kb.memset("dve", xacc[:, 0, :], 1.0)
            kb.dma("sp", out_d[0:128, :], xacc[:, 0, :])
            kb.finish([out_d])
            return nc
        kb.dma("sp", wr, V(wr_d.ap.rearrange("(kc p) n -> p kc n", p=128), wr_d.keys), "wr")
        kb.dma("sp", rbrow, rb_d, "rb")
        pbr = bk(7).k("bk7")
        kb.cp("dve", xT32, V(Bf.ap.unsqueeze(2).to_broadcast([128, 16, 128]), Bf.keys))
        for kc in range(16):
            kb.mm(pbr[:, 0:36], xT32[:, kc, :], wr[:, kc, :], start=(kc == 0), stop=(kc == 15))
        kb.tt("dve", brow, pbr[:, 0:36], rbrow, ALU.add)
        kb.tt("dve", wr2, wr, V(Af.ap.unsqueeze(2).to_broadcast([128, 16, 36]), Af.keys), ALU.mult)
        egvs = [V(t.ap.rearrange("e (kc p) n -> p e kc n", p=128), t.keys) for t in eg_ds]
        euvs = [V(t.ap.rearrange("e (kc p) n -> p e kc n", p=128), t.keys) for t in eu_ds]
        edvs = [V(t.ap.rearrange("e (kc p) n -> p e kc n", p=128), t.keys) for t in ed_ds]

        if P2CUT == 0:
            kb.memset("dve", xacc[:, 0, :], 1.0)
            kb.cp("dve", xacc[:, 0, 0:36], brow)
            kb.dma("sp", out_d[0:128, :], xacc[:, 0, :])
            kb.finish([out_d])
            return nc
        for pb_ in range(NT // SPB):
            t0 = pb_ * SPB
            NTT = SPB // 128
            kb.wait_all("sp", [xm_d])
            for tt in range(NTT):
                kb.dma("sp", xacc[:, tt, :], xm_d[t0 + tt * 128:t0 + (tt + 1) * 128, :], f"xb{tt}")

            def rt32(tt, k4, pbank, phase):
                if phase == 0:
                    kb.cp("dve", V(xT32.ap[:, k4 * 4:(k4 + 1) * 4, :].rearrange("p a b -> p (a b)"), xT32.keys), pbank[:, 0:512])
                    return xT32
                if k4 == 3:
                    pl = bk(6).k("bk6")
                    for kc in range(16):
                        kb.mm(pl[:, 0:36], xT32[:, kc, :], wr2[:, kc, :], start=(kc == 0), stop=(kc == 15))
                    if P2CUT != 25:
                        route(tt, pl)
                    else:
                        kb.cp("dve", lg, pl[:, 0:36])

            def route(tt, pl):
                kb.tt("dve", lg, pl[:, 0:36], brow, ALU.add)
                r_ = lambda j: rt[:, j:j + 1]
                kb.op("dve", lambda: nc.vector.tensor_reduce(out=r_(0).ap, in_=lg[:, 0:4].ap, axis=AX.X, op=ALU.max),
                      [rt], [lg])
                kb.ts("dve", r_(1), r_(0), -1.0)
                kb.act(rt[:, 8:12], lg[:, 0:4], AF.Exp, bias=r_(1), accum=r_(2))
                kb.recip(r_(3), r_(2))
                kb.ts("dve", rt[:, 12:16], lg[:, 0:4], r_(0), None, ALU.is_equal)
                kb.ts("dve", rt[:, 16:20], rt[:, 12:16], 1e30, -1e30, ALU.mult, ALU.add)
                em3 = V(em.ap.rearrange("p (g e) -> p g e", e=8), em.keys)
                el3 = V(lg.ap[:, 4:36].rearrange("p (g e) -> p g e", e=8), lg.keys)
                pen = V(rt.ap[:, 16:20].unsqueeze(2).to_broadcast([128, 4, 8]), rt.keys)
                kb.tt("dve", em3, el3, pen, ALU.add)
                kb.op("dve", lambda: nc.vector.tensor_reduce(out=r_(4).ap, in_=em.ap, axis=AX.X, op=ALU.max), [rt], [em])
                kb.ts("dve", oh1, em, r_(4), None, ALU.is_equal)
                kb.stt(em2, oh1, -1e30, em, ALU.mult, ALU.add)
                kb.op("dve", lambda: nc.vector.tensor_reduce(out=r_(5).ap, in_=em2.ap, axis=AX.X, op=ALU.max), [rt], [em2])
                kb.ts("dve", oh2, em2, r_(5), None, ALU.is_equal)
                kb.ts("dve", r_(6), r_(4), -1.0)
                kb.act(r_(7), r_(5), AF.Exp, bias=r_(6))
                kb.ts("dve", r_(20), r_(7), 1.0, None, ALU.add)
                kb.recip(r_(21), r_(20))
                kb.tt("dve", r_(22), r_(7), r_(21), ALU.mult)
                kb.tt("dve", r_(23), r_(21), r_(3), ALU.mult)
                kb.tt("dve", r_(24), r_(22), r_(3), ALU.mult)
                kb.ts("dve", coef[:, tt, :], oh1, r_(23))
                kb.stt(coef[:, tt, :], oh2, r_(24), coef[:, tt, :], ALU.mult, ALU.add)

            if P2CUT >= 2:
                norm_T(xacc, NTT, Af, Bf, hT2, SPB, 4, rt32=(rt32 if (P2CUT >= 3 or P2CUT == 25) else None))

            st = Stream(2)
            for e in range(nexp):
                egv, euv, edv, el_ = egvs[e // nh], euvs[e // nh], edvs[e // nh], e % nh
                for q in range(4):
                    st.add(lambda v=egv, e=el_, q=q: wload(v[:, e, :, q * 256:(q + 1) * 256], (16, 256)))
                    st.add(lambda v=euv, e=el_, q=q: wload(v[:, e, :, q * 256:(q + 1) * 256], (16, 256)))
                for q in range(4):
                    st.add(lambda v=edv, e=el_, q=q: wload(v[:, e, :, q * 512:(q + 1) * 512], (8, 512)))
            for e in range(nexp if (P2CUT >= 4 and P2CUT != 25) else 0):
                base = e * 12
                for q in range(4):
                    Wg = st.get(base + 2 * q)
                    Wu = st.get(base + 2 * q + 1)
                    for f2 in range(2):
                        ft = q * 2 + f2
                        for hh in range(SPB // 512):
                            pgt = bk(2 * hh).k(f"bk{2 * hh}")
                            put = bk(2 * hh + 1).k(f"bk{2 * hh + 1}")
                            tsl = slice(hh * 512, (hh + 1) * 512)
                            for kc in range(16):
                                kb.mm(pgt, Wg[:, kc, f2 * 128:(f2 + 1) * 128], hT2[:, kc, tsl], start=(kc == 0), stop=(kc == 15))
                            for kc in range(16):
                                kb.mm(put, Wu[:, kc, f2 * 128:(f2 + 1) * 128], hT2[:, kc, tsl], start=(kc == 0), stop=(kc == 15))
                            kb.act(gsil, pgt, AF.Silu)
                            kb.tt("dve", actT[:, ft, tsl], gsil, put, ALU.mult)
                for q in range(4):
                    Wd = st.get(base + 8 + q)
                    cs = slice(q * 512, (q + 1) * 512)
                    for tt in range(NTT):
                        pd = bk(4 + tt % 2).k(f"bk{4 + tt % 2}")
                        for ft in range(8):
                            kb.mm(pd, actT[:, ft, tt * 128:(tt + 1) * 128], Wd[:, ft, :], start=(ft == 0), stop=(ft == 7))
                        kb.tt("dve", dtmp, pd, gtf_b[:, cs], ALU.mult)
                        kb.stt(xacc[:, tt, cs], dtmp, coef[:, tt, e:e + 1], xacc[:, tt, cs], ALU.mult, ALU.add)
            if final:
                fng_b = V(ring[0].ap.bitcast(F32), ring[0].keys)
                kb.dma("sp", fng_b, rowb_d[:, 2 * D:3 * D], "fng")
            for tt in range(NTT):
                if final:
                    kb.act(junk, xacc[:, tt, :], AF.Square, accum=ss[:, tt:tt + 1])
                    kb.act(rstd[:, tt:tt + 1], ss[:, tt:tt + 1], AF.Sqrt, bias=EPS, scale=1.0 / D)
                    kb.recip(rstd[:, tt:tt + 1], rstd[:, tt:tt + 1])
                    kb.stt(xacc[:, tt, :], xacc[:, tt, :], rstd[:, tt:tt + 1], fng_b, ALU.mult, ALU.mult)
                kb.dma("sp", out_d[t0 + tt * 128:t0 + (tt + 1) * 128, :], xacc[:, tt, :], f"xo{tt}")
        kb.finish([out_d])
    return nc


def p2_host_inputs(layer, inp, mod, final):
    l = layer
    w_in = inp["w_in"][l]
    o_rest = 1792
    o_mo = o_rest + 2048
    o_hg = o_rest + 3080 + 1536
    o_ga = o_rest + 3080 + 2048
    wg = np.ascontiguousarray(np.concatenate([w_in[:, o_ga:o_ga + 6144], w_in[:, o_mo:o_mo + 1024],
                                              w_in[:, o_hg:o_hg + 512]], axis=1))
    pabc = np.ascontiguousarray(np.concatenate([inp["p_a"][l], inp["p_b"][l], inp["p_c"][l]], axis=0))
    m = mod[l]
    sh_m, sc_m, gt_m, sh_f, sc_f, gt_f = [m[i * D:(i + 1) * D] for i in range(6)]
    colz = lambda v: v.reshape(-1, 128).T
    pc = np.zeros((128, P2C_N), np.float32)
    pc[:, P2_GM:P2_GM + 16] = colz(inp["norm_mix"][l])
    pc[:, P2_SCM:P2_SCM + 16] = colz(sc_m)
    pc[:, P2_SHM:P2_SHM + 16] = colz(sh_m)
    pc[:, P2_GF:P2_GF + 16] = colz(inp["norm_ffn"][l])
    pc[:, P2_SCF:P2_SCF + 16] = colz(sc_f)
    pc[:, P2_SHF:P2_SHF + 16] = colz(sh_f)
    pc[:, P2_MLN:P2_MLN + 8] = colz(inp["ml_norm"][l])
    pc[:, P2_HGN:P2_HGN + 4] = colz(inp["hg_norm"][l])
    rowb = np.ascontiguousarray(np.broadcast_to(np.concatenate([gt_m, gt_f, inp["final_norm"]])[None, :], (128, 3 * D)))
    sel = np.zeros((8, 4, 128), np.float32)
    for h in range(4):
        sel[2 * h:2 * h + 2, h, :] = 1.0
    return {
        "wg": wg, "pabc": pabc, "wout": np.ascontiguousarray(inp["w_out"][l]),
        "wr": np.ascontiguousarray(np.concatenate([inp["moe_gw"][l], inp["moe_ew"][l]], axis=1)),
        "rb": np.ascontiguousarray(np.broadcast_to(np.concatenate([inp["moe_gb"][l], inp["moe_eb"][l]])[None, :], (128, 36))),
        "eg0": inp["ex_gate"][l][:16], "eg1": inp["ex_gate"][l][16:],
        "eu0": inp["ex_up"][l][:16], "eu1": inp["ex_up"][l][16:],
        "ed0": inp["ex_down"][l][:16], "ed1": inp["ex_down"][l][16:],
        "pc2": pc, "rowb": rowb.astype(np.float32), "ident": np.eye(128, dtype=np.float32),
        "sel": sel.reshape(8, 512),
    }


_CACHE = {}


def _prog(key, fn):
    if key not in _CACHE:
        _CACHE[key] = fn()
    return _CACHE[key]


def kernel(**inp):
    inp = {k: np.asarray(v) for k, v in inp.items()}
    T = SEQ
    cores = list(range(NCORES))
    c_col = np.ascontiguousarray(inp["c"][0].reshape(16, 128).T)
    adaw = inp["ada_w"]
    adab = inp["ada_b"]
    in_maps = []
    for c in cores:
        l, part = c // 4, c % 4
        cols = slice(part * 3072, (part + 1) * 3072)
        in_maps.append({"c": c_col, "w": np.ascontiguousarray(adaw[l][:, cols]),
                        "b": np.ascontiguousarray(adab[l][cols].reshape(24, 128).T)})
    res = run_bass_kernel_spmd(_prog("p0", build_p0), in_maps, core_ids=cores)
    mod = [np.concatenate([res.results[l * 4 + p]["mod"].T.reshape(-1) for p in range(4)]) for l in range(2)]

    x = np.ascontiguousarray(inp["x"][0])
    vfirst = None
    for l in range(2):
        in_maps = []
        for c in cores:
            m_ = p1_host_inputs(l, inp, mod, c, T)
            m_["x"] = x
            if l == 1:
                m_["vfirst"] = vfirst[c]
            in_maps.append(m_)
        r1 = run_bass_kernel_spmd(_prog(("p1", l), lambda: build_p1(l, T)), in_maps, core_ids=cores).results
        if l == 0:
            vfirst = [np.ascontiguousarray(r1[c]["vT"]) for c in cores]
        yaT = np.concatenate([r1[c]["yaT"] for c in cores], axis=0)
        obT = np.concatenate([r1[c]["obT"] for c in cores], axis=0)
        ocT = np.concatenate([r1[c]["ocT"] for c in cores], axis=0)
        ssb = np.stack([r1[c]["ssb"].T.reshape(-1) for c in cores], axis=0)
        ssc = np.stack([r1[c]["ssc"].T.reshape(-1) for c in cores], axis=0)
        common = p2_host_inputs(l, inp, mod, l == 1)
        in_maps = []
        for c in cores:
            ts_ = slice(c * NT2, (c + 1) * NT2)
            m_ = dict(common)
            m_["x"] = np.ascontiguousarray(x[ts_])
            m_["yaT"] = np.ascontiguousarray(yaT[:, ts_])
            m_["obT"] = np.ascontiguousarray(obT[:, ts_])
            m_["ocT"] = np.ascontiguousarray(ocT[:, ts_])
            m_["ssb"] = np.ascontiguousarray(ssb[:, ts_])
            m_["ssc"] = np.ascontiguousarray(ssc[:, ts_])
            in_maps.append(m_)
        p2 = _prog(("p2", l == 1), lambda: build_p2(l == 1))
        r2 = []
        r2 += run_bass_kernel_spmd(p2, in_maps, core_ids=cores).results
        x = np.ascontiguousarray(np.concatenate([r2[c]["xout"] for c in cores], axis=0))
    return x[None].astype(np.float32)
```

```python
import math
CUT, PJ, PX = 99, 5, 1
P2CUT = 99
from contextlib import ExitStack
import numpy as np
import concourse.bass as bass
import concourse.mybir as mybir
from concourse.bass_utils import run_bass_kernel_spmd

F32 = mybir.dt.float32
BF16 = mybir.dt.bfloat16
AF = mybir.ActivationFunctionType
ALU = mybir.AluOpType
AX = mybir.AxisListType

D = 2048
SEQ = 16384
NCORES = 8
C0 = math.exp(-0.5)
EPS = 1e-6


class V:
    __slots__ = ("ap", "keys")

    def __init__(self, ap, keys):
        self.ap = ap
        self.keys = tuple(keys)

    def __getitem__(self, idx):
        return V(self.ap[idx], self.keys)

    def k(self, *keys):
        return V(self.ap, keys)


class KB:
    def __init__(self, nc, es):
        self.nc = nc
        self.es = es
        self.E = {"pe": nc.tensor, "act": nc.scalar, "dve": nc.vector, "pool": nc.gpsimd, "sp": nc.sync}
        self.sem = {}
        self.cnt = {}
        for e in ("pe", "act", "dve", "pool"):
            self.sem[e] = es.enter_context(nc.semaphore("s_" + e))
            self.cnt[e] = 0
        self.waited = {e: {} for e in self.E}
        self.buf = {}
        self.dsem = {}
        self.dcnt = {}
        self.dq = {}
        self.main_es = es
        self.cap = None

    def sb(self, name, shape, dt=F32):
        t = self.es.enter_context(self.nc.sbuf_tensor("sb_" + name, list(shape), dt))
        return V(t[:], (name,))

    def ps(self, name, shape, dt=F32):
        t = self.es.enter_context(self.nc.psum_tensor("ps_" + name, list(shape), dt))
        return V(t[:], (name,))

    def _deps(self, eng, outs, ins):
        toks = set()
        for v in ins:
            for key in v.keys:
                b = self.buf.get(key)
                if b is not None and b[0] is not None:
                    toks.add(b[0])
        for v in outs:
            for key in v.keys:
                b = self.buf.get(key)
                if b is not None:
                    if b[0] is not None:
                        toks.add(b[0])
                    toks.update(b[1].values())
        w = self.waited[eng]
        for (sname, val, owner) in sorted(toks):
            if owner == "pe" and eng == "pe":
                continue
            if w.get(sname, 0) < val:
                sem = self.sem[sname] if sname in self.sem else self.dsem[sname]
                self.E[eng].wait_ge(sem, val)
                w[sname] = val

    def _record(self, tok, outs, ins):
        for v in ins:
            for key in v.keys:
                b = self.buf.setdefault(key, [None, {}])
                b[1][tok[0]] = tok
        for v in outs:
            for key in v.keys:
                self.buf[key] = [tok, {}]

    def op(self, eng, fn, outs, ins):
        if self.cap is not None:
            self.cap.append(("op", eng, fn, outs, ins))
            return
        self._deps(eng, outs, ins)
        ins_obj = fn()
        self.cnt[eng] += 1
        ins_obj.then_inc(self.sem[eng], 1)
        tok = (eng, self.cnt[eng], eng)
        self._record(tok, outs, ins)

    NDSEM = 6

    def dma(self, q, out, in_, stream=None):
        if self.cap is not None:
            self.cap.append(("dma", q, out, in_))
            return
        n = self.dq.get(q, 0)
        self.dq[q] = n + 1
        stream = f"{q}{n % self.NDSEM}"
        if stream not in self.dsem:
            self.dsem[stream] = self.main_es.enter_context(self.nc.semaphore("d_" + stream))
            self.dcnt[stream] = 0
        self._deps(q, [out], [in_])
        w = self.waited[q]
        if w.get(stream, 0) < self.dcnt[stream]:
            self.E[q].wait_ge(self.dsem[stream], self.dcnt[stream])
            w[stream] = self.dcnt[stream]
        self.dcnt[stream] += 16
        self.E[q].dma_start(out=out.ap, in_=in_.ap).then_inc(self.dsem[stream], 16)
        tok = (stream, self.dcnt[stream], "dma")
        self._record(tok, [out], [in_])

    def wait_all(self, eng, vs):
        self._deps(eng, vs, vs)

    def mm(self, out, lhsT, rhs, start=True, stop=True):
        self.op("pe", lambda: self.nc.tensor.matmul(out.ap, lhsT=lhsT.ap, rhs=rhs.ap, start=start, stop=stop),
                [out], [lhsT, rhs])

    def tr(self, out, in_, ident):
        self.op("pe", lambda: self.nc.tensor.transpose(out.ap, in_.ap, ident.ap), [out], [in_, ident])

    def act(self, out, in_, func, bias=None, scale=None, accum=None):
        ins = [in_]
        kw = {}
        if bias is not None:
            if isinstance(bias, V):
                ins.append(bias)
                kw["bias"] = bias.ap
            else:
                kw["bias"] = float(bias)
        if scale is not None:
            if isinstance(scale, V):
                ins.append(scale)
                kw["scale"] = scale.ap
            else:
                kw["scale"] = float(scale)
        outs = [out]
        if accum is not None:
            outs.append(accum)
            kw["accum_out"] = accum.ap
        self.op("act", lambda: self.nc.scalar.activation(out=out.ap, in_=in_.ap, func=func, **kw), outs, ins)

    def ts(self, eng, out, in0, s1, s2=None, op0=ALU.mult, op1=None):
        ins = [in0]
        a1 = s1.ap if isinstance(s1, V) else float(s1)
        if isinstance(s1, V):
            ins.append(s1)
        kw = {}
        if op1 is not None:
            a2 = s2.ap if isinstance(s2, V) else float(s2)
            if isinstance(s2, V):
                ins.append(s2)
            kw["op1"] = op1
        else:
            a2 = None
        self.op(eng, lambda: self.E[eng].tensor_scalar(out=out.ap, in0=in0.ap, scalar1=a1, scalar2=a2, op0=op0, **kw),
                [out], ins)

    def tt(self, eng, out, in0, in1, op):
        self.op(eng, lambda: self.E[eng].tensor_tensor(out=out.ap, in0=in0.ap, in1=in1.ap, op=op), [out], [in0, in1])

    def stt(self, out, in0, scalar, in1, op0, op1):
        ins = [in0, in1]
        a = scalar.ap if isinstance(scalar, V) else float(scalar)
        if isinstance(scalar, V):
            ins.append(scalar)
        self.op("dve", lambda: self.nc.vector.scalar_tensor_tensor(out=out.ap, in0=in0.ap, scalar=a, in1=in1.ap,
                                                                   op0=op0, op1=op1), [out], ins)

    def cp(self, eng, out, in_):
        if eng == "act":
            self.act(out, in_, AF.Copy)
        else:
            self.op(eng, lambda: self.E[eng].tensor_copy(out=out.ap, in_=in_.ap), [out], [in_])

    def scan(self, out, d0, d1, init, op0, op1):
        self.op("dve", lambda: self.nc.vector.tensor_tensor_scan(out=out.ap, data0=d0.ap, data1=d1.ap, initial=init,
                                                                 op0=op0, op1=op1), [out], [d0, d1])

    def memset(self, eng, out, val):
        self.op(eng, lambda: self.E[eng].memset(out.ap, val), [out], [])

    def recip(self, out, in_):
        self.op("dve", lambda: self.nc.vector.reciprocal(out=out.ap, in_=in_.ap), [out], [in_])

    def replay(self, lists):
        self.cap = None
        pos = [0] * len(lists)
        tot = sum(len(l) for l in lists)
        for _ in range(tot):
            best, bi = None, -1
            for i, l in enumerate(lists):
                if pos[i] < len(l):
                    f = pos[i] / len(l)
                    if best is None or f < best:
                        best, bi = f, i
            it = lists[bi][pos[bi]]
            pos[bi] += 1
            if it[0] == "op":
                self.op(it[1], it[2], it[3], it[4])
            else:
                self.dma(it[1], it[2], it[3])

    def barrier(self):
        for eng in self.E:
            w = self.waited[eng]
            for e2, sem in self.sem.items():
                if e2 != eng and self.cnt[e2] > w.get(e2, 0):
                    self.E[eng].wait_ge(sem, self.cnt[e2])
                    w[e2] = self.cnt[e2]
            for st_, sem in self.dsem.items():
                if self.dcnt[st_] > w.get(st_, 0):
                    self.E[eng].wait_ge(sem, self.dcnt[st_])
                    w[st_] = self.dcnt[st_]

    def finish(self, outs):
        self._deps("sp", outs, outs)


def dram_in(nc, name, shape, dt=F32):
    return V(nc.dram_tensor(name, list(shape), dt, kind="ExternalInput").ap(), ("dram_" + name,))


def dram_out(nc, name, shape, dt=F32):
    return V(nc.dram_tensor(name, list(shape), dt, kind="ExternalOutput").ap(), ("dram_" + name,))


TB = 256
PC_N = 96
PC_G, PC_SC, PC_SH = 0, 16, 32
PC_MU = 48
PC_W0, PC_A0, PC_KK, PC_KA, PC_RK, PC_LNW, PC_LNB, PC_V0 = 54, 55, 56, 57, 58, 59, 60, 61
PC_CQ, PC_CK, PC_CBQ, PC_CBK = 62, 66, 70, 71
PC_IB, PC_FB = 72, 73
PC_LB0, PC_LB1 = 74, 75
PC_MUV = 76
PM_W2, PM_A2, PM_G2, PM_V2, PM_V1 = 0, 64, 128, 192, 256
PM_ID, PM_ONES, PM_M5, PM_MM, PM_TRI, PM_MH, PM_RST = 384, 512, 640, 960, 1088, 1216, 1280
PM_RSTH = PM_RST + TB
PM_N = PM_RSTH + TB
LH = 32


def p1_cols(layer):
    cols = [("r", 64), ("k", 64), ("v", 64), ("xw", 64), ("xa", 64), ("xg", 128),
            ("mq", 128), ("mk", 128), ("mv", 128), ("mif", 2),
            ("hq", 128), ("hf", 128), ("hi", 64)]
    if layer == 1:
        cols += [("va0", 128), ("va1", 128), ("va2", 128), ("va3", 128)]
    return cols


def build_p1(layer, T, stage=99):
    nc = bass.Bass("TRN2", target_bir_lowering=False)
    cols = p1_cols(layer)
    NC1 = sum(m for _, m in cols)
    coff = {}
    o = 0
    for n, m in cols:
        coff[n] = (o, m)
        o += m
    NB = T // TB
    NCH = TB // 64
    NCM = TB // 128
    NCHH = TB // LH
    x_d = dram_in(nc, "x", [T, D])
    w1_d = dram_in(nc, "w1", [D, NC1])
    pc_d = dram_in(nc, "pc", [128, PC_N])
    pm_d = dram_in(nc, "pm", [128, PM_N])
    if layer == 1:
        vf_d = dram_in(nc, "vfirst", [64, T])
    ya_d = dram_out(nc, "yaT", [64, T])
    if layer == 0:
        vo_d = dram_out(nc, "vT", [64, T])
    ob_d = dram_out(nc, "obT", [128, T])
    oc_d = dram_out(nc, "ocT", [64, T])
    ssb_d = dram_out(nc, "ssb", [128, T // 128])
    ssc_d = dram_out(nc, "ssc", [LH, T // LH])

    with ExitStack() as es:
        kb = KB(nc, es)
        sb, ps = kb.sb, kb.ps
        pc = sb("pc", [128, PC_N])
        pm = sb("pm", [128, PM_N])
        Wb = sb("Wb", [128, 16, NC1], BF16)
        kb.dma("sp", pc, pc_d, "pc")
        kb.dma("sp", pm, pm_d, "pm")
        w1v = V(w1_d.ap.rearrange("(kc p) n -> p kc n", p=128), w1_d.keys)
        for kc in range(16):
            kb.dma("pool", Wb[:, kc, :], w1v[:, kc, :], "w1")
        ident = pm[:, PM_ID:PM_ID + 128]
        ones = pm[:, PM_ONES:PM_ONES + 128]
        col = lambda j, n=128: pc[0:n, j:j + 1]

        Acol = sb("Acol", [128, 16])
        kb.stt(Acol, pc[:, PC_SC:PC_SC + 16], 1.0, pc[:, PC_G:PC_G + 16], ALU.add, ALU.mult)
        Bcol = pc[:, PC_SH:PC_SH + 16]
        omka = sb("omka", [64, 1])
        kb.ts("dve", omka, col(PC_KA, 64), -1.0, 1.0, ALU.mult, ALU.add)
        ib15 = sb("ib15", [128, 1])
        kb.ts("dve", ib15, col(PC_IB), 1.0 / 15.0)
        fb15 = sb("fb15", [128, 1])
        kb.ts("dve", fb15, col(PC_FB), 1.0 / 15.0)
        lb = sb("lb", [128, 1])
        oml = sb("oml", [128, 1])
        if layer == 0:
            kb.memset("dve", lb, 0.0)
        else:
            dlb = sb("dlb", [128, 1])
            kb.tt("dve", dlb, col(PC_LB1), col(PC_LB0), ALU.subtract)
            kb.act(lb, dlb, AF.Sigmoid)
        kb.ts("dve", oml, lb, -1.0, 1.0, ALU.mult, ALU.add)

        xs = [sb(f"xs{i}", [128, TB // 128, D]) for i in range(2)]
        junk = sb("junk", [128, D], BF16)
        ss = sb("ss", [128, 4])
        rstd = sb("rstd", [128, 4])
        hT = [sb("hT0", [128, 16, TB], BF16)] * 2
        psT = [ps(f"psT{i}", [128, 512]) for i in range(2)]
        psP = [ps(f"psP{i}", [128, 512]) for i in range(2)]
        psR = ps("psR", [128, 512])
        psR2 = ps("psR2", [128, 512])
        psM = ps("psM", [128, 512])
        psH = ps("psH", [128, 512])
        qa = psR[0:64, 0:320].k("psR")
        qb = psR[0:64, 320:384].k("psR")
        qc = psR[0:64, 384:448].k("psR")
        qd = psR[0:64, 448:512].k("psR")
        qblk0 = psR[0:64, 0:TB].k("psR")
        qblk1 = psR[0:64, 256:256 + TB].k("psR")
        sa = psR2[0:64, 0:128].k("psR2")
        sbx = psR2[0:64, 128:192].k("psR2")
        sc_ = psR2[0:64, 192:256].k("psR2")
        sd = psR2[0:64, 256:448].k("psR2")
        se = psR2[0:64, 448:512].k("psR2")
        sblk0 = psR2[0:64, 0:TB].k("psR2")
        sblk1 = psR2[0:64, 256:256 + TB].k("psR2")
        mA = psM[:, 0:129].k("psM")
        mG = psM[:, 132:136].k("psM")
        hS = psM[:, 136:200].k("psM")
        hO = psM[0:64, 200:200 + LH].k("psM")
        hA = psM[0:LH, 264:264 + LH].k("psM")
        hT2 = psH[0:LH, 0:192].k("psH")
        ho = psH[0:LH, 192:256].k("psH")
        mB = psT[0][:, 256:384].k("psT0")
        mC = psT[1][:, 256:384].k("psT1")
        mD = psP[0][:, 256:385].k("psP0")
        mE = psP[1][:, 256:384].k("psP1")

        HAL = {"r": 1, "k": 1, "v": 1, "xw": 1, "xa": 1, "xg": 1, "mq": 3, "mk": 3,
               "va0": 1, "va1": 1, "va2": 1, "va3": 1}
        raw = {}
        for n, m in cols:
            h = HAL.get(n, 0)
            raw[n] = sb("raw_" + n, [m, h + TB])
            if h:
                kb.memset("pool", raw[n][:, 0:h], 0.0)

        def newt(name, m=64, w=TB):
            return sb(name, [m, w])

        tmp64 = newt("tmp64")
        tmp128 = newt("tmp128", 128)
        r_, k0, v_, xw, xa = [newt("rw_" + n) for n in ("r", "k0", "v", "xw", "xa")]
        xg = newt("rw_xg", 128)
        sg, cumS, p_, pinv, pprev, dcl = [newt("rw_" + n) for n in ("sg", "cumS", "p", "pinv", "pprev", "dcl")]
        txw = xw
        cumP = xa
        pLp = dcl
        pL = newt("rw_pL", 64, NCH)
        icl, kk, rn, t1, bt_, Bp = [newt("rw_" + n) for n in ("icl", "kk", "rn", "t1", "bt", "Bp")]
        kk2 = tmp64
        kkn = kk
        kmod = t1
        bvec = icl
        at_ = pprev
        kt_ = pinv
        rt_ = p_
        Kp = dcl
        sxg = xg
        rk = tmp64
        gate, bonus, ynT = [newt("rw_" + n) for n in ("gate", "bonus", "ynT")]
        yf = ynT
        if layer == 1:
            vall = sb("rw_vall", [128, 4, TB])
            vv1 = newt("rw_vv1", 32)
            vg, vfb, dv = [newt("rw_" + n) for n in ("vg", "vfb", "dv")]
        A5 = sb("rw_A5", [64, 320])
        PT = [sb(f"rw_PT{j}", [64, 128]) for j in range(5)]
        XT = [sb(f"rw_XT{j}", [64, 64]) for j in range(2)]
        tok3 = sb("rw_tok3", [64, 192])
        M0 = sb("rw_M0", [64, 64])
        U_ = sb("rw_U", [64, 64])
        Hst = sb("rw_H", [64, 64])
        kb.memset("pool", Hst, 0.0)
        Ysb = sb("rw_Y", [64, 64])
        ysq = sb("rw_ysq", [64, 64])
        st = sb("rw_st", [64, 8])
        yn = sb("rw_yn", [64, 64])

        mq, mk = [newt("ml_" + n, 128) for n in ("q", "k")]
        mq_acc, mk_acc = mq, mk
        gif = sb("ml_gif", [128, 2])
        mg = sb("ml_g", [128, 12])
        S0T = sb("ml_S0T", [128, 128])
        Vp = sb("ml_Vp", [128, 132])
        kb.memset("pool", Vp, 1.0)
        ktg = sb("ml_ktg", [128, 128])
        numE = sb("ml_numE", [128, 132])
        mh = sb("ml_h", [128, 128])
        mjunk = sb("ml_junk", [128, 128])
        Cst = sb("ml_C", [128, 132])
        kb.memset("pool", Cst, 0.0)
        obuf = newt("ml_obuf", 128)
        ssb_all = sb("ssb_all", [128, T // 128])
        kb.memset("pool", ssb_all, 0.0)

        hsf, hkraw, hcum, hp, hpinv, hdcl, hqs = [newt("hg_" + n, 128) for n in (
            "sf", "kraw", "cum", "p", "pinv", "dcl", "qs")]
        hf_ = hsf
        hlogf = hsf
        hpLp = hdcl
        hqt = hqs
        hkt = hpinv
        hkp = hdcl
        hpL = newt("hg_pL", 128, NCHH)
        attT = sb("hg_attT", [LH, LH])
        tok2 = sb("hg_tok2", [LH, 192])
        Sst = sb("hg_S", [128, 64])
        kb.memset("pool", Sst, 0.0)
        osb = sb("hg_o", [LH, 64])
        hjunk = sb("hg_junk", [LH, 64])
        ocbuf = newt("hg_ocbuf", 64)
        ssc_all = sb("ssc_all", [LH, T // LH])
        kb.memset("pool", ssc_all, 0.0)

        m5 = pm[0:64, PM_M5:PM_M5 + 320]
        maskM = pm[:, PM_MM:PM_MM + 128]
        triM = pm[:, PM_TRI:PM_TRI + 128]
        maskH = pm[0:LH, PM_MH:PM_MH + LH]
        rstH = pm[:, PM_RSTH:PM_RSTH + TB]
        idLH = pm[0:LH, PM_ID:PM_ID + LH]
        rst64 = pm[0:64, PM_RST:PM_RST + TB]
        rst128 = pm[:, PM_RST:PM_RST + TB]
        id64 = pm[0:64, PM_ID:PM_ID + 64]
        ones64 = pm[0:64, PM_ONES:PM_ONES + 64]

        evac_i = [0]

        def evac(out, in_):
            e = ("act", "dve")[evac_i[0] % 2]
            evac_i[0] += 1
            kb.cp(e, out, in_)

        def shift(out, rawt, mucol, m, eng="pool"):
            t = tmp64 if m == 64 else tmp128
            kb.tt(eng, t, rawt[:, 0:TB], rawt[:, 1:1 + TB], ALU.subtract)
            kb.stt(out, t, mucol, rawt[:, 1:1 + TB], ALU.mult, ALU.add)

        for b in range(NB):
            t0 = b * TB
            sl = b % 2
            X = xs[sl]
            H = hT[sl]
            if b == 0:
                for tt in range(TB // 128):
                    kb.dma("sp", X[:, tt, :], x_d[tt * 128:(tt + 1) * 128, :], f"x{sl}{tt}")
            if b + 1 < NB:
                for tt in range(TB // 128):
                    kb.dma("sp", xs[1 - sl][:, tt, :], x_d[t0 + TB + tt * 128:t0 + TB + (tt + 1) * 128, :],
                           f"x{1 - sl}{tt}")
            for tt in range(TB // 128):
                kb.act(junk, X[:, tt, :], AF.Square, accum=ss[:, tt:tt + 1])
                kb.act(rstd[:, tt:tt + 1], ss[:, tt:tt + 1], AF.Sqrt, bias=EPS, scale=1.0 / D)
                kb.recip(rstd[:, tt:tt + 1], rstd[:, tt:tt + 1])
                kb.ts("pool", X[:, tt, :], X[:, tt, :], rstd[:, tt:tt + 1])
            for kc in range(16):
                pT = psT[kc % 2][:, 0:TB].k(f"psT{kc % 2}")
                for tt in range(TB // 128):
                    kb.tr(pT[:, tt * 128:(tt + 1) * 128], X[:, tt, kc * 128:(kc + 1) * 128], ident)
                if kc % 2 == 0:
                    kb.act(H[:, kc, :], pT[:, 0:TB], AF.Identity, bias=Bcol[:, kc:kc + 1], scale=Acol[:, kc:kc + 1])
                else:
                    kb.ts("dve", H[:, kc, :], pT[:, 0:TB], Acol[:, kc:kc + 1], Bcol[:, kc:kc + 1], ALU.mult, ALU.add)
            for ci, (n, m) in enumerate(cols):
                off = coff[n][0]
                pp = psP[ci % 2][:, 0:TB].k(f"psP{ci % 2}")
                for kc in range(16):
                    kb.mm(pp[0:m, 0:TB], Wb[:, kc, off:off + m], H[:, kc, :], start=(kc == 0), stop=(kc == 15))
                h = HAL.get(n, 0)
                evac(raw[n][:, h:h + TB], pp[0:m, 0:TB])

            if stage < 1:
                continue
            shift(r_, raw["r"], col(PC_MU + 0, 64), 64)
            shift(k0, raw["k"], col(PC_MU + 1, 64), 64)
            shift(v_, raw["v"], col(PC_MU + 2, 64), 64)
            shift(xw, raw["xw"], col(PC_MU + 3, 64), 64)
            shift(xa, raw["xa"], col(PC_MU + 4, 64), 64)
            shift(xg, raw["xg"], col(PC_MU + 5, 128), 128)
            kb.act(txw, xw, AF.Tanh)
            kb.mm(qblk0, pm[0:64, PM_W2:PM_W2 + 64], txw)
            kb.act(sg, qblk0, AF.Sigmoid, bias=col(PC_W0, 64))
            kb.scan(cumS, rst64, sg, 0.0, ALU.mult, ALU.add)
            kb.mm(qblk1, pm[0:64, PM_A2:PM_A2 + 64], xa)
            kb.act(icl, qblk1, AF.Sigmoid, bias=col(PC_A0, 64))
            kb.tt("pool", cumP, cumS, sg, ALU.subtract)
            kb.act(p_, cumS, AF.Exp, scale=-C0)
            kb.act(pinv, cumS, AF.Exp, scale=C0)
            kb.act(pprev, cumP, AF.Exp, scale=-C0)
            cum3 = V(cumS.ap.rearrange("p (c l) -> p c l", l=64), cumS.keys)
            dcl3 = V(dcl.ap.rearrange("p (c l) -> p c l", l=64), dcl.keys)
            lastb = V(cum3.ap[:, :, 63:64].to_broadcast([64, NCH, 64]), cumS.keys)
            kb.tt("dve", dcl3, lastb, cum3, ALU.subtract)
            kb.act(pLp, dcl, AF.Exp, scale=-C0)
            kb.act(pL, V(cum3.ap[:, :, 63], cumS.keys), AF.Exp, scale=-C0)
            kb.ts("pool", kk, k0, col(PC_KK, 64))
            kb.tt("pool", kk2, kk, kk, ALU.mult)
            kb.mm(sblk0, ones64, kk2)
            kb.ts("dve", rn, sblk0, 1e-24, None, ALU.max)
            kb.act(rn, rn, AF.Sqrt)
            kb.recip(rn, rn)
            kb.tt("pool", kkn, kk, rn, ALU.mult)
            kb.ts("dve", t1, icl, col(PC_KA, 64), omka, ALU.mult, ALU.add)
            kb.tt("pool", kmod, k0, t1, ALU.mult)
            kb.tt("pool", bvec, kkn, icl, ALU.mult)
            kb.stt(at_, kkn, -1.0, pprev, ALU.mult, ALU.mult)
            kb.tt("pool", bt_, bvec, pinv, ALU.mult)
            kb.tt("dve", kt_, kmod, pinv, ALU.mult)
            kb.tt("pool", rt_, r_, p_, ALU.mult)
            kb.tt("dve", Bp, bvec, pLp, ALU.mult)
            kb.tt("pool", Kp, kmod, pLp, ALU.mult)
            if layer == 1:
                for i in range(4):
                    kb.tt("pool", tmp128, raw[f"va{i}"][:, 0:TB], raw[f"va{i}"][:, 1:1 + TB], ALU.subtract)
                    kb.stt(vall[:, i, :], tmp128, pc[:, PC_MUV + i:PC_MUV + i + 1], raw[f"va{i}"][:, 1:1 + TB],
                           ALU.mult, ALU.add)
                for i in range(4):
                    kb.mm(sblk1[0:32, :], pm[:, PM_V1 + 32 * i:PM_V1 + 32 * (i + 1)], vall[:, i, :],
                          start=(i == 0), stop=(i == 3))
                kb.cp("act", vv1, sblk1[0:32, :])
                kb.mm(sblk1, pm[0:32, PM_V2:PM_V2 + 64], vv1)
                kb.act(vg, sblk1, AF.Sigmoid, bias=col(PC_V0, 64))
                kb.dma("sp", vfb, vf_d[:, t0:t0 + TB], "vf")
                kb.tt("pool", dv, vfb, v_, ALU.subtract)
                kb.tt("pool", dv, dv, vg, ALU.mult)
                kb.tt("pool", v_, v_, dv, ALU.add)
            else:
                kb.dma("sp", vo_d[:, t0:t0 + TB], v_, "vo")
            kb.act(sxg, xg, AF.Sigmoid)
            kb.mm(sblk0, pm[:, PM_G2:PM_G2 + 64], sxg)
            kb.cp("act", gate, sblk0)
            kb.stt(rk, r_, col(PC_RK, 64), kmod, ALU.mult, ALU.mult)
            kb.mm(sblk1, ones64, rk)
            kb.tt("dve", bonus, sblk1, v_, ALU.mult)

            capA, capB, capC = [], [], []
            kb.cap = capA
            for c in range(NCH if stage >= 2 else 0):
                cs = slice(c * 64, (c + 1) * 64)
                P5 = qa
                kb.mm(qa[:, 0:64], bt_[:, cs], at_[:, cs])
                kb.mm(qa[:, 64:128], at_[:, cs], bt_[:, cs])
                kb.mm(qa[:, 128:192], kt_[:, cs], at_[:, cs])
                kb.mm(qa[:, 192:256], bt_[:, cs], rt_[:, cs])
                kb.mm(qa[:, 256:320], kt_[:, cs], rt_[:, cs])
                kb.tt("dve", A5, P5, m5, ALU.mult)
                if CUT < 1:
                    continue
                Tj, Pj = A5[:, 0:64], A5[:, 64:128]
                kb.tt("pool", XT[0], Tj, id64, ALU.add)
                xcur = 0
                for j in range(PJ):
                    kb.mm(sa[:, 0:64], Pj, Tj)
                    kb.mm(sa[:, 64:128], Tj, Pj)
                    evac(PT[j], sa)
                    Tj, Pj = PT[j][:, 0:64], PT[j][:, 64:128]
                    if not PX:
                        continue
                    kb.mm(sbx, Pj, XT[xcur])
                    kb.tt("dve", XT[1 - xcur], XT[xcur], sbx, ALU.add)
                    xcur = 1 - xcur
                XTf = XT[xcur]
                if CUT < 2:
                    continue
                kb.tr(sd[:, 0:64], v_[:, cs], id64)
                kb.tr(sd[:, 64:128], Bp[:, cs], id64)
                kb.tr(sd[:, 128:192], Kp[:, cs], id64)
                evac(tok3, sd)
                Vt, Bt, Kt = tok3[:, 0:64], tok3[:, 64:128], tok3[:, 128:192]
                if CUT < 3:
                    continue
                kb.mm(qb, at_[:, cs], Hst, start=True, stop=False)
                kb.mm(qb, A5[:, 128:192], Vt, start=False, stop=True)
                evac(M0, qb)
                kb.mm(qc, XTf, M0)
                evac(U_, qc)
                kb.mm(qd, rt_[:, cs], Hst, start=True, stop=False)
                kb.mm(qd, A5[:, 192:256], U_, start=False, stop=False)
                kb.mm(qd, A5[:, 256:320], Vt, start=False, stop=True)
                kb.mm(sc_, Bt, U_, start=True, stop=False)
                kb.mm(sc_, Kt, Vt, start=False, stop=True)
                kb.stt(Hst, Hst, pL[:, c:c + 1], sc_, ALU.mult, ALU.add)
                if CUT < 4:
                    continue
                kb.act(Ysb, qd, AF.Identity, accum=st[:, 0:1])
                kb.act(ysq, Ysb, AF.Square, accum=st[:, 1:2])
                kb.ts("dve", st[:, 2:3], st[:, 0:1], 1.0 / 64.0)
                kb.tt("dve", st[:, 3:4], st[:, 2:3], st[:, 2:3], ALU.mult)
                kb.stt(st[:, 4:5], st[:, 1:2], 1.0 / 64.0, st[:, 3:4], ALU.mult, ALU.subtract)
                kb.act(st[:, 5:6], st[:, 4:5], AF.Sqrt, bias=64e-5)
                kb.recip(st[:, 5:6], st[:, 5:6])
                kb.ts("dve", yn, Ysb, st[:, 2:3], st[:, 5:6], ALU.subtract, ALU.mult)
                kb.tr(se, yn, id64)
                evac(ynT[:, cs], se)
            kb.ts("dve", yf, ynT, col(PC_LNW, 64), col(PC_LNB, 64), ALU.mult, ALU.add)
            kb.tt("pool", yf, yf, bonus, ALU.add)
            kb.tt("pool", yf, yf, gate, ALU.mult)
            kb.dma("sp", ya_d[:, t0:t0 + TB], yf, "ya")

            kb.cap = capB
            for (acc, rw, cw, cb, outq) in ((mq_acc, raw["mq"], PC_CQ, PC_CBQ, mq), (mk_acc, raw["mk"], PC_CK, PC_CBK, mk)):
                kb.ts("dve", acc, rw[:, 0:TB], col(cw), col(cb), ALU.mult, ALU.add)
                for j in range(1, 4):
                    kb.stt(acc, rw[:, j:j + TB], col(cw + j), acc, ALU.mult, ALU.add)
                kb.act(outq, acc, AF.Silu)
            for c in range(NCM):
                cs = slice(c * 128, (c + 1) * 128)
                gi = b * NCM + c
                kb.tr(mG[:, 0:2], raw["mif"][:, cs], pm[0:2, PM_ID:PM_ID + 2])
                kb.cp("dve", gif, mG[:, 0:2])
                kb.act(mg[:, 0:1], gif[:, 0:1], AF.Tanh, bias=ib15, scale=1.0 / 15.0)
                kb.act(mg[:, 1:2], gif[:, 1:2], AF.Tanh, bias=fb15, scale=1.0 / 15.0)
                kb.act(mg[:, 2:3], mg[:, 1:2], AF.Exp, scale=-15.0)
                kb.act(mg[:, 3:4], mg[:, 2:3], AF.Ln, bias=1.0)
                kb.mm(mG[:, 2:3], triM, mg[:, 3:4])
                kb.mm(mG[:, 3:4], ones, mg[:, 3:4])
                kb.cp("dve", mg[:, 10:12], mG[:, 2:4])
                kb.act(mg[:, 4:5], mg[:, 10:11], AF.Exp, scale=-1.0)
                kb.stt(mg[:, 5:6], mg[:, 0:1], 15.0, mg[:, 10:11], ALU.mult, ALU.add)
                kb.act(mg[:, 6:7], mg[:, 5:6], AF.Exp, bias=math.log(128.0 ** -0.5))
                kb.act(mg[:, 7:8], mg[:, 11:12], AF.Exp, scale=-1.0)
                kb.mm(mA[:, 0:128], mk[:, cs], mq[:, cs])
                kb.stt(S0T, mA[:, 0:128], mg[:, 6:7], maskM, ALU.mult, ALU.mult)
                kb.tr(mB, raw["mv"][:, cs], ident)
                kb.cp("act", Vp[:, 0:128], mB)
                kb.tr(mC, mk[:, cs], ident)
                kb.ts("dve", ktg, mC, mg[:, 6:7])
                kb.mm(mD, S0T, Vp[:, 0:129], start=True, stop=False)
                kb.mm(mD, mq[:, cs], Cst[:, 0:129], start=False, stop=True)
                kb.ts("dve", numE[:, 0:129], mD, mg[:, 4:5])
                kb.act(mg[:, 8:9], numE[:, 128:129], AF.Abs)
                kb.ts("dve", mg[:, 8:9], mg[:, 8:9], 1.0, None, ALU.max)
                kb.recip(mg[:, 9:10], mg[:, 8:9])
                kb.ts("dve", mh, numE[:, 0:128], mg[:, 9:10])
                kb.act(mjunk, mh, AF.Square, accum=ssb_all[:, gi:gi + 1])
                kb.tr(mE, mh, ident)
                evac(obuf[:, cs], mE)
                kb.mm(mA, ktg, Vp[:, 0:129])
                kb.ts("dve", Cst[:, 0:129], Cst[:, 0:129], mg[:, 7:8])
                kb.stt(Cst[:, 0:129], mA, mg[:, 7:8], Cst[:, 0:129], ALU.mult, ALU.add)
            kb.dma("sp", ob_d[:, t0:t0 + TB], obuf, "ob")

            kb.cap = capC
            kb.act(hsf, raw["hf"], AF.Sigmoid)
            kb.ts("dve", hf_, hsf, oml, lb, ALU.mult, ALU.add)
            kb.act(hlogf, hf_, AF.Ln)
            kb.act(hkraw, raw["hf"], AF.Sigmoid, scale=-1.0)
            kb.scan(hcum, rstH, hlogf, 0.0, ALU.mult, ALU.add)
            kb.act(hp, hcum, AF.Exp)
            kb.act(hpinv, hcum, AF.Exp, scale=-1.0)
            hc3 = V(hcum.ap.rearrange("p (c l) -> p c l", l=LH), hcum.keys)
            hd3 = V(hdcl.ap.rearrange("p (c l) -> p c l", l=LH), hdcl.keys)
            hlast = V(hc3.ap[:, :, LH - 1:LH].to_broadcast([128, NCHH, LH]), hcum.keys)
            kb.tt("dve", hd3, hlast, hc3, ALU.subtract)
            kb.act(hpLp, hdcl, AF.Exp)
            kb.act(hpL, V(hc3.ap[:, :, LH - 1], hcum.keys), AF.Exp)
            kb.act(hqs, raw["hq"], AF.Silu)
            kb.tt("pool", hqt, hqs, hp, ALU.mult)
            kb.stt(hkt, hkraw, oml, hpinv, ALU.mult, ALU.mult)
            kb.stt(hkp, hkraw, oml, hpLp, ALU.mult, ALU.mult)
            for c in range(NCHH):
                cs = slice(c * LH, (c + 1) * LH)
                gi = b * NCHH + c
                kb.mm(hA, hkt[:, cs], hqt[:, cs])
                kb.tt("dve", attT, hA, maskH, ALU.mult)
                kb.tr(hT2[:, 0:64], raw["hi"][:, cs], id64)
                kb.tr(hT2[:, 64:192], hkp[:, cs], ident)
                evac(tok2, hT2)
                kb.mm(ho, attT, tok2[:, 0:64], start=True, stop=False)
                kb.mm(ho, hqt[:, cs], Sst, start=False, stop=True)
                kb.mm(hS, tok2[:, 64:192], tok2[:, 0:64])
                kb.stt(Sst, Sst, hpL[:, c:c + 1], hS, ALU.mult, ALU.add)
                kb.cp("act", osb, ho)
                kb.act(hjunk, osb, AF.Square, accum=ssc_all[:, gi:gi + 1])
                kb.tr(hO, osb, idLH)
                evac(ocbuf[:, cs], hO)
            kb.dma("sp", oc_d[:, t0:t0 + TB], ocbuf, "oc")

            kb.replay([capA, capB, capC])
            for n, h in HAL.items():
                if n in raw:
                    kb.cp("pool", raw[n][:, 0:h], raw[n][:, TB:TB + h])

        kb.dma("sp", ssb_d, ssb_all, "ssb")
        kb.dma("sp", ssc_d, ssc_all, "ssc")
        outs = [ya_d, ob_d, oc_d, ssb_d, ssc_d] + ([vo_d] if layer == 0 else [])
        kb.finish(outs)
    return nc


def p1_host_inputs(layer, inp, mod, core, T):
    l = layer
    hd = core
    ph, hf = core // 2, core % 2
    w_in = inp["w_in"][l]
    RW = 512
    o_r, o_k, o_v, o_xw, o_xa, o_xg = 0, 512, 1024, 1536, 1600, 1664
    o_rest = 1792
    o_mq, o_mk, o_mv, o_mo, o_mi, o_mf = (o_rest, o_rest + 512, o_rest + 1024, o_rest + 2048, o_rest + 3072, o_rest + 3076)
    o_hq = o_rest + 3080
    o_hf, o_hi, o_hg = o_hq + 512, o_hq + 1024, o_hq + 1536
    h64 = slice(hd * 64, hd * 64 + 64)

    def rng(o, n):
        return list(range(o, o + n))

    idx = (rng(o_r + hd * 64, 64) + rng(o_k + hd * 64, 64) + rng(o_v + hd * 64, 64) + rng(o_xw, 64) + rng(o_xa, 64)
           + rng(o_xg, 128)
           + rng(o_mq + ph * 128, 128) + rng(o_mk + ph * 128, 128) + rng(o_mv + ph * 256 + hf * 128, 128)
           + [o_mi + ph, o_mf + ph]
           + rng(o_hq + ph * 128, 128) + rng(o_hf + ph * 128, 128) + rng(o_hi + ph * 128 + hf * 64, 64))
    if l == 1:
        idx += rng(o_v, 512)
    w1 = np.ascontiguousarray(w_in[:, idx])
    pc = np.zeros((128, PC_N), np.float32)
    pc[:, PC_G:PC_G + 16] = inp["norm_mix"][l].reshape(16, 128).T
    sh_m, sc_m = mod[l][0:D], mod[l][D:2 * D]
    pc[:, PC_SC:PC_SC + 16] = sc_m.reshape(16, 128).T
    pc[:, PC_SH:PC_SH + 16] = sh_m.reshape(16, 128).T
    mu = inp["rw_mu"][l]
    pc[0:64, PC_MU + 0] = mu[o_r + hd * 64:o_r + hd * 64 + 64]
    pc[0:64, PC_MU + 1] = mu[o_k + hd * 64:o_k + hd * 64 + 64]
    pc[0:64, PC_MU + 2] = mu[o_v + hd * 64:o_v + hd * 64 + 64]
    pc[0:64, PC_MU + 3] = mu[o_xw:o_xw + 64]
    pc[0:64, PC_MU + 4] = mu[o_xa:o_xa + 64]
    pc[0:128, PC_MU + 5] = mu[o_xg:o_xg + 128]
    pc[0:64, PC_W0] = inp["rw_w0"][l][h64]
    pc[0:64, PC_A0] = inp["rw_a0"][l][h64]
    pc[0:64, PC_KK] = inp["rw_kk"][l][h64]
    pc[0:64, PC_KA] = inp["rw_ka"][l][h64]
    pc[0:64, PC_RK] = inp["rw_rk"][l][h64]
    pc[0:64, PC_LNW] = inp["rw_lnw"][l][h64]
    pc[0:64, PC_LNB] = inp["rw_lnb"][l][h64]
    if l == 1:
        pc[0:64, PC_V0] = inp["rw_v0"][0][h64]
        pc[:, PC_MUV:PC_MUV + 4] = mu[o_v:o_v + 512].reshape(4, 128).T
    cw = inp["ml_conv_w"][l]
    cb = inp["ml_conv_b"][l]
    for j in range(4):
        pc[:, PC_CQ + j] = cw[j, ph * 128:(ph + 1) * 128]
        pc[:, PC_CK + j] = cw[j, 512 + ph * 128:512 + (ph + 1) * 128]
    pc[:, PC_CBQ] = cb[ph * 128:(ph + 1) * 128]
    pc[:, PC_CBK] = cb[512 + ph * 128:512 + (ph + 1) * 128]
    pc[:, PC_IB] = inp["ml_ib"][l][ph]
    pc[:, PC_FB] = inp["ml_fb"][l][ph]
    pc[:, PC_LB0] = inp["hg_lb"][0][ph * 128:(ph + 1) * 128]
    pc[:, PC_LB1] = inp["hg_lb"][1][ph * 128:(ph + 1) * 128]
    pm = np.zeros((128, PM_N), np.float32)
    pm[0:64, PM_W2:PM_W2 + 64] = inp["rw_w2"][l][:, h64]
    pm[0:64, PM_A2:PM_A2 + 64] = inp["rw_a2"][l][:, h64]
    pm[0:128, PM_G2:PM_G2 + 64] = inp["rw_g2"][l][:, h64]
    if l == 1:
        pm[0:32, PM_V2:PM_V2 + 64] = inp["rw_v2"][0][:, h64]
        pm[:, PM_V1:PM_V1 + 128] = inp["rw_v1"][0].reshape(4, 128, 32).transpose(1, 0, 2).reshape(128, 128)
    pm[:, PM_ID:PM_ID + 128] = np.eye(128, dtype=np.float32)
    pm[:, PM_ONES:PM_ONES + 128] = 1.0
    s_ = np.arange(64)[:, None]
    t_ = np.arange(64)[None, :]
    mu_s = (s_ < t_).astype(np.float32)
    ml_s = (s_ > t_).astype(np.float32)
    mu_i = (s_ <= t_).astype(np.float32)
    pm[0:64, PM_M5:PM_M5 + 320] = np.concatenate([mu_s, ml_s, mu_s, mu_i, mu_i], axis=1)
    s2 = np.arange(128)[:, None]
    t2 = np.arange(128)[None, :]
    pm[:, PM_MM:PM_MM + 128] = (s2 <= t2).astype(np.float32)
    pm[:, PM_TRI:PM_TRI + 128] = (s2 <= t2).astype(np.float32)
    pm[0:LH, PM_MH:PM_MH + LH] = mu_i[0:LH, 0:LH]
    rsth = np.ones((128, TB), np.float32)
    rsth[:, ::LH] = 0.0
    pm[:, PM_RSTH:PM_RSTH + TB] = rsth
    rst = np.ones((128, TB), np.float32)
    rst[:, ::64] = 0.0
    pm[:, PM_RST:PM_RST + TB] = rst
    return {"w1": w1, "pc": pc, "pm": pm}


def build_p0():
    nc = bass.Bass("TRN2", target_bir_lowering=False)
    NCT = 24
    c_d = dram_in(nc, "c", [128, 16])
    w_d = dram_in(nc, "w", [D, NCT * 128])
    b_d = dram_in(nc, "b", [128, NCT])
    o_d = dram_out(nc, "mod", [128, NCT])
    with ExitStack() as es:
        kb = KB(nc, es)
        cc = kb.sb("cc", [128, 16])
        bb = kb.sb("bb", [128, NCT])
        oo = kb.sb("oo", [128, NCT])
        wb = [kb.sb(f"w{i}", [128, 16, 512]) for i in range(2)]
        pp = kb.ps("pp", [128, 512])
        kb.dma("sp", cc, c_d, "c")
        kb.dma("sp", bb, b_d, "b")
        kb.act(cc, cc, AF.Silu)
        wv = V(w_d.ap.rearrange("(kc p) n -> p kc n", p=128), w_d.keys)
        for pc_ in range(NCT // 4):
            W = wb[pc_ % 2]
            kb.dma("sp", W, wv[:, :, pc_ * 512:(pc_ + 1) * 512], f"w{pc_ % 2}")
            for j in range(4):
                ct = pc_ * 4 + j
                for kc in range(16):
                    kb.mm(pp[:, ct:ct + 1], W[:, kc, j * 128:(j + 1) * 128], cc[:, kc:kc + 1],
                          start=(kc == 0), stop=(kc == 15))
        kb.tt("dve", oo, pp[:, 0:NCT], bb, ALU.add)
        kb.dma("sp", o_d, oo, "o")
        kb.finish([o_d])
    return nc


NT2 = 2048
SPA = 512
SPB = 1024
P2C_N = 128
(P2_GM, P2_SCM, P2_SHM, P2_GF, P2_SCF, P2_SHF, P2_MLN, P2_HGN) = (0, 16, 32, 48, 64, 80, 96, 104)
NWG = 7680


def build_p2(final, NT=NT2, nexp=32, a_only=False):
    nc = bass.Bass("TRN2", target_bir_lowering=False)
    x_d = dram_in(nc, "x", [NT, D])
    ya_d = dram_in(nc, "yaT", [512, NT])
    ob_d = dram_in(nc, "obT", [1024, NT])
    oc_d = dram_in(nc, "ocT", [512, NT])
    ssb_d = dram_in(nc, "ssb", [8, NT])
    ssc_d = dram_in(nc, "ssc", [8, NT])
    sel_d = dram_in(nc, "sel", [8, 4 * 128])
    wg_d = dram_in(nc, "wg", [D, NWG])
    pabc_d = dram_in(nc, "pabc", [D, D])
    wo_d = dram_in(nc, "wout", [D, D])
    wr_d = dram_in(nc, "wr", [D, 36])
    rb_d = dram_in(nc, "rb", [128, 36])
    nh = nexp // 2
    eg_ds = [dram_in(nc, f"eg{i}", [nh, D, 1024]) for i in range(2)]
    eu_ds = [dram_in(nc, f"eu{i}", [nh, D, 1024]) for i in range(2)]
    ed_ds = [dram_in(nc, f"ed{i}", [nh, 1024, D]) for i in range(2)]
    pc_d = dram_in(nc, "pc2", [128, P2C_N])
    rowb_d = dram_in(nc, "rowb", [128, 3 * D])
    id_d = dram_in(nc, "ident", [128, 128])
    if a_only:
        xm_d = dram_out(nc, "xmid", [NT, D])
    else:
        xm_d = V(nc.dram_tensor("xmid", [NT, D], F32, kind="Internal").ap(), ("dram_xmid",))
    out_d = dram_out(nc, "xout", [NT, D])

    with ExitStack() as es:
        kb = KB(nc, es)
        sb, ps = kb.sb, kb.ps
        pc = sb("pc2", [128, P2C_N])
        rowb = sb("rowb", [128, D])
        ident = sb("ident", [128, 128])
        sel = sb("sel", [8, 512])
        kb.dma("sp", pc, pc_d, "pc")
        kb.dma("sp", rowb, rowb_d[:, D:2 * D], "rowb")
        kb.dma("sp", ident, id_d, "id")
        kb.dma("sp", sel, sel_d, "sel")
        ones = sb("ones", [128, 128])
        kb.memset("dve", ones, 1.0)
        Am = sb("Am", [128, 16])
        kb.stt(Am, pc[:, P2_SCM:P2_SCM + 16], 1.0, pc[:, P2_GM:P2_GM + 16], ALU.add, ALU.mult)
        Bm = pc[:, P2_SHM:P2_SHM + 16]
        Af = sb("Af", [128, 16])
        kb.stt(Af, pc[:, P2_SCF:P2_SCF + 16], 1.0, pc[:, P2_GF:P2_GF + 16], ALU.add, ALU.mult)
        Bf = pc[:, P2_SHF:P2_SHF + 16]
        gtf_b = rowb[:, 0:D]

        banks = [ps(f"bk{i}", [128, 512]) for i in range(8)]
        bk = lambda i: banks[i]

        RING = 4
        ring = [sb(f"ring{i}", [128, 4096], BF16) for i in range(RING)]
        ring_i = [0]

        def wload(src_view, shape3):
            r = ring[ring_i[0] % RING]
            nm = f"ring{ring_i[0] % RING}"
            ring_i[0] += 1
            a, b_ = shape3
            dst = V(r.ap[:, 0:a * b_].rearrange("p (a b) -> p a b", b=b_), r.keys)
            kb.dma("pool", dst, src_view, nm)
            return dst

        class Stream:
            def __init__(self, look):
                self.items = []
                self.bufs = {}
                self.next = 0
                self.look = look

            def add(self, fn):
                self.items.append(fn)
                return len(self.items) - 1

            def get(self, i):
                while self.next < len(self.items) and self.next <= i + self.look:
                    self.bufs[self.next] = self.items[self.next]()
                    self.next += 1
                return self.bufs.pop(i)

        wgv = V(wg_d.ap.rearrange("(kc p) n -> p kc n", p=128), wg_d.keys)
        pav = V(pabc_d.ap.rearrange("(kc p) n -> p kc n", p=128), pabc_d.keys)
        wov = V(wo_d.ap.rearrange("(kc p) n -> p kc n", p=128), wo_d.keys)

        es_main = kb.es
        es_a = ExitStack()
        kb.es = es_a
        xs = sb("xs", [128, SPA // 128, D])
        gtm_b = sb("gtm_b", [128, D])
        kb.dma("sp", gtm_b, rowb_d[:, 0:D], "gtm")
        xn = [sb(f"xn{i}", [128, D]) for i in range(2)]
        junk = sb("junk", [128, D], BF16)
        ss = sb("ss", [128, 8])
        rstd = sb("rstd", [128, 8])
        hT = sb("hT", [128, 16, SPA], BF16)
        yT = sb("yT", [128, 16, SPA], BF16)
        zT = sb("zT", [128, 16, SPA], BF16)
        ssrow = sb("ssrow", [8, 2, SPA])
        rsb = sb("rsb", [128, SPA])
        otmp = sb("otmp", [128, SPA])
        sgt = sb("sgt", [128, SPA])
        zacc = sb("zacc", [128, SPA])
        ztmp = sb("ztmp", [128, SPA])
        pbuf = [sb(f"pbuf{i}", [128, 16, 128], BF16) for i in range(2)]

        def norm_T(X, ntile, Acol, Bcol, Hout, wtok, b0, rt32=None):
            for tt in range(ntile):
                kb.act(junk, X[:, tt, :], AF.Square, accum=ss[:, tt:tt + 1])
                kb.act(rstd[:, tt:tt + 1], ss[:, tt:tt + 1], AF.Sqrt, bias=EPS, scale=1.0 / D)
                kb.recip(rstd[:, tt:tt + 1], rstd[:, tt:tt + 1])
                xx = xn[tt % 2]
                kb.ts("dve", xx, X[:, tt, :], rstd[:, tt:tt + 1])
                for k4 in range(4):
                    pb = bk(b0 + k4 % 2).k(f"bk{b0 + k4 % 2}")
                    for j in range(4):
                        kc = k4 * 4 + j
                        kb.tr(pb[:, j * 128:(j + 1) * 128], xx[:, kc * 128:(kc + 1) * 128], ident)
                    src32 = None
                    if rt32 is not None:
                        src32 = rt32(tt, k4, pb, 0)
                    for j in range(4):
                        kc = k4 * 4 + j
                        src = pb[:, j * 128:(j + 1) * 128] if src32 is None else src32[:, kc, :]
                        kb.act(Hout[:, kc, tt * 128:(tt + 1) * 128], src, AF.Identity,
                               bias=Bcol[:, kc:kc + 1], scale=Acol[:, kc:kc + 1])
                    if rt32 is not None:
                        rt32(tt, k4, pb, 1)

        for sp_ in range(NT // SPA):
            t0 = sp_ * SPA
            for tt in range(SPA // 128):
                kb.dma("sp", xs[:, tt, :], x_d[t0 + tt * 128:t0 + (tt + 1) * 128, :], f"xa{tt}")
            norm_T(xs, SPA // 128, Am, Bm, hT, SPA, 0)
            kb.dma("sp", ssrow[:, 0, :], ssb_d[:, t0:t0 + SPA], "ssr0")
            kb.dma("sp", ssrow[:, 1, :], ssc_d[:, t0:t0 + SPA], "ssr1")
            for i in range(4):
                kb.dma("pool", yT[:, i, :], ya_d[i * 128:(i + 1) * 128, t0:t0 + SPA], "yTa")
            st = Stream(2)
            for i in range(12):
                c0 = 6144 + i * 128
                st.add(lambda c0=c0: wload(wgv[:, :, c0:c0 + 128], (16, 128)))
            for i in range(12):
                isb = i < 8
                hd = (i // 2) if isb else (i - 8)
                if (isb and i % 2 == 0) or (not isb):
                    pr = bk(2).k("bk2")
                    kb.mm(pr[:, 0:SPA], sel[:, hd * 128:(hd + 1) * 128], ssrow[:, 0 if isb else 1, :])
                    kb.act(rsb, pr[:, 0:SPA], AF.Sqrt, bias=EPS, scale=(1.0 / 256.0 if isb else 1.0 / 128.0))
                    kb.recip(rsb, rsb)
                src = ob_d[i * 128:(i + 1) * 128, t0:t0 + SPA] if isb else oc_d[(i - 8) * 128:(i - 7) * 128, t0:t0 + SPA]
                kb.dma("sp", otmp, src, "otmp")
                W = st.get(i)
                pg = bk(3).k("bk3")
                for kc in range(16):
                    kb.mm(pg[:, 0:SPA], W[:, kc, :], hT[:, kc, :], start=(kc == 0), stop=(kc == 15))
                kb.act(sgt, pg[:, 0:SPA], AF.Sigmoid if isb else AF.Silu)
                kb.tt("dve", otmp, otmp, rsb, ALU.mult)
                ncol = (P2_MLN + i) if isb else (P2_HGN + i - 8)
                kb.stt(yT[:, 4 + i, :], otmp, pc[:, ncol:ncol + 1], sgt, ALU.mult, ALU.mult)
            st = Stream(3)
            for dt in range(16):
                for br in range(3):
                    c0 = br * 2048 + dt * 128
                    st.add(lambda c0=c0: wload(wgv[:, :, c0:c0 + 128], (16, 128)))
            CH = ((0, 4), (4, 12), (12, 16))
            kb.dma("pool", pbuf[0], pav[:, :, 0:128], "pbuf0")
            for dt in range(16):
                if dt + 1 < 16:
                    kb.dma("pool", pbuf[(dt + 1) % 2], pav[:, :, (dt + 1) * 128:(dt + 2) * 128], f"pbuf{(dt + 1) % 2}")
                Wp = pbuf[dt % 2]
                Wg3 = [None, None, None]
                for br in range(3):
                    Wg3[br] = st.get(dt * 3 + br)
                    pg = bk(4 + br % 2).k(f"bk{4 + br % 2}")
                    for kc in range(16):
                        kb.mm(pg[:, 0:SPA], Wg3[br][:, kc, :], hT[:, kc, :], start=(kc == 0), stop=(kc == 15))
                    kb.act(sgt, pg[:, 0:SPA], AF.Sigmoid)
                    pq = bk(6 + br % 2).k(f"bk{6 + br % 2}")
                    lo, hi = CH[br]
                    for ch in range(lo, hi):
                        kb.mm(pq[:, 0:SPA], Wp[:, ch, :], yT[:, ch, :], start=(ch == lo), stop=(ch == hi - 1))
                    if br == 0:
                        kb.tt("dve", zacc, sgt, pq[:, 0:SPA], ALU.mult)
                    elif br == 1:
                        kb.tt("dve", ztmp, sgt, pq[:, 0:SPA], ALU.mult)
                        kb.tt("dve", zacc, zacc, ztmp, ALU.add)
                    else:
                        kb.tt("dve", ztmp, sgt, pq[:, 0:SPA], ALU.mult)
                        kb.tt("dve", zT[:, dt, :], zacc, ztmp, ALU.add)
            st = Stream(2)
            for p8 in range(8):
                st.add(lambda p8=p8: wload(wov[:, :, p8 * 256:(p8 + 1) * 256], (16, 256)))
            for p8 in range(8):
                W = st.get(p8)
                cs = slice(p8 * 256, (p8 + 1) * 256)
                for tt in range(SPA // 128):
                    po = bk(tt % 2).k(f"bk{tt % 2}")
                    for kc in range(16):
                        kb.mm(po[:, 0:256], zT[:, kc, tt * 128:(tt + 1) * 128], W[:, kc, :], start=(kc == 0), stop=(kc == 15))
                    kb.tt("dve", ztmp[:, 0:256], po[:, 0:256], gtm_b[:, cs], ALU.mult)
                    kb.tt("dve", xs[:, tt, cs], xs[:, tt, cs], ztmp[:, 0:256], ALU.add)
            for tt in range(SPA // 128):
                kb.dma("sp", xm_d[t0 + tt * 128:t0 + (tt + 1) * 128, :], xs[:, tt, :], f"xm{tt}")

        if a_only:
            kb.finish([xm_d])
            kb.barrier()
            es_a.close()
            return nc
        kb.barrier()
        es_a.close()
        kb.es = es_main
        actT_raw = sb("actT", [128, 8 * SPB], BF16)
        actT = V(actT_raw.ap.rearrange("p (a b) -> p a b", b=SPB), actT_raw.keys)
        xn = [V(actT_raw.ap[:, 0:2 * D].bitcast(F32), actT_raw.keys)] * 2
        junk = actT_raw[:, 2 * D:3 * D]
        ss = sb("ssb_", [128, 8])
        rstd = sb("rstdb", [128, 8])
        xacc = sb("xacc", [128, SPB // 128, D])
        hT2 = sb("hT2", [128, 16, SPB], BF16)
        xT32 = sb("xT32", [128, 16, 128])
        wr = sb("wr", [128, 16, 36])
        wr2 = wr
        rbrow = sb("rbrow", [128, 36])
        brow = sb("brow", [128, 36])
        lg = sb("lg", [128, 36])
        rt = sb("rt", [128, 48])
        em = sb("em", [128, 32])
        em2 = sb("em2", [128, 32])
        oh1 = sb("oh1", [128, 32])
        oh2 = sb("oh2", [128, 32])
        coef = sb("coef", [128, SPB // 128, 32])
        gsil = sb("gsil", [128, 512])
        dtmp = sb("dtmp", [128, 512])
        if P2CUT < 0:
            kb.memset("dve", xacc[:, 0, :], 1.0)
            kb.dma("sp", out_d[0:128, :], xacc[:, 0, :])
            kb.finish([out_d])
            return nc
        kb.dma("sp", wr, V(wr_d.ap.rearrange("(kc p) n -> p kc n", p=128), wr_d.keys), "wr")
        kb.dma("sp", rbrow, rb_d, "rb")
        pbr = bk(7).k("bk7")
        kb.cp("dve", xT32, V(Bf.ap.unsqueeze(2).to_broadcast([128, 16, 128]), Bf.keys))
        for kc in range(16):
            kb.mm(pbr[:, 0:36], xT32[:, kc, :], wr[:, kc, :], start=(kc == 0), stop=(kc == 15))
        kb.tt("dve", brow, pbr[:, 0:36], rbrow, ALU.add)
        kb.tt("dve", wr2, wr, V(Af.ap.unsqueeze(2).to_broadcast([128, 16, 36]), Af.keys), ALU.mult)
        egvs = [V(t.ap.rearrange("e (kc p) n -> p e kc n", p=128), t.keys) for t in eg_ds]
        euvs = [V(t.ap.rearrange("e (kc p) n -> p e kc n", p=128), t.keys) for t in eu_ds]
        edvs = [V(t.ap.rearrange("e (kc p) n -> p e kc n", p=128), t.keys) for t in ed_ds]

        if P2CUT == 0:
            kb.memset("dve", xacc[:, 0, :], 1.0)
            kb.cp("dve", xacc[:, 0, 0:36], brow)
            kb.dma("sp", out_d[0:128, :], xacc[:, 0, :])
            kb.finish([out_d])
            return nc
        for pb_ in range(NT // SPB):
            t0 = pb_ * SPB
            NTT = SPB // 128
            kb.wait_all("sp", [xm_d])
            for tt in range(NTT):
                kb.dma("sp", xacc[:, tt, :], xm_d[t0 + tt * 128:t0 + (tt + 1) * 128, :], f"xb{tt}")

            def rt32(tt, k4, pbank, phase):
                if phase == 0:
                    kb.cp("dve", V(xT32.ap[:, k4 * 4:(k4 + 1) * 4, :].rearrange("p a b -> p (a b)"), xT32.keys), pbank[:, 0:512])
                    return xT32
                if k4 == 3:
                    pl = bk(6).k("bk6")
                    for kc in range(16):
                        kb.mm(pl[:, 0:36], xT32[:, kc, :], wr2[:, kc, :], start=(kc == 0), stop=(kc == 15))
                    if P2CUT != 25:
                        route(tt, pl)
                    else:
                        kb.cp("dve", lg, pl[:, 0:36])

            def route(tt, pl):
                kb.tt("dve", lg, pl[:, 0:36], brow, ALU.add)
                r_ = lambda j: rt[:, j:j + 1]
                kb.op("dve", lambda: nc.vector.tensor_reduce(out=r_(0).ap, in_=lg[:, 0:4].ap, axis=AX.X, op=ALU.max),
                      [rt], [lg])
                kb.ts("dve", r_(1), r_(0), -1.0)
                kb.act(rt[:, 8:12], lg[:, 0:4], AF.Exp, bias=r_(1), accum=r_(2))
                kb.recip(r_(3), r_(2))
                kb.ts("dve", rt[:, 12:16], lg[:, 0:4], r_(0), None, ALU.is_equal)
                kb.ts("dve", rt[:, 16:20], rt[:, 12:16], 1e30, -1e30, ALU.mult, ALU.add)
                em3 = V(em.ap.rearrange("p (g e) -> p g e", e=8), em.keys)
                el3 = V(lg.ap[:, 4:36].rearrange("p (g e) -> p g e", e=8), lg.keys)
                pen = V(rt.ap[:, 16:20].unsqueeze(2).to_broadcast([128, 4, 8]), rt.keys)
                kb.tt("dve", em3, el3, pen, ALU.add)
                kb.op("dve", lambda: nc.vector.tensor_reduce(out=r_(4).ap, in_=em.ap, axis=AX.X, op=ALU.max), [rt], [em])
                kb.ts("dve", oh1, em, r_(4), None, ALU.is_equal)
                kb.stt(em2, oh1, -1e30, em, ALU.mult, ALU.add)
                kb.op("dve", lambda: nc.vector.tensor_reduce(out=r_(5).ap, in_=em2.ap, axis=AX.X, op=ALU.max), [rt], [em2])
                kb.ts("dve", oh2, em2, r_(5), None, ALU.is_equal)
                kb.ts("dve", r_(6), r_(4), -1.0)
                kb.act(r_(7), r_(5), AF.Exp, bias=r_(6))
                kb.ts("dve", r_(20), r_(7), 1.0, None, ALU.add)
                kb.recip(r_(21), r_(20))
                kb.tt("dve", r_(22), r_(7), r_(21), ALU.mult)
                kb.tt("dve", r_(23), r_(21), r_(3), ALU.mult)
                kb.tt("dve", r_(24), r_(22), r_(3), ALU.mult)
                kb.ts("dve", coef[:, tt, :], oh1, r_(23))
                kb.stt(coef[:, tt, :], oh2, r_(24), coef[:, tt, :], ALU.mult, ALU.add)

            if P2CUT >= 2:
                norm_T(xacc, NTT, Af, Bf, hT2, SPB, 4, rt32=(rt32 if (P2CUT >= 3 or P2CUT == 25) else None))

            st = Stream(2)
            for e in range(nexp):
                egv, euv, edv, el_ = egvs[e // nh], euvs[e // nh], edvs[e // nh], e % nh
                for q in range(4):
                    st.add(lambda v=egv, e=el_, q=q: wload(v[:, e, :, q * 256:(q + 1) * 256], (16, 256)))
                    st.add(lambda v=euv, e=el_, q=q: wload(v[:, e, :, q * 256:(q + 1) * 256], (16, 256)))
                for q in range(4):
                    st.add(lambda v=edv, e=el_, q=q: wload(v[:, e, :, q * 512:(q + 1) * 512], (8, 512)))
            for e in range(nexp if (P2CUT >= 4 and P2CUT != 25) else 0):
                base = e * 12
                for q in range(4):
                    Wg = st.get(base + 2 * q)
                    Wu = st.get(base + 2 * q + 1)
                    for f2 in range(2):
                        ft = q * 2 + f2
                        for hh in range(SPB // 512):
                            pgt = bk(2 * hh).k(f"bk{2 * hh}")
                            put = bk(2 * hh + 1).k(f"bk{2 * hh + 1}")
                            tsl = slice(hh * 512, (hh + 1) * 512)
                            for kc in range(16):
                                kb.mm(pgt, Wg[:, kc, f2 * 128:(f2 + 1) * 128], hT2[:, kc, tsl], start=(kc == 0), stop=(kc == 15))
                            for kc in range(16):
                                kb.mm(put, Wu[:, kc, f2 * 128:(f2 + 1) * 128], hT2[:, kc, tsl], start=(kc == 0), stop=(kc == 15))
                            kb.act(gsil, pgt, AF.Silu)
                            kb.tt("dve", actT[:, ft, tsl], gsil, put, ALU.mult)
                for q in range(4):
                    Wd = st.get(base + 8 + q)
                    cs = slice(q * 512, (q + 1) * 512)
                    for tt in range(NTT):
                        pd = bk(4 + tt % 2).k(f"bk{4 + tt % 2}")
                        for ft in range(8):
                            kb.mm(pd, actT[:, ft, tt * 128:(tt + 1) * 128], Wd[:, ft, :], start=(ft == 0), stop=(ft == 7))
                        kb.tt("dve", dtmp, pd, gtf_b[:, cs], ALU.mult)
                        kb.stt(xacc[:, tt, cs], dtmp, coef[:, tt, e:e + 1], xacc[:, tt, cs], ALU.mult, ALU.add)
            if final:
                fng_b = V(ring[0].ap.bitcast(F32), ring[0].keys)
                kb.dma("sp", fng_b, rowb_d[:, 2 * D:3 * D], "fng")
            for tt in range(NTT):
                if final:
                    kb.act(junk, xacc[:, tt, :], AF.Square, accum=ss[:, tt:tt + 1])
                    kb.act(rstd[:, tt:tt + 1], ss[:, tt:tt + 1], AF.Sqrt, bias=EPS, scale=1.0 / D)
                    kb.recip(rstd[:, tt:tt + 1], rstd[:, tt:tt + 1])
                    kb.stt(xacc[:, tt, :], xacc[:, tt, :], rstd[:, tt:tt + 1], fng_b, ALU.mult, ALU.mult)
                kb.dma("sp", out_d[t0 + tt * 128:t0 + (tt + 1) * 128, :], xacc[:, tt, :], f"xo{tt}")
        kb.finish([out_d])
    return nc


def p2_host_inputs(layer, inp, mod, final):
    l = layer
    w_in = inp["w_in"][l]
    o_rest = 1792
    o_mo = o_rest + 2048
    o_hg = o_rest + 3080 + 1536
    o_ga = o_rest + 3080 + 2048
    wg = np.ascontiguousarray(np.concatenate([w_in[:, o_ga:o_ga + 6144], w_in[:, o_mo:o_mo + 1024],
                                              w_in[:, o_hg:o_hg + 512]], axis=1))
    pabc = np.ascontiguousarray(np.concatenate([inp["p_a"][l], inp["p_b"][l], inp["p_c"][l]], axis=0))
    m = mod[l]
    sh_m, sc_m, gt_m, sh_f, sc_f, gt_f = [m[i * D:(i + 1) * D] for i in range(6)]
    colz = lambda v: v.reshape(-1, 128).T
    pc = np.zeros((128, P2C_N), np.float32)
    pc[:, P2_GM:P2_GM + 16] = colz(inp["norm_mix"][l])
    pc[:, P2_SCM:P2_SCM + 16] = colz(sc_m)
    pc[:, P2_SHM:P2_SHM + 16] = colz(sh_m)
    pc[:, P2_GF:P2_GF + 16] = colz(inp["norm_ffn"][l])
    pc[:, P2_SCF:P2_SCF + 16] = colz(sc_f)
    pc[:, P2_SHF:P2_SHF + 16] = colz(sh_f)
    pc[:, P2_MLN:P2_MLN + 8] = colz(inp["ml_norm"][l])
    pc[:, P2_HGN:P2_HGN + 4] = colz(inp["hg_norm"][l])
    rowb = np.ascontiguousarray(np.broadcast_to(np.concatenate([gt_m, gt_f, inp["final_norm"]])[None, :], (128, 3 * D)))
    sel = np.zeros((8, 4, 128), np.float32)
    for h in range(4):
        sel[2 * h:2 * h + 2, h, :] = 1.0
    return {
        "wg": wg, "pabc": pabc, "wout": np.ascontiguousarray(inp["w_out"][l]),
        "wr": np.ascontiguousarray(np.concatenate([inp["moe_gw"][l], inp["moe_ew"][l]], axis=1)),
        "rb": np.ascontiguousarray(np.broadcast_to(np.concatenate([inp["moe_gb"][l], inp["moe_eb"][l]])[None, :], (128, 36))),
        "eg0": inp["ex_gate"][l][:16], "eg1": inp["ex_gate"][l][16:],
        "eu0": inp["ex_up"][l][:16], "eu1": inp["ex_up"][l][16:],
        "ed0": inp["ex_down"][l][:16], "ed1": inp["ex_down"][l][16:],
        "pc2": pc, "rowb": rowb.astype(np.float32), "ident": np.eye(128, dtype=np.float32),
        "sel": sel.reshape(8, 512),
    }


_CACHE = {}


def _prog(key, fn):
    if key not in _CACHE:
        _CACHE[key] = fn()
    return _CACHE[key]


def kernel(**inp):
    inp = {k: np.asarray(v) for k, v in inp.items()}
    T = SEQ
    cores = list(range(NCORES))
    c_col = np.ascontiguousarray(inp["c"][0].reshape(16, 128).T)
    adaw = inp["ada_w"]
    adab = inp["ada_b"]
    in_maps = []
    for c in cores:
        l, part = c // 4, c % 4
        cols = slice(part * 3072, (part + 1) * 3072)
        in_maps.append({"c": c_col, "w": np.ascontiguousarray(adaw[l][:, cols]),
                        "b": np.ascontiguousarray(adab[l][cols].reshape(24, 128).T)})
    res = run_bass_kernel_spmd(_prog("p0", build_p0), in_maps, core_ids=cores)
    mod = [np.concatenate([res.results[l * 4 + p]["mod"].T.reshape(-1) for p in range(4)]) for l in range(2)]

    x = np.ascontiguousarray(inp["x"][0])
    vfirst = None
    for l in range(2):
        in_maps = []
        for c in cores:
            m_ = p1_host_inputs(l, inp, mod, c, T)
            m_["x"] = x
            if l == 1:
                m_["vfirst"] = vfirst[c]
            in_maps.append(m_)
        r1 = run_bass_kernel_spmd(_prog(("p1", l), lambda: build_p1(l, T)), in_maps, core_ids=cores).results
        if l == 0:
            vfirst = [np.ascontiguousarray(r1[c]["vT"]) for c in cores]
        yaT = np.concatenate([r1[c]["yaT"] for c in cores], axis=0)
        obT = np.concatenate([r1[c]["obT"] for c in cores], axis=0)
        ocT = np.concatenate([r1[c]["ocT"] for c in cores], axis=0)
        ssb = np.stack([r1[c]["ssb"].T.reshape(-1) for c in cores], axis=0)
        ssc = np.stack([r1[c]["ssc"].T.reshape(-1) for c in cores], axis=0)
        common = p2_host_inputs(l, inp, mod, l == 1)
        in_maps = []
        for c in cores:
            ts_ = slice(c * NT2, (c + 1) * NT2)
            m_ = dict(common)
            m_["x"] = np.ascontiguousarray(x[ts_])
            m_["yaT"] = np.ascontiguousarray(yaT[:, ts_])
            m_["obT"] = np.ascontiguousarray(obT[:, ts_])
            m_["ocT"] = np.ascontiguousarray(ocT[:, ts_])
            m_["ssb"] = np.ascontiguousarray(ssb[:, ts_])
            m_["ssc"] = np.ascontiguousarray(ssc[:, ts_])
            in_maps.append(m_)
        p2 = _prog(("p2", l == 1), lambda: build_p2(l == 1))
        r2 = []
        r2 += run_bass_kernel_spmd(p2, in_maps, core_ids=cores).results
        x = np.ascontiguousarray(np.concatenate([r2[c]["xout"] for c in cores], axis=0))
    return x[None].astype(np.float32)
```
